# Optimizing a Trainium2 kernel written in Bass

```python
import jax, jax.numpy as jnp
from jax import lax
import numpy as np

D_MODEL = 1024
BATCH = 8
SEQ = 4096
DEPTH = 2

N_A = DEPTH // 2
N_B = DEPTH - N_A
N_DENSE = (DEPTH + 1) // 2
N_MOE = DEPTH // 2

GLA_HEADS = 4
GLA_DK = D_MODEL // 2 // GLA_HEADS
GLA_DV = D_MODEL // GLA_HEADS
GLA_QK = GLA_HEADS * GLA_DK
GLA_V = GLA_HEADS * GLA_DV
GLA_GATE_RANK = 16
GLA_GATE_NORM = 16.0
GLA_CHUNK = 64
GLA_IN = 2 * GLA_QK + GLA_V + GLA_GATE_RANK + GLA_V

SB_HEADS = 8
SB_HEAD_DIM = D_MODEL // SB_HEADS
SB_BLOCK = 128

FFN_DIM = 2816
N_EXPERTS = 8
TOP_K = 2
EXPERT_DIM = 3584

RMS_EPS = 1e-6

kernel_name = "yoco_gla_stickbreaking_moe"


def rms_norm(x, g):
    xf = x.astype(jnp.float32)
    y = xf * lax.rsqrt(jnp.mean(xf * xf, axis=-1, keepdims=True) + RMS_EPS)
    return (y * g.astype(jnp.float32)).astype(x.dtype)


def swiglu(h, w_gate, w_up, w_down):
    return (jax.nn.silu(h @ w_gate) * (h @ w_up)) @ w_down


def gla_mixer(h, w_in, w_gate_up, b_gate, g_head, w_out):
    B, S, _ = h.shape
    H, dk, dv, C = GLA_HEADS, GLA_DK, GLA_DV, GLA_CHUNK
    nc = S // C
    proj = h @ w_in
    q, k, v, g_lr, r = jnp.split(
        proj, [GLA_QK, 2 * GLA_QK, 2 * GLA_QK + GLA_V, 2 * GLA_QK + GLA_V + GLA_GATE_RANK], axis=-1)
    log_a = jax.nn.log_sigmoid((g_lr @ w_gate_up + b_gate).astype(jnp.float32)) / GLA_GATE_NORM

    def to_chunks(t, d):
        return t.reshape(B, nc, C, H, d).transpose(1, 0, 3, 2, 4).astype(jnp.float32)

    qc = to_chunks(q, dk) * (dk ** -0.5)
    kc = to_chunks(k, dk)
    vc = to_chunks(v, dv)
    gc = to_chunks(log_a, dk)
    b = jnp.cumsum(gc, axis=3)
    b_end = b[:, :, :, -1:, :]
    q_dec = qc * jnp.exp(b)
    k_inv = kc * jnp.exp(-b)
    k_end = kc * jnp.exp(b_end - b)
    decay_end = jnp.exp(b_end[:, :, :, 0, :])

    tri = jnp.tril(jnp.ones((C, C), dtype=bool))
    attn = jnp.where(tri, jnp.einsum('nbhid,nbhjd->nbhij', q_dec, k_inv), 0.0)
    o_intra = jnp.einsum('nbhij,nbhjv->nbhiv', attn, vc)

    def step(state, inp):
        q_n, k_n, v_n, dec_n = inp
        o_n = jnp.einsum('bhid,bhdv->bhiv', q_n, state)
        state = dec_n[..., None] * state + jnp.einsum('bhid,bhiv->bhdv', k_n, v_n)
        return state, o_n

    state0 = jnp.zeros((B, H, dk, dv), dtype=jnp.float32)
    _, o_inter = lax.scan(step, state0, (q_dec, k_end, vc, decay_end))
    o = (o_inter + o_intra).transpose(1, 0, 3, 2, 4).reshape(B, S, H, dv)
    o = rms_norm(o, g_head).reshape(B, S, GLA_V)
    o = (o * jax.nn.silu(r.astype(jnp.float32))).astype(h.dtype)
    return o @ w_out


def stick_breaking_mixer(h, k, v, w_q, w_out):
    B, S, _ = h.shape
    H, hd = SB_HEADS, SB_HEAD_DIM
    q = (h @ w_q).reshape(B, S, H, hd).transpose(0, 2, 1, 3)
    scale = hd ** -0.5
    outs = []
    for blk in range(S // SB_BLOCK):
        t0 = blk * SB_BLOCK
        t1 = t0 + SB_BLOCK
        qb = q[:, :, t0:t1]
        kb = k[:, :, :t1]
        vb = v[:, :, :t1]
        z = jnp.einsum('bhtd,bhsd->bhts', qb, kb).astype(jnp.float32) * scale
        t_idx = t0 + jnp.arange(SB_BLOCK)[:, None]
        s_idx = jnp.arange(t1)[None, :]
        causal = s_idx < t_idx
        log_beta = jax.nn.log_sigmoid(z)
        log_keep = jnp.where(causal, jax.nn.log_sigmoid(-z), 0.0)
        log_stay = lax.cumsum(log_keep, axis=3, reverse=True) - log_keep
        w = jnp.where(causal, jnp.exp(log_beta + log_stay), 0.0)
        outs.append(jnp.einsum('bhts,bhsd->bhtd', w.astype(vb.dtype), vb))
    o = jnp.concatenate(outs, axis=2).transpose(0, 2, 1, 3).reshape(B, S, H * hd)
    return o @ w_out


def moe_swiglu(h, w_router, w_gate, w_up, w_down):
    logits = (h @ w_router).astype(jnp.float32)
    top_val, top_idx = lax.top_k(logits, TOP_K)
    top_w = jax.nn.softmax(top_val, axis=-1)
    gates = jnp.sum(jax.nn.one_hot(top_idx, N_EXPERTS, dtype=jnp.float32) * top_w[..., None], axis=-2)
    out = jnp.zeros_like(h)
    for e in range(N_EXPERTS):
        y = swiglu(h, w_gate[e], w_up[e], w_down[e])
        out = out + gates[..., e:e + 1].astype(h.dtype) * y
    return out


def setup_inputs(seed: int = 0) -> dict:
    key = jax.random.key(seed)
    ks = jax.random.split(key, 20)
    D = D_MODEL

    def w(k, shape, fan_in):
        return jax.random.normal(k, shape, dtype=jnp.float32) * (fan_in ** -0.5)

    def gain(k, shape):
        return 1.0 + 0.02 * jax.random.normal(k, shape, dtype=jnp.float32)

    return {
        "x": jax.random.normal(ks[0], (BATCH, SEQ, D), dtype=jnp.float32),
        "attn_norm": gain(ks[1], (DEPTH, D)),
        "ffn_norm": gain(ks[2], (DEPTH, D)),
        "kv_norm": gain(ks[3], (D,)),
        "final_norm": gain(ks[4], (D,)),
        "gla_w_in": w(ks[5], (N_A, D, GLA_IN), D),
        "gla_w_gate_up": w(ks[6], (N_A, GLA_GATE_RANK, GLA_QK), GLA_GATE_RANK),
        "gla_b_gate": 0.1 * jax.random.normal(ks[7], (N_A, GLA_QK), dtype=jnp.float32),
        "gla_head_norm": gain(ks[8], (N_A, GLA_DV)),
        "gla_w_out": w(ks[9], (N_A, GLA_V, D), GLA_V),
        "sb_w_kv": w(ks[10], (D, 2 * SB_HEADS * SB_HEAD_DIM), D),
        "sb_w_q": w(ks[11], (N_B, D, SB_HEADS * SB_HEAD_DIM), D),
        "sb_w_out": w(ks[12], (N_B, SB_HEADS * SB_HEAD_DIM, D), SB_HEADS * SB_HEAD_DIM),
        "ffn_w_gate": w(ks[13], (N_DENSE, D, FFN_DIM), D),
        "ffn_w_up": w(ks[14], (N_DENSE, D, FFN_DIM), D),
        "ffn_w_down": w(ks[15], (N_DENSE, FFN_DIM, D), FFN_DIM),
        "moe_w_router": w(ks[16], (N_MOE, D, N_EXPERTS), D),
        "moe_w_gate": w(ks[17], (N_MOE, N_EXPERTS, D, EXPERT_DIM), D),
        "moe_w_up": w(ks[18], (N_MOE, N_EXPERTS, D, EXPERT_DIM), D),
        "moe_w_down": w(ks[19], (N_MOE, N_EXPERTS, EXPERT_DIM, D), EXPERT_DIM),
    }


def reference(x, attn_norm, ffn_norm, kv_norm, final_norm, gla_w_in, gla_w_gate_up, gla_b_gate,
              gla_head_norm, gla_w_out, sb_w_kv, sb_w_q, sb_w_out, ffn_w_gate, ffn_w_up, ffn_w_down,
              moe_w_router, moe_w_gate, moe_w_up, moe_w_down):
    B, S, D = x.shape
    h = x
    k_shared = None
    v_shared = None
    for layer in range(DEPTH):
        if layer < N_A:
            h = h + gla_mixer(rms_norm(h, attn_norm[layer]), gla_w_in[layer], gla_w_gate_up[layer],
                              gla_b_gate[layer], gla_head_norm[layer], gla_w_out[layer])
        else:
            if layer == N_A:
                kv = rms_norm(h, kv_norm) @ sb_w_kv
                k_s, v_s = jnp.split(kv, 2, axis=-1)
                k_shared = k_s.reshape(B, S, SB_HEADS, SB_HEAD_DIM).transpose(0, 2, 1, 3)
                v_shared = v_s.reshape(B, S, SB_HEADS, SB_HEAD_DIM).transpose(0, 2, 1, 3)
            ib = layer - N_A
            h = h + stick_breaking_mixer(rms_norm(h, attn_norm[layer]), k_shared, v_shared,
                                         sb_w_q[ib], sb_w_out[ib])
        hn = rms_norm(h, ffn_norm[layer])
        if layer % 2 == 0:
            i = layer // 2
            h = h + swiglu(hn, ffn_w_gate[i], ffn_w_up[i], ffn_w_down[i])
        else:
            i = layer // 2
            h = h + moe_swiglu(hn, moe_w_router[i], moe_w_gate[i], moe_w_up[i], moe_w_down[i])
    return rms_norm(h, final_norm)
```

```python
import math
import numpy as np
from contextlib import ExitStack
import concourse.bass as bass
import concourse.mybir as mybir
from concourse.bass_utils import run_bass_kernel_spmd

F32 = mybir.dt.float32
BF16 = mybir.dt.bfloat16
AF = mybir.ActivationFunctionType
ALU = mybir.AluOpType

S_LEN = 4096
D = 1024
NT = S_LEN // 128
EPS = 1e-6
GLA_IN = 3088
FFN_DIM = 2816
EXPERT_DIM = 3584
NEXP = 8


class Buf:
    __slots__ = ("w", "r", "name")

    def __init__(self, name=""):
        self.w = []
        self.r = {}
        self.name = name


class _Eng:
    def __init__(self, name, h, sem, sid):
        self.name, self.h, self.sem, self.sid = name, h, sem, sid
        self.cnt = 0
        self.ops = []
        self.seen = {}


class Sched:
    NDMA = 12

    def __init__(self, nc, es):
        self.nc = nc
        self.sems = []
        self.eng = {}
        for name, h in [("pe", nc.tensor), ("act", nc.scalar), ("dve", nc.vector),
                        ("pool", nc.gpsimd), ("sp", nc.sync)]:
            sem = es.enter_context(nc.semaphore("s_" + name))
            self.sems.append(sem)
            self.eng[name] = _Eng(name, h, sem, len(self.sems) - 1)
        self.dq = {}
        for q in ("sp", "act", "pool"):
            lst = []
            for i in range(self.NDMA):
                sem = es.enter_context(nc.semaphore("d_%s%d" % (q, i)))
                self.sems.append(sem)
                lst.append([len(self.sems) - 1, 0])
            self.dq[q] = [lst, 0]

    def _wait(self, e, tok):
        if tok is None:
            return
        sid, v = tok
        if e.name == "pe" and sid == e.sid:
            return
        if e.seen.get(sid, 0) >= v:
            return
        e.seen[sid] = v
        sem = self.sems[sid]
        e.ops.append(lambda h, sem=sem, v=v: h.wait_ge(sem, v))

    def _deps(self, e, reads, writes, xr=(), dma_fill=False):
        for b in reads:
            for tk in b.w:
                self._wait(e, tk)
        for b in xr:
            for tk in b.w:
                self._wait(e, tk)
            for sid, v in list(b.r.items()):
                self._wait(e, (sid, v))
        for b in writes:
            if dma_fill and not b.r and b.w and all(sid >= 5 for sid, _ in b.w):
                continue
            for tk in b.w:
                self._wait(e, tk)
            for sid, v in list(b.r.items()):
                self._wait(e, (sid, v))

    def op(self, eng, fn, reads=(), writes=(), inc=True, xr=()):
        e = self.eng[eng]
        self._deps(e, reads, writes, xr)
        reads = list(reads) + list(xr)
        if inc:
            e.cnt += 1
            v = e.cnt
            sem = e.sem
            e.ops.append(lambda h, fn=fn, sem=sem: fn(h).then_inc(sem, 1))
        else:
            v = e.cnt + 1
            e.ops.append(lambda h, fn=fn: fn(h))
        tok = (e.sid, v)
        for b in writes:
            b.w = [tok]
            b.r = {}
        for b in reads:
            b.r[e.sid] = v

    def dma(self, q, out, in_, reads=(), writes=(), fn=None, **kw):
        e = self.eng[q]
        fill = {id(b): (not b.r and bool(b.w) and all(sid >= 5 for sid, _ in b.w)) for b in writes}
        self._deps(e, reads, writes, dma_fill=True)
        lst, idx = self.dq[q]
        self.dq[q][1] = (idx + 1) % len(lst)
        ent = lst[idx]
        sid = ent[0]
        if ent[1] > 0:
            self._wait(e, (sid, ent[1]))
        ent[1] += 16
        v = ent[1]
        sem = self.sems[sid]
        if fn is None:
            e.ops.append(lambda h, out=out, in_=in_, sem=sem, kw=kw:
                         h.dma_start(out=out, in_=in_, **kw).then_inc(sem, 16))
        else:
            e.ops.append(lambda h, fn=fn, sem=sem: fn(h).then_inc(sem, 16))
        tok = (sid, v)
        for b in writes:
            if fill[id(b)]:
                b.w = [t for t in b.w if t[0] != sid] + [tok]
            else:
                b.w = [tok]
            b.r = {}
        for b in reads:
            b.r[sid] = v

    def barrier(self):
        toks = []
        for e in self.eng.values():
            if e.cnt > 0:
                toks.append((e.sid, e.cnt))
        for q, (lst, _) in self.dq.items():
            for sid, v in lst:
                if v > 0:
                    toks.append((sid, v))
        for e in self.eng.values():
            for t in toks:
                self._wait(e, t)

    def emit(self):
        nc = self.nc
        self.barrier()
        with nc.Block() as block:
            @block.sync
            def _(h):
                for f in self.eng["sp"].ops:
                    f(h)

            @block.tensor
            def _(h):
                for f in self.eng["pe"].ops:
                    f(h)

            @block.scalar
            def _(h):
                for f in self.eng["act"].ops:
                    f(h)

            @block.vector
            def _(h):
                for f in self.eng["dve"].ops:
                    f(h)

            @block.gpsimd
            def _(h):
                for f in self.eng["pool"].ops:
                    f(h)


class KB:
    def __init__(self, nc, es):
        self.nc = nc
        self.es = es
        self.S = Sched(nc, es)
        self.banks = [es.enter_context(nc.psum_tensor("pb%d" % i, [128, 512], F32)) for i in range(8)]
        self.PB = [Buf("pb%d" % i) for i in range(8)]
        self.n = 0

    def sb(self, st, shape, dt, name=None):
        self.n += 1
        return st.enter_context(self.nc.sbuf_tensor("%s_%d" % (name or "t", self.n), shape, dt))

    def consts(self):
        S, es = self.S, self.es
        self.identf = self.sb(es, [128, 128], F32, "identf")
        self.ident = self.sb(es, [128, 128], BF16, "ident")
        self.trif = self.sb(es, [128, 128], F32, "trif")
        self.epst = self.sb(es, [128, 1], F32, "eps")
        self.bconst = Buf("const")
        bc = self.bconst
        identf, ident, trif, epst = self.identf, self.ident, self.trif, self.epst
        S.op("pool", lambda h: h.memset(identf[:], 0.0), writes=[bc])
        S.op("pool", lambda h: h.affine_select(out=identf[:], in_=identf[:], pattern=[[-1, 128]],
                                               compare_op=ALU.not_equal, fill=1.0, base=0, channel_multiplier=1),
             writes=[bc])
        S.op("dve", lambda h: h.tensor_copy(out=ident[:], in_=identf[:]), writes=[bc])
        S.op("pool", lambda h: h.memset(trif[:], 1.0), writes=[bc])
        S.op("pool", lambda h: h.affine_select(out=trif[:], in_=trif[:], pattern=[[1, 128]],
                                               compare_op=ALU.is_ge, fill=0.0, base=0, channel_multiplier=-1),
             writes=[bc])
        S.op("pool", lambda h: h.memset(trif[0:64, 64:128], 0.0), writes=[bc])
        S.op("dve", lambda h: h.memset(epst[:], EPS), writes=[bc])

    def norm_T(self, st_tmp, xt, bx, outs):
        S = self.S
        junk, bj, ss, bss, xn, bxn = st_tmp
        S.op("act", lambda h: h.activation(out=junk[:], in_=xt, func=AF.Square, accum_out=ss[:, 0:1]),
             reads=[bx], writes=[bj, bss])
        S.op("act", lambda h: h.activation(out=ss[:, 1:2], in_=ss[:, 0:1], func=AF.Sqrt, scale=1.0 / D,
                                           bias=self.epst[:, 0:1]), reads=[self.bconst], writes=[bss])
        S.op("dve", lambda h: h.reciprocal(out=ss[:, 2:3], in_=ss[:, 1:2]), writes=[bss])
        pT = self.banks[0][:].bitcast(BF16)
        for (g, bgain, dst, bdst) in outs:
            S.op("dve", lambda h, g=g: h.scalar_tensor_tensor(out=xn[:], in0=xt, scalar=ss[:, 2:3], in1=g,
                                                            op0=ALU.mult, op1=ALU.mult),
                 reads=[bx, bss, bgain, self.bconst], writes=[bxn])
            for kc in range(8):
                S.op("pe", lambda h, kc=kc: h.transpose(out=pT[:, kc * 128:(kc + 1) * 128],
                                                        in_=xn[:, kc * 128:(kc + 1) * 128], identity=self.ident[:]),
                     reads=[bxn, self.bconst], writes=[self.PB[0]], inc=(kc == 7))
            S.op("act", lambda h, dst=dst: h.copy(out=dst, in_=pT.rearrange("p (a b) -> p a b", a=8)),
                 xr=[self.PB[0]], writes=[bdst])

    def norm_tmp(self, st):
        return (self.sb(st, [128, 1024], BF16, "junk"), Buf("junk"), self.sb(st, [128, 4], F32, "ss"), Buf("ss"),
                self.sb(st, [128, 1024], BF16, "xn"), Buf("xn"))

    def load_w_bf16(self, dst, src, bdst, kcs, ncols, c0=0):
        for kc in range(kcs):
            self.S.dma("pool", dst[:, kc, :], src[kc * 128:(kc + 1) * 128, c0:c0 + ncols], writes=[bdst])


def stage_gla(kb, x, h1, attn_norm0, w_in, w_gu, b_gate, g_head, w_out, ntiles=NT):
    nc, S, banks, PB = kb.nc, kb.S, kb.banks, kb.PB
    with ExitStack() as st:
        sb = lambda shape, dt, name=None: kb.sb(st, shape, dt, name)
        w_in_sb = sb([128, 8, GLA_IN], BF16, "w_in")
        w_out_sb = sb([128, 8, 1024], BF16, "w_out")
        bw = Buf("w")
        kb.load_w_bf16(w_in_sb, w_in, bw, 8, GLA_IN)
        kb.load_w_bf16(w_out_sb, w_out, bw, 8, 1024)
        wgu = sb([17, 512], F32, "wgu")
        S.dma("sp", wgu[0:16, :], w_gu, writes=[bw])
        S.dma("sp", wgu[16:17, :], b_gate, writes=[bw])
        g_attn = sb([128, 1024], F32, "g_attn")
        S.dma("sp", g_attn[:], attn_norm0.partition_broadcast(128), writes=[bw])
        g_hd = sb([128, 256], F32, "g_hd")
        S.dma("sp", g_hd[:], g_head.partition_broadcast(128), writes=[bw])
        ntmp = kb.norm_tmp(st)
        xts = [sb([128, 1024], F32, "xt") for _ in range(2)]
        bxts = [Buf("xt") for _ in range(2)]
        xnT = sb([128, 8, 128], BF16, "xnT"); bxnT = Buf("xnT")
        v_bf = sb([128, 1024], BF16, "v"); bv = Buf("v")
        sr = sb([128, 1024], F32, "sr"); bsr = Buf("sr")
        glrT = sb([17, 128], F32, "glrT"); bglr = Buf("glrT")
        S.op("dve", lambda h: h.memset(glrT[:], 1.0), writes=[bglr])
        e1 = sb([128, 512], F32, "e1"); be1 = Buf("e1")
        lap = sb([128, 512], F32, "lap"); blap = Buf("lap")
        ek_tm = sb([128, 512], F32, "ek_tm"); bek = Buf("ek_tm")
        kinv_tm = sb([128, 512], BF16, "kinv_tm"); bkinv = Buf("kinv_tm")
        eq = sb([128, 512], F32, "eq"); beq = Buf("eq")
        ekT = sb([128, 512], F32, "ekT"); bekT = Buf("ekT")
        dec = sb([128, 4, 2], F32, "dec"); bdec = Buf("dec")
        q_even = sb([128, 4, 128], BF16, "q_even"); q_odd = sb([128, 4, 128], BF16, "q_odd"); bq = Buf("q")
        S.op("dve", lambda h: h.memset(q_even[:], 0.0), writes=[bq])
        S.op("dve", lambda h: h.memset(q_odd[:], 0.0), writes=[bq])
        kinvT = sb([128, 4, 128], BF16, "kinvT"); bkT = Buf("kinvT")
        attnT = sb([128, 4, 128], BF16, "attnT"); battn = Buf("attnT")
        Sf = sb([128, 4, 256], F32, "Sf"); bSf = Buf("Sf")
        Sb = sb([128, 4, 256], BF16, "Sb"); bSb = Buf("Sb")
        Sm = sb([128, 4, 256], BF16, "Sm"); bSm = Buf("Sm")
        t1 = sb([128, 256], F32, "t1"); bt1 = Buf("t1")
        S.op("dve", lambda h: h.memset(Sf[:], 0.0), writes=[bSf])
        S.op("dve", lambda h: h.memset(Sb[:], 0.0), writes=[bSb])
        hs = sb([128, 8], F32, "hs"); bhs = Buf("hs")
        og = sb([128, 256], F32, "og"); bog = Buf("og")
        og_bf = sb([128, 1024], BF16, "og_bf"); bogb = Buf("og_bf")
        ogT = sb([128, 8, 128], BF16, "ogT"); bogT = Buf("ogT")
        h1t = sb([128, 1024], F32, "h1t"); bh1 = Buf("h1t")
        junk2 = sb([128, 256], BF16, "junk2"); bj2 = Buf("junk2")
        pTb = banks[0][:].bitcast(BF16)
        qscale = math.log(128.0 ** -0.5)
        lnq = sb([128, 1], F32, "lnq")
        S.op("dve", lambda h: h.memset(lnq[:], qscale), writes=[kb.bconst])

        for t in range(ntiles):
            xt = xts[t % 2]; bx = bxts[t % 2]
            S.dma("sp", xt[:], x[t * 128:(t + 1) * 128, :], writes=[bx])
            kb.norm_T(ntmp, xt[:], bx, [(g_attn[:], bw, xnT[:], bxnT)])

            def proj_tm(bank, c0, n=512):
                for kc in range(8):
                    S.op("pe", lambda h, kc=kc: h.matmul(banks[bank][:, 0:n], lhsT=xnT[:, kc, :], rhs=w_in_sb[:, kc, c0:c0 + n],
                                                         start=(kc == 0), stop=(kc == 7)),
                         reads=[bxnT, bw], writes=[PB[bank]], inc=(kc == 7))

            def proj_fm(bank, c0, m, col0):
                for kc in range(8):
                    S.op("pe", lambda h, kc=kc: h.matmul(banks[bank][0:m, col0:col0 + 128], lhsT=w_in_sb[:, kc, c0:c0 + m],
                                                         rhs=xnT[:, kc, :], start=(kc == 0), stop=(kc == 7)),
                         reads=[bxnT, bw], writes=[PB[bank]], inc=(kc == 7))

            proj_tm(1, 512)
            proj_tm(2, 1024); proj_tm(3, 1536)
            S.op("act", lambda h: h.copy(out=v_bf[:, 0:512], in_=banks[2][:]), xr=[PB[2]], writes=[bv])
            S.op("act", lambda h: h.copy(out=v_bf[:, 512:1024], in_=banks[3][:]), xr=[PB[3]], writes=[bv])
            proj_tm(2, 2064); proj_tm(3, 2576)
            S.op("act", lambda h: h.activation(out=sr[:, 0:512], in_=banks[2][:], func=AF.Silu), xr=[PB[2]], writes=[bsr])
            S.op("act", lambda h: h.activation(out=sr[:, 512:1024], in_=banks[3][:], func=AF.Silu), xr=[PB[3]], writes=[bsr])
            proj_fm(6, 2048, 16, 0)
            S.op("dve", lambda h: h.tensor_copy(out=glrT[0:16, :], in_=banks[6][0:16, 0:128]), xr=[PB[6]], writes=[bglr])
            for hh in range(4):
                proj_fm(4, hh * 128, 128, hh * 128)
            for hh in range(4):
                proj_fm(5, 512 + hh * 128, 128, hh * 128)
            S.op("pe", lambda h: h.matmul(banks[6][:], lhsT=glrT[:, :], rhs=wgu[:, :], start=True, stop=True, skip_group_check=True),
                 reads=[bglr, bw], writes=[PB[6]])
            S.op("act", lambda h: h.activation(out=e1[:], in_=banks[6][:], func=AF.Exp, scale=-1.0), xr=[PB[6]], writes=[be1])
            S.op("act", lambda h: h.activation(out=lap[:], in_=e1[:], func=AF.Ln, bias=1.0), reads=[be1], writes=[blap])
            S.op("pe", lambda h: h.matmul(banks[6][:], lhsT=kb.trif[:], rhs=lap[:], start=True, stop=True, skip_group_check=True),
                 reads=[blap, kb.bconst], writes=[PB[6]])
            S.op("act", lambda h: h.activation(out=ek_tm[:], in_=banks[6][:], func=AF.Exp, scale=1.0 / 16), xr=[PB[6]], writes=[bek])
            S.op("dve", lambda h: h.tensor_tensor(out=kinv_tm[:], in0=banks[1][:], in1=ek_tm[:], op=ALU.mult),
                 xr=[PB[1]], reads=[bek], writes=[bkinv])
            for hh in range(4):
                S.op("pe", lambda h, hh=hh: h.matmul(banks[7][:, hh * 128:(hh + 1) * 128], lhsT=lap[:, hh * 128:(hh + 1) * 128],
                                                     rhs=kb.trif[:], start=True, stop=True, skip_group_check=True),
                     reads=[blap, kb.bconst], writes=[PB[7]], inc=(hh == 3))
            S.op("act", lambda h: h.activation(out=eq[:], in_=banks[7][:], func=AF.Exp, scale=-1.0 / 16, bias=lnq[:, 0:1]),
                 xr=[PB[7]], reads=[kb.bconst], writes=[beq])
            S.op("act", lambda h: h.activation(out=ekT[:], in_=banks[7][:], func=AF.Exp, scale=1.0 / 16), xr=[PB[7]], writes=[bekT])
            cbv = banks[7][:].rearrange("p (h c i) -> p h c i", h=4, c=2)
            S.op("act", lambda h: h.activation(out=dec[:], in_=cbv[:, :, :, 63], func=AF.Exp, scale=-1.0 / 16),
                 xr=[PB[7]], writes=[bdec])
            qv = banks[4][:].rearrange("p (h i) -> p h i", h=4)
            eqv = eq[:].rearrange("p (h i) -> p h i", h=4)
            S.op("dve", lambda h: h.tensor_tensor(out=q_even[:, :, 0:64], in0=qv[:, :, 0:64], in1=eqv[:, :, 0:64], op=ALU.mult),
                 xr=[PB[4]], reads=[beq], writes=[bq])
            S.op("dve", lambda h: h.tensor_tensor(out=q_odd[:, :, 64:128], in0=qv[:, :, 64:128], in1=eqv[:, :, 64:128], op=ALU.mult),
                 xr=[PB[4]], reads=[beq], writes=[bq])
            S.op("dve", lambda h: h.tensor_tensor(out=kinvT[:].rearrange("p h i -> p (h i)"), in0=banks[5][:], in1=ekT[:], op=ALU.mult),
                 xr=[PB[5]], reads=[bekT], writes=[bkT])
            for hh in range(4):
                S.op("pe", lambda h, hh=hh: h.matmul(banks[7][:, hh * 128:(hh + 1) * 128], lhsT=kinvT[:, hh, :], rhs=q_even[:, hh, :],
                                                     start=True, stop=False), reads=[bkT, bq], writes=[PB[7]], inc=False)
                S.op("pe", lambda h, hh=hh: h.matmul(banks[7][:, hh * 128:(hh + 1) * 128], lhsT=kinvT[:, hh, :], rhs=q_odd[:, hh, :],
                                                     start=False, stop=True), reads=[bkT, bq], writes=[PB[7]], inc=(hh == 3))
            for hh in range(4):
                S.op("dve", lambda h, hh=hh: h.tensor_tensor(out=attnT[:, hh, :], in0=banks[7][:, hh * 128:(hh + 1) * 128],
                                                             in1=kb.trif[:], op=ALU.mult),
                     xr=[PB[7]], reads=[kb.bconst], writes=[battn])
            for hh in range(4):
                ob = 2 + hh // 2
                oc = (hh % 2) * 256
                vs = v_bf[:, hh * 256:(hh + 1) * 256]
                S.op("pe", lambda h, hh=hh, ob=ob, oc=oc: h.matmul(banks[ob][:, oc:oc + 256], lhsT=q_even[:, hh, :], rhs=Sb[:, hh, :],
                                                                   start=True, stop=False),
                     reads=[bq, bSb], writes=[PB[ob]], inc=False)
                S.op("pe", lambda h, hh=hh, ob=ob, oc=oc, vs=vs: h.matmul(banks[ob][:, oc:oc + 256], lhsT=attnT[:, hh, :], rhs=vs,
                                                                          start=False, stop=False),
                     reads=[battn, bv], writes=[PB[ob]], inc=False)
                S.op("pe", lambda h, hh=hh: h.matmul(banks[6][:, 0:256], lhsT=kinv_tm[0:64, hh * 128:(hh + 1) * 128],
                                                     rhs=v_bf[0:64, hh * 256:(hh + 1) * 256], start=True, stop=True),
                     reads=[bkinv, bv], writes=[PB[6]])
                S.op("dve", lambda h, hh=hh: h.tensor_tensor(out=t1[:], in0=banks[6][:, 0:256], in1=Sf[:, hh, :], op=ALU.add),
                     xr=[PB[6]], reads=[bSf], writes=[bt1])
                S.op("dve", lambda h, hh=hh: h.tensor_scalar(out=Sf[:, hh, :], in0=t1[:], scalar1=dec[:, hh, 0:1], scalar2=None, op0=ALU.mult),
                     reads=[bt1, bdec], writes=[bSf])
                S.op("act", lambda h, hh=hh: h.copy(out=Sm[:, hh, :], in_=Sf[:, hh, :]), reads=[bSf], writes=[bSm])
                S.op("pe", lambda h, hh=hh, ob=ob, oc=oc: h.matmul(banks[ob][:, oc:oc + 256], lhsT=q_odd[:, hh, :], rhs=Sm[:, hh, :],
                                                                   start=False, stop=True),
                     reads=[bq, bSm], writes=[PB[ob]])
                S.op("pe", lambda h, hh=hh: h.matmul(banks[6][:, 256:512], lhsT=kinv_tm[64:128, hh * 128:(hh + 1) * 128],
                                                     rhs=v_bf[64:128, hh * 256:(hh + 1) * 256], start=True, stop=True),
                     reads=[bkinv, bv], writes=[PB[6]])
                S.op("dve", lambda h, hh=hh: h.tensor_tensor(out=t1[:], in0=banks[6][:, 256:512], in1=Sf[:, hh, :], op=ALU.add),
                     xr=[PB[6]], reads=[bSf], writes=[bt1])
                S.op("dve", lambda h, hh=hh: h.tensor_scalar(out=Sf[:, hh, :], in0=t1[:], scalar1=dec[:, hh, 1:2], scalar2=None, op0=ALU.mult),
                     reads=[bt1, bdec], writes=[bSf])
                S.op("act", lambda h, hh=hh: h.copy(out=Sb[:, hh, :], in_=Sf[:, hh, :]), reads=[bSf], writes=[bSb])
                S.op("act", lambda h, hh=hh, ob=ob, oc=oc: h.activation(out=junk2[:], in_=banks[ob][:, oc:oc + 256], func=AF.Square,
                                                                        accum_out=hs[:, 0:1]), xr=[PB[ob]], writes=[bj2, bhs])
                S.op("act", lambda h: h.activation(out=hs[:, 1:2], in_=hs[:, 0:1], func=AF.Sqrt, scale=1.0 / 256, bias=kb.epst[:, 0:1]),
                     reads=[kb.bconst], writes=[bhs])
                S.op("dve", lambda h: h.reciprocal(out=hs[:, 2:3], in_=hs[:, 1:2]), writes=[bhs])
                S.op("dve", lambda h, ob=ob, oc=oc: h.scalar_tensor_tensor(out=og[:], in0=banks[ob][:, oc:oc + 256], scalar=hs[:, 2:3], in1=g_hd[:],
                                                                           op0=ALU.mult, op1=ALU.mult),
                     xr=[PB[ob]], reads=[bhs, bw], writes=[bog])
                S.op("dve", lambda h, hh=hh: h.tensor_tensor(out=og_bf[:, hh * 256:(hh + 1) * 256], in0=og[:], in1=sr[:, hh * 256:(hh + 1) * 256], op=ALU.mult),
                     reads=[bog, bsr], writes=[bogb])
            for kc in range(8):
                S.op("pe", lambda h, kc=kc: h.transpose(out=pTb[:, kc * 128:(kc + 1) * 128], in_=og_bf[:, kc * 128:(kc + 1) * 128],
                                                        identity=kb.ident[:]), reads=[bogb, kb.bconst], writes=[PB[0]], inc=(kc == 7))
            S.op("act", lambda h: h.copy(out=ogT[:], in_=pTb.rearrange("p (a b) -> p a b", a=8)), xr=[PB[0]], writes=[bogT])
            for half in range(2):
                bank = 4 + half
                for kc in range(8):
                    S.op("pe", lambda h, kc=kc, half=half, bank=bank: h.matmul(banks[bank][:], lhsT=ogT[:, kc, :],
                                                                               rhs=w_out_sb[:, kc, half * 512:(half + 1) * 512],
                                                                               start=(kc == 0), stop=(kc == 7)),
                         reads=[bogT, bw], writes=[PB[bank]], inc=(kc == 7))
                S.op("dve", lambda h, half=half, bank=bank, xt=xt: h.tensor_tensor(out=h1t[:, half * 512:(half + 1) * 512], in0=banks[bank][:],
                                                                                  in1=xt[:, half * 512:(half + 1) * 512], op=ALU.add),
                     xr=[PB[bank]], reads=[bx], writes=[bh1])
            S.dma("sp", h1[t * 128:(t + 1) * 128, :], h1t[:], reads=[bh1])
        S.barrier()


def swiglu_acc(kb, st_bufs, xnT, bxnT, ntok, wg, wu, wd, F, acc, bacc, gate_ap_fn, bgate=None, wload=None, acc_init=False):
    S, banks, PB = kb.S, kb.banks, kb.PB
    (wg_sb, wu_sb, wd_sb, bws, aT, baT, sg, bsg) = st_bufs
    nfc = F // 128
    GS = 4
    ngr = (nfc + GS - 1) // GS
    ntt = ntok // 512
    nsub = ntok // 128
    cnt = getattr(kb, "_swg_cnt", 0)
    for gi in range(ngr):
        f0 = gi * GS
        nf = min(GS, nfc - f0)
        slot = cnt % 2
        cnt += 1
        bw = bws[slot]
        if wload is not None:
            wload(slot, f0, nf, bw, wg_sb, wu_sb, wd_sb)
        else:
            for kc in range(8):
                S.dma("pool", wg_sb[slot][:, kc, 0:nf * 128], wg[kc * 128:(kc + 1) * 128, f0 * 128:(f0 + nf) * 128], writes=[bw])
                S.dma("pool", wu_sb[slot][:, kc, 0:nf * 128], wu[kc * 128:(kc + 1) * 128, f0 * 128:(f0 + nf) * 128], writes=[bw])
            for fc in range(nf):
                S.dma("pool", wd_sb[slot][:, fc, :], wd[(f0 + fc) * 128:(f0 + fc + 1) * 128, :], writes=[bw])
        for fc in range(nf):
            for tt in range(ntt):
                pg = 2 + (tt % 2) * 2
                pu = pg + 1
                for kc in range(8):
                    S.op("pe", lambda h, kc=kc, fc=fc, tt=tt, pg=pg, slot=slot: h.matmul(
                        banks[pg][:], lhsT=wg_sb[slot][:, kc, fc * 128:(fc + 1) * 128], rhs=xnT[:, kc, tt * 512:(tt + 1) * 512],
                        start=(kc == 0), stop=(kc == 7)), reads=[bw, bxnT], writes=[PB[pg]], inc=(kc == 7))
                for kc in range(8):
                    S.op("pe", lambda h, kc=kc, fc=fc, tt=tt, pu=pu, slot=slot: h.matmul(
                        banks[pu][:], lhsT=wu_sb[slot][:, kc, fc * 128:(fc + 1) * 128], rhs=xnT[:, kc, tt * 512:(tt + 1) * 512],
                        start=(kc == 0), stop=(kc == 7)), reads=[bw, bxnT], writes=[PB[pu]], inc=(kc == 7))
                sgt = sg[tt % 2]
                S.op("act", lambda h, pg=pg, sgt=sgt: h.activation(out=sgt[:], in_=banks[pg][:], func=AF.Silu),
                     xr=[PB[pg]], writes=[bsg[tt % 2]])
                S.op("dve", lambda h, pu=pu, sgt=sgt, fc=fc, tt=tt, slot=slot: h.tensor_tensor(
                    out=aT[slot][:, fc, tt * 512:(tt + 1) * 512], in0=banks[pu][:], in1=sgt[:], op=ALU.mult),
                    xr=[PB[pu]], reads=[bsg[tt % 2]], writes=[baT[slot]])
        for sub in range(nsub):
            for half in range(2):
                pd = (6, 7, 0, 1)[(sub * 2 + half) % 4]
                for fc in range(nf):
                    S.op("pe", lambda h, fc=fc, sub=sub, half=half, pd=pd, slot=slot, nf=nf: h.matmul(
                        banks[pd][:], lhsT=aT[slot][:, fc, sub * 128:(sub + 1) * 128], rhs=wd_sb[slot][:, fc, half * 512:(half + 1) * 512],
                        start=(fc == 0), stop=(fc == nf - 1)), reads=[baT[slot], bw], writes=[PB[pd]], inc=(fc == nf - 1))
                g = gate_ap_fn(sub)
                if acc_init and gi == 0:
                    S.op("dve", lambda h, sub=sub, half=half, pd=pd: h.tensor_copy(out=acc[:, sub, half * 512:(half + 1) * 512], in_=banks[pd][:]),
                         xr=[PB[pd]], writes=[bacc])
                    continue
                S.op("dve", lambda h, sub=sub, half=half, pd=pd, g=g: h.scalar_tensor_tensor(
                    out=acc[:, sub, half * 512:(half + 1) * 512], in0=banks[pd][:], scalar=(1.0 if g is None else g),
                    in1=acc[:, sub, half * 512:(half + 1) * 512], op0=ALU.mult, op1=ALU.add),
                    xr=[PB[pd]], reads=([bacc] if bgate is None else [bacc, bgate]), writes=[bacc])
    kb._swg_cnt = cnt


def swiglu_bufs(kb, st, ntok):
    sb = lambda shape, dt, name=None: kb.sb(st, shape, dt, name)
    wg_sb = [sb([128, 8, 512], BF16, "wg") for _ in range(2)]
    wu_sb = [sb([128, 8, 512], BF16, "wu") for _ in range(2)]
    wd_sb = [sb([128, 4, 1024], BF16, "wd") for _ in range(2)]
    bws = [Buf("w0"), Buf("w1")]
    aT0 = sb([128, 4, ntok], BF16, "aT")
    aT = [aT0, aT0]
    baT0 = Buf("aT0")
    baT = [baT0, baT0]
    sg = [sb([128, 512], F32, "sg") for _ in range(2)]
    bsg = [Buf("sg0"), Buf("sg1")]
    return (wg_sb, wu_sb, wd_sb, bws, aT, baT, sg, bsg)


def stage_ffn(kb, h1, h2, ffn_norm0, wg, wu, wd, TB=2048):
    S = kb.S
    with ExitStack() as st:
        sb = lambda shape, dt, name=None: kb.sb(st, shape, dt, name)
        g_bc = sb([128, 1024], F32, "g_ffn")
        bg = Buf("g")
        S.dma("sp", g_bc[:], ffn_norm0.partition_broadcast(128), writes=[bg])
        ntmp = kb.norm_tmp(st)
        xnT = sb([128, 8, TB], BF16, "xnT"); bxnT = Buf("xnT")
        acc = sb([128, TB // 128, 1024], F32, "acc"); bacc = Buf("acc")
        bufs = swiglu_bufs(kb, st, TB)
        for blk in range(S_LEN // TB):
            for sub in range(TB // 128):
                r0 = blk * TB + sub * 128
                S.dma("sp", acc[:, sub, :], h1[r0:r0 + 128, :], writes=[bacc])
                kb.norm_T(ntmp, acc[:, sub, :], bacc, [(g_bc[:], bg, xnT[:, :, sub * 128:(sub + 1) * 128], bxnT)])
            swiglu_acc(kb, bufs, xnT, bxnT, TB, wg, wu, wd, FFN_DIM, acc, bacc, lambda sub: None)
            for sub in range(TB // 128):
                r0 = blk * TB + sub * 128
                S.dma("sp", h2[r0:r0 + 128, :], acc[:, sub, :], reads=[bacc])
        S.barrier()


def stage_qkv(kb, h2, qT_d, kT_d, v_d, kv_norm, attn_norm1, w_kv, w_q):
    S, banks, PB = kb.S, kb.banks, kb.PB
    with ExitStack() as st:
        sb = lambda shape, dt, name=None: kb.sb(st, shape, dt, name)
        bw = Buf("w")
        wq_sb = sb([128, 8, 1024], BF16, "wq")
        wkv_sb = sb([128, 8, 2048], BF16, "wkv")
        kb.load_w_bf16(wq_sb, w_q, bw, 8, 1024)
        kb.load_w_bf16(wkv_sb, w_kv, bw, 8, 2048)
        g_kv = sb([128, 1024], F32, "g_kv")
        g_q = sb([128, 1024], F32, "g_q")
        S.dma("sp", g_kv[:], kv_norm.partition_broadcast(128), writes=[bw])
        S.dma("sp", g_q[:], attn_norm1.partition_broadcast(128), writes=[bw])
        ntmp = kb.norm_tmp(st)
        TB = 512
        xt = [sb([128, 1024], F32, "xt") for _ in range(2)]
        bxt = [Buf("xt0"), Buf("xt1")]
        xkvT = sb([128, 8, TB], BF16, "xkvT"); bxkv = Buf("xkvT")
        xqT = sb([128, 8, TB], BF16, "xqT"); bxq = Buf("xqT")
        stg = [sb([128, 512], BF16, "stg") for _ in range(3)]
        bstg = [Buf("stg%d" % i) for i in range(3)]
        sc = 128.0 ** -0.5
        n = 0
        for blk in range(S_LEN // TB):
            for sub in range(TB // 128):
                r0 = blk * TB + sub * 128
                x_ = xt[sub % 2]; bx_ = bxt[sub % 2]
                S.dma("sp", x_[:], h2[r0:r0 + 128, :], writes=[bx_])
                kb.norm_T(ntmp, x_[:], bx_, [(g_kv[:], bw, xkvT[:, :, sub * 128:(sub + 1) * 128], bxkv),
                                            (g_q[:], bw, xqT[:, :, sub * 128:(sub + 1) * 128], bxq)])
            t0 = blk * TB
            for hh in range(8):
                for (wsb, c0, xT_, bx2, dst, scale) in ((wq_sb, hh * 128, xqT, bxq, qT_d, sc), (wkv_sb, hh * 128, xkvT, bxkv, kT_d, 1.0)):
                    bank = 1 + (n % 3); sidx = n % 3; n += 1
                    for kc in range(8):
                        S.op("pe", lambda h, kc=kc, wsb=wsb, c0=c0, xT_=xT_, bank=bank: h.matmul(
                            banks[bank][:], lhsT=wsb[:, kc, c0:c0 + 128], rhs=xT_[:, kc, :], start=(kc == 0), stop=(kc == 7)),
                            reads=[bw, bx2], writes=[PB[bank]], inc=(kc == 7))
                    S.op("act", lambda h, bank=bank, sidx=sidx, scale=scale: h.activation(out=stg[sidx][:], in_=banks[bank][:], func=AF.Copy, scale=scale),
                         xr=[PB[bank]], writes=[bstg[sidx]])
                    S.dma("sp", dst[hh, :, t0:t0 + TB], stg[sidx][:], reads=[bstg[sidx]])
            for sub in range(TB // 128):
                for half in range(2):
                    bank = 1 + (n % 3); sidx = n % 3; n += 1
                    for kc in range(8):
                        S.op("pe", lambda h, kc=kc, sub=sub, half=half, bank=bank: h.matmul(
                            banks[bank][:], lhsT=xkvT[:, kc, sub * 128:(sub + 1) * 128], rhs=wkv_sb[:, kc, 1024 + half * 512:1024 + (half + 1) * 512],
                            start=(kc == 0), stop=(kc == 7)), reads=[bw, bxkv], writes=[PB[bank]], inc=(kc == 7))
                    S.op("act", lambda h, bank=bank, sidx=sidx: h.copy(out=stg[sidx][:], in_=banks[bank][:]), xr=[PB[bank]], writes=[bstg[sidx]])
                    S.dma("sp", v_d[t0 + sub * 128:t0 + (sub + 1) * 128, half * 512:(half + 1) * 512], stg[sidx][:], reads=[bstg[sidx]])
        S.barrier()


def stage_sb(kb, qT_d, kT_d, v_d, oT_d, nheads=8, nqb=NT):
    S, banks, PB = kb.S, kb.banks, kb.PB
    with ExitStack() as st:
        sb = lambda shape, dt, name=None: kb.sb(st, shape, dt, name)
        qT = [sb([128, S_LEN], BF16, "qT") for _ in range(2)]
        kT = [sb([128, S_LEN], BF16, "kT") for _ in range(2)]
        vh = [sb([128, NT, 128], BF16, "vh") for _ in range(2)]
        bqkv = [Buf("qkv0"), Buf("qkv1")]
        ones = sb([128, 1], F32, "ones")
        cmask = sb([128, 128], F32, "cmask")
        bc = Buf("c")
        S.op("dve", lambda h: h.memset(ones[:], 1.0), writes=[bc])
        S.op("pool", lambda h: h.memset(cmask[:], 1.0), writes=[bc])
        S.op("pool", lambda h: h.affine_select(out=cmask[:], in_=cmask[:], pattern=[[-1, 128]], compare_op=ALU.is_gt,
                                               fill=0.0, base=0, channel_multiplier=1), writes=[bc])
        E = [sb([128, 512], F32, "E") for _ in range(2)]; bE = [Buf("E0"), Buf("E1")]
        SP = [sb([128, S_LEN + 1], F32, "SP") for _ in range(2)]; bSP = [Buf("SP0"), Buf("SP1")]
        for i in range(2):
            S.op("dve", lambda h, i=i: h.memset(SP[i][:, 0:1], 0.0), writes=[bSP[i]])
        G = sb([128, 512], F32, "G"); bG = Buf("G")
        ARG = [sb([128, S_LEN], F32, "ARG") for _ in range(2)]; bARG = [Buf("ARG0"), Buf("ARG1")]
        nt = sb([128, 2], F32, "nt"); bnts = [Buf("nt0"), Buf("nt1")]
        W = sb([128, S_LEN], BF16, "W"); bW = Buf("W")
        WT = sb([128, NT, 128], BF16, "WT"); bWT = Buf("WT")
        oTs = [sb([128, 128], BF16, "oTs") for _ in range(2)]; boT = [Buf("oT0"), Buf("oT1")]
        items = [(hh, tb) for hh in range(nheads) for tb in range(nqb)]
        N = len(items)
        cneg = sb([128, 128], F32, "cneg")
        S.op("dve", lambda h: h.tensor_scalar(out=cneg[:], in0=cmask[:], scalar1=-1.0, scalar2=1e30, op0=ALU.add, op1=ALU.mult),
             writes=[bc])
        TRB = (0, 3, 4, 5)

        bWc = [Buf("W%d" % c) for c in range(4)]
        bWTc = [Buf("WT%d" % c) for c in range(4)]

        def act_extras(j):
            ex = []
            if 0 <= j - 3 < N:
                def f(i=j - 3):
                    hh, tb = items[i]
                    r = i % 2
                    S.op("act", lambda h: h.copy(out=oTs[r][:], in_=banks[7][:, 0:128]), xr=[PB[7]], writes=[boT[r]])
                    S.dma("sp", oT_d[hh, :, tb * 128:(tb + 1) * 128], oTs[r][:], reads=[boT[r]])
                ex.append(f)
            if 0 <= j - 2 < N:
                hh, tb = items[j - 2]
                nb = tb + 1
                for c in range((nb + 7) // 8):
                    def f(c=c, nb=nb):
                        b0 = c * 8
                        nbb = min(8, nb - b0)
                        pT = banks[TRB[c]][:].bitcast(BF16)
                        S.op("act", lambda h: h.copy(out=WT[:, b0:b0 + nbb, :].rearrange("p a b -> p (a b)"), in_=pT[:, 0:nbb * 128]),
                             xr=[PB[TRB[c]]], writes=[bWTc[c]])
                    ex.append(f)
            if 0 <= j - 1 < N:
                hh, tb = items[j - 1]
                ns = (tb + 1) * 128
                r = (j - 1) % 2
                for c in range((ns + 1023) // 1024):
                    def f(c=c, ns=ns, r=r):
                        c0 = c * 1024
                        c1 = min(ns, c0 + 1024)
                        S.op("act", lambda h: h.activation(out=W[:, c0:c1], in_=ARG[r][:, c0:c1], func=AF.Exp, bias=nt[:, r:r + 1]),
                             reads=[bARG[r], bnts[r]], writes=[bWc[c]])
                    ex.append(f)
            return ex

        def pe_extras(j):
            ex = []
            if 0 <= j - 2 < N:
                hh, tb = items[j - 2]
                sl = hh % 2
                nb = tb + 1
                for c in range((nb + 7) // 8):
                    def f(c=c, nb=nb, sl=sl):
                        for b in range(c * 8, min(nb, c * 8 + 8)):
                            S.op("pe", lambda h, b=b: h.matmul(banks[7][:, 0:128], lhsT=vh[sl][:, b, :], rhs=WT[:, b, :], start=(b == 0), stop=(b == nb - 1)),
                                 reads=[bqkv[sl], bWTc[c]], writes=[PB[7]], inc=(b == min(nb, c * 8 + 8) - 1))
                    ex.append(f)
            if 0 <= j - 1 < N:
                hh, tb = items[j - 1]
                nb = tb + 1
                for c in range((nb + 7) // 8):
                    def f(c=c, nb=nb):
                        b0 = c * 8
                        nbb = min(8, nb - b0)
                        pT = banks[TRB[c]][:].bitcast(BF16)
                        for jj in range(nbb):
                            S.op("pe", lambda h, jj=jj: h.transpose(out=pT[:, jj * 128:(jj + 1) * 128], in_=W[:, (b0 + jj) * 128:(b0 + jj + 1) * 128],
                                                                    identity=kb.ident[:]), reads=[bWc[c], kb.bconst], writes=[PB[TRB[c]]], inc=(jj == nbb - 1))
                    ex.append(f)
            return ex

        def pump(aex, pex, cnt):
            if aex:
                aex.pop(0)()
                cnt[0] += 1
            if pex and (cnt[0] >= cnt[1] + 2 or not aex):
                pex.pop(0)()
                cnt[1] += 1

        def s1(i, aex, pex, cnt):
            hh, tb = items[i]
            sl = hh % 2
            r = i % 2
            if tb == 0:
                S.dma("sp", qT[sl][:], qT_d[hh], writes=[bqkv[sl]])
                S.dma("sp", kT[sl][:], kT_d[hh], writes=[bqkv[sl]])
                S.dma("sp", vh[sl][:], v_d[:, hh * 128:(hh + 1) * 128].rearrange("(b p) d -> p b d", p=128), writes=[bqkv[sl]])
            ns = (tb + 1) * 128
            nkt = (ns + 511) // 512
            ZB = (1, 2, 6)

            def zmm(kt):
                s0 = kt * 512
                w = min(512, ns - s0)
                zb = ZB[kt % 3]
                S.op("pe", lambda h: h.matmul(banks[zb][:, 0:w], lhsT=qT[sl][:, tb * 128:(tb + 1) * 128],
                                              rhs=kT[sl][:, s0:s0 + w], start=True, stop=True),
                     reads=[bqkv[sl]], writes=[PB[zb]])

            zmm(0)
            for kt in range(nkt):
                s0 = kt * 512
                w = min(512, ns - s0)
                zb = ZB[kt % 3]
                if kt + 1 < nkt:
                    zmm(kt + 1)
                e_ = E[kt % 2]; be_ = bE[kt % 2]
                S.op("act", lambda h, zb=zb, w=w, e_=e_: h.activation(out=e_[:, 0:w], in_=banks[zb][:, 0:w], func=AF.Exp),
                     xr=[PB[zb]], writes=[be_])
                S.op("act", lambda h, w=w, e_=e_, r=r, s0=s0: h.activation(out=SP[r][:, 1 + s0:1 + s0 + w], in_=e_[:, 0:w], func=AF.Ln, bias=1.0),
                     reads=[be_], writes=[bSP[r]])
                pump(aex, pex, cnt)
                if kt == nkt - 1:
                    d0 = 1 + ns - 128
                    S.op("dve", lambda h, r=r, d0=d0: h.tensor_tensor(out=SP[r][:, d0:d0 + 128], in0=SP[r][:, d0:d0 + 128], in1=cmask[:], op=ALU.mult),
                         reads=[bc], writes=[bSP[r]])
                init = 0.0 if kt == 0 else G[:, 511:512]
                S.op("dve", lambda h, r=r, s0=s0, w=w, init=init: h.tensor_tensor_scan(
                    out=G[:, 0:w], data0=ones[:, 0:1].to_broadcast([128, w]), data1=SP[r][:, s0:s0 + w], initial=init,
                    op0=ALU.mult, op1=ALU.add), reads=[bSP[r], bc], writes=[bG])
                S.op("dve", lambda h, r=r, s0=s0, w=w, zb=zb: h.tensor_tensor(out=ARG[r][:, s0:s0 + w], in0=banks[zb][:, 0:w], in1=G[:, 0:w], op=ALU.add),
                     xr=[PB[zb]], reads=[bG], writes=[bARG[r]])
            lw = ns - (nkt - 1) * 512
            S.op("dve", lambda h, lw=lw, r=r: h.tensor_scalar(out=nt[:, r:r + 1], in0=G[:, lw - 1:lw], scalar1=-1.0, scalar2=None, op0=ALU.mult),
                 reads=[bG], writes=[bnts[r]])
            S.op("dve", lambda h, r=r, ns=ns: h.tensor_tensor(out=ARG[r][:, ns - 128:ns], in0=ARG[r][:, ns - 128:ns], in1=cneg[:], op=ALU.add),
                 reads=[bc], writes=[bARG[r]])

        for j in range(N + 3):
            aex = act_extras(j)
            pex = pe_extras(j)
            cnt = [0, 0]
            if j < N:
                s1(j, aex, pex, cnt)
            while aex or pex:
                pump(aex, pex, cnt)
        S.barrier()


def stage_attn_out(kb, oT_d, h2, h3, w_out):
    S, banks, PB = kb.S, kb.banks, kb.PB
    with ExitStack() as st:
        sb = lambda shape, dt, name=None: kb.sb(st, shape, dt, name)
        bw = Buf("w")
        wo_sb = sb([128, 8, 1024], BF16, "wo")
        kb.load_w_bf16(wo_sb, w_out, bw, 8, 1024)
        TB = 512
        oT = [sb([128, 8, TB], BF16, "oT") for _ in range(2)]; boT = [Buf("oT0"), Buf("oT1")]
        xt = [sb([128, 1024], F32, "xt") for _ in range(2)]; bxt = [Buf("x0"), Buf("x1")]
        n = 0
        for blk in range(S_LEN // TB):
            sl = blk % 2
            for hh in range(8):
                S.dma("sp", oT[sl][:, hh, :], oT_d[hh, :, blk * TB:(blk + 1) * TB], writes=[boT[sl]])
            for sub in range(TB // 128):
                r0 = blk * TB + sub * 128
                x_ = xt[n % 2]; bx_ = bxt[n % 2]; n += 1
                S.dma("sp", x_[:], h2[r0:r0 + 128, :], writes=[bx_])
                for half in range(2):
                    bank = 1 + half
                    for kc in range(8):
                        S.op("pe", lambda h, kc=kc, sub=sub, half=half, bank=bank, sl=sl: h.matmul(
                            banks[bank][:], lhsT=oT[sl][:, kc, sub * 128:(sub + 1) * 128], rhs=wo_sb[:, kc, half * 512:(half + 1) * 512],
                            start=(kc == 0), stop=(kc == 7)), reads=[boT[sl], bw], writes=[PB[bank]], inc=(kc == 7))
                    S.op("dve", lambda h, half=half, bank=bank, x_=x_: h.tensor_tensor(out=x_[:, half * 512:(half + 1) * 512], in0=banks[bank][:],
                                                                                      in1=x_[:, half * 512:(half + 1) * 512], op=ALU.add),
                         xr=[PB[bank]], reads=[bx_], writes=[bx_])
                S.dma("sp", h3[r0:r0 + 128, :], x_[:], reads=[bx_])
        S.barrier()


def stage_moe(kb, h3, out, ffn_norm1, final_norm, w_router, wg, wu, wd, TB=2048, nexp=NEXP):
    S, banks, PB = kb.S, kb.banks, kb.PB
    with ExitStack() as st:
        sb = lambda shape, dt, name=None: kb.sb(st, shape, dt, name)
        bg = Buf("g")
        g_bc = sb([128, 1024], F32, "g_ffn1")
        g_fin = sb([128, 1024], F32, "g_fin")
        S.dma("sp", g_bc[:], ffn_norm1.partition_broadcast(128), writes=[bg])
        S.dma("sp", g_fin[:], final_norm.partition_broadcast(128), writes=[bg])
        wr_sb = sb([128, 8, 8], BF16, "wr")
        for kc in range(8):
            S.dma("pool", wr_sb[:, kc, :], w_router[kc * 128:(kc + 1) * 128, :], writes=[bg])
        ntmp = kb.norm_tmp(st)
        xnT = sb([128, 8, TB], BF16, "xnT"); bxnT = Buf("xnT")
        acc = sb([128, TB // 128, 1024], F32, "acc"); bacc = Buf("acc")
        nsub = TB // 128
        gates = sb([128, nsub, 8], F32, "gates"); bgates = Buf("gates")
        lg = sb([128, 8], F32, "lg"); blg = Buf("lg")
        m1 = sb([128, 8], F32, "m1"); mk1 = sb([128, 8], F32, "mk1"); mk2 = sb([128, 8], F32, "mk2"); l2 = sb([128, 8], F32, "l2")
        ot = sb([128, 1024], F32, "ot"); bot = Buf("ot")
        bufs = swiglu_bufs(kb, st, TB)
        for blk in range(S_LEN // TB):
            for sub in range(nsub):
                r0 = blk * TB + sub * 128
                S.dma("sp", acc[:, sub, :], h3[r0:r0 + 128, :], writes=[bacc])
                kb.norm_T(ntmp, acc[:, sub, :], bacc, [(g_bc[:], bg, xnT[:, :, sub * 128:(sub + 1) * 128], bxnT)])
                for kc in range(8):
                    S.op("pe", lambda h, kc=kc, sub=sub: h.matmul(banks[1][:, 0:8], lhsT=xnT[:, kc, sub * 128:(sub + 1) * 128], rhs=wr_sb[:, kc, :],
                                                                  start=(kc == 0), stop=(kc == 7)), reads=[bxnT, bg], writes=[PB[1]], inc=(kc == 7))
                S.op("dve", lambda h: h.tensor_copy(out=lg[:], in_=banks[1][:, 0:8]), xr=[PB[1]], writes=[blg])
                S.op("dve", lambda h: h.tensor_reduce(out=m1[:, 0:1], in_=lg[:], axis=mybir.AxisListType.X, op=ALU.max), writes=[blg])
                S.op("dve", lambda h: h.tensor_scalar(out=mk1[:], in0=lg[:], scalar1=m1[:, 0:1], scalar2=None, op0=ALU.is_equal), writes=[blg])
                S.op("dve", lambda h: h.scalar_tensor_tensor(out=l2[:], in0=mk1[:], scalar=-1e30, in1=lg[:], op0=ALU.mult, op1=ALU.add), writes=[blg])
                S.op("dve", lambda h: h.tensor_reduce(out=m1[:, 1:2], in_=l2[:], axis=mybir.AxisListType.X, op=ALU.max), writes=[blg])
                S.op("dve", lambda h: h.tensor_scalar(out=mk2[:], in0=l2[:], scalar1=m1[:, 1:2], scalar2=None, op0=ALU.is_equal), writes=[blg])
                S.op("dve", lambda h: h.tensor_tensor(out=m1[:, 2:3], in0=m1[:, 0:1], in1=m1[:, 1:2], op=ALU.subtract), writes=[blg])
                S.op("act", lambda h: h.activation(out=m1[:, 3:4], in_=m1[:, 2:3], func=AF.Sigmoid), writes=[blg])
                S.op("act", lambda h: h.activation(out=m1[:, 4:5], in_=m1[:, 2:3], func=AF.Sigmoid, scale=-1.0), writes=[blg])
                S.op("dve", lambda h: h.tensor_scalar(out=mk1[:], in0=mk1[:], scalar1=m1[:, 3:4], scalar2=None, op0=ALU.mult), writes=[blg])
                S.op("dve", lambda h, sub=sub: h.scalar_tensor_tensor(out=gates[:, sub, :], in0=mk2[:], scalar=m1[:, 4:5], in1=mk1[:], op0=ALU.mult, op1=ALU.add),
                     reads=[blg], writes=[bgates])
            for e in range(nexp):
                swiglu_acc(kb, bufs, xnT, bxnT, TB, wg[e], wu[e], wd[e], EXPERT_DIM, acc, bacc,
                           lambda sub, e=e: gates[:, sub, e:e + 1], bgates)
            junk, bj, ss, bss, xn, bxn = ntmp
            for sub in range(nsub):
                r0 = blk * TB + sub * 128
                S.op("act", lambda h, sub=sub: h.activation(out=junk[:], in_=acc[:, sub, :], func=AF.Square, accum_out=ss[:, 0:1]),
                     reads=[bacc, bgates], writes=[bj, bss])
                S.op("act", lambda h: h.activation(out=ss[:, 1:2], in_=ss[:, 0:1], func=AF.Sqrt, scale=1.0 / D, bias=kb.epst[:, 0:1]),
                     reads=[kb.bconst], writes=[bss])
                S.op("dve", lambda h: h.reciprocal(out=ss[:, 2:3], in_=ss[:, 1:2]), writes=[bss])
                S.op("dve", lambda h, sub=sub: h.scalar_tensor_tensor(out=ot[:], in0=acc[:, sub, :], scalar=ss[:, 2:3], in1=g_fin[:], op0=ALU.mult, op1=ALU.mult),
                     reads=[bacc, bss, bg], writes=[bot])
                S.dma("sp", out[r0:r0 + 128, :], ot[:], reads=[bot])
        S.barrier()


I32 = mybir.dt.int32
MOE_G = 1024
MOE_NG = 15


def stage_moe_sparse(kb, h3, out, xs_d, y_d, ffn_norm1, final_norm, w_router, wg, wu, wd, ngroups=MOE_NG):
    S, banks, PB, nc = kb.S, kb.banks, kb.PB, kb.nc
    G = MOE_G
    wg2 = wg.rearrange("e k f -> (e k) f")
    wu2 = wu.rearrange("e k f -> (e k) f")
    wd2 = wd.rearrange("e f d -> (e f) d")
    IOA = bass.IndirectOffsetOnAxis
    with ExitStack() as st0:
        sb0 = lambda shape, dt, name=None: kb.sb(st0, shape, dt, name)
        bg = Buf("g")
        g_bc = sb0([128, 1024], F32, "g_ffn1")
        g_fin = sb0([128, 1024], F32, "g_fin")
        S.dma("sp", g_bc[:], ffn_norm1.partition_broadcast(128), writes=[bg])
        S.dma("sp", g_fin[:], final_norm.partition_broadcast(128), writes=[bg])
        w1_all = sb0([128, NT], F32, "w1_all"); w2_all = sb0([128, NT], F32, "w2_all"); bwa = Buf("w_all")
        slot_i = [sb0([128, NT], I32, "slot1_i"), sb0([128, NT], I32, "slot2_i")]; bslot = Buf("slot")
        widx1 = sb0([128, MOE_NG, 8], I32, "widx1"); widx2 = sb0([128, MOE_NG, 28], I32, "widx2"); bwidx = Buf("widx")
        bxs = Buf("xs_d"); by = Buf("y_d"); bxs0 = Buf("xs_zero")
        with ExitStack() as st:
            sb = lambda shape, dt, name=None: kb.sb(st, shape, dt, name)
            wr_sb = sb([128, 8, 8], BF16, "wr")
            for kc in range(8):
                S.dma("pool", wr_sb[:, kc, :], w_router[kc * 128:(kc + 1) * 128, :], writes=[bg])
            zt = sb([128, 1024], BF16, "zt"); bz = Buf("zt")
            S.op("pool", lambda h: h.memset(zt[:], 0.0), writes=[bz])
            for blk in range(MOE_NG * G // 128):
                S.dma("sp", xs_d[blk * 128:(blk + 1) * 128, :], zt[:], reads=[bz], writes=[bxs0])
            xn_parts = [sb([128, 8, 1024], BF16, "xn_all%d" % i) for i in range(4)]; bxn = Buf("xn_all")
            xn_row = lambda t: xn_parts[t // 8][:, t % 8, :]
            xts = [sb([128, 1024], F32, "xt") for _ in range(2)]; bxts = [Buf("x0"), Buf("x1")]
            junk = sb([128, 1024], BF16, "junk"); bj = Buf("junk")
            ss = sb([128, 4], F32, "ss"); bss = Buf("ss")
            xnT = sb([128, 8, 128], BF16, "xnT"); bxnT = Buf("xnT")
            sel = sb([128, NT, 8], F32, "sel"); mk1a = sb([128, NT, 8], F32, "mk1a"); mk2a = sb([128, NT, 8], F32, "mk2a"); bsel = Buf("sel")
            lg = sb([128, 8], F32, "lg"); blg = Buf("lg")
            m1 = sb([128, 8], F32, "m1"); l2 = sb([128, 8], F32, "l2")
            pT = banks[0][:].bitcast(BF16)
            for t in range(NT):
                xt = xts[t % 2]; bx = bxts[t % 2]
                S.dma("sp", xt[:], h3[t * 128:(t + 1) * 128, :], writes=[bx])
                S.op("act", lambda h, xt=xt: h.activation(out=junk[:], in_=xt[:], func=AF.Square, accum_out=ss[:, 0:1]),
                     reads=[bx], writes=[bj, bss])
                S.op("act", lambda h: h.activation(out=ss[:, 1:2], in_=ss[:, 0:1], func=AF.Sqrt, scale=1.0 / D, bias=kb.epst[:, 0:1]),
                     reads=[kb.bconst], writes=[bss])
                S.op("dve", lambda h: h.reciprocal(out=ss[:, 2:3], in_=ss[:, 1:2]), writes=[bss])
                S.op("dve", lambda h, xt=xt, t=t: h.scalar_tensor_tensor(out=xn_row(t), in0=xt[:], scalar=ss[:, 2:3], in1=g_bc[:],
                                                                         op0=ALU.mult, op1=ALU.mult), reads=[bx, bss, bg], writes=[bxn])
                for kc in range(8):
                    S.op("pe", lambda h, kc=kc, t=t: h.transpose(out=pT[:, kc * 128:(kc + 1) * 128], in_=xn_row(t)[:, kc * 128:(kc + 1) * 128],
                                                                 identity=kb.ident[:]), reads=[bxn, kb.bconst], writes=[PB[0]], inc=(kc == 7))
                S.op("act", lambda h: h.copy(out=xnT[:], in_=pT.rearrange("p (a b) -> p a b", a=8)), xr=[PB[0]], writes=[bxnT])
                for kc in range(8):
                    S.op("pe", lambda h, kc=kc: h.matmul(banks[1][:, 0:8], lhsT=xnT[:, kc, :], rhs=wr_sb[:, kc, :],
                                                         start=(kc == 0), stop=(kc == 7)), reads=[bxnT, bg], writes=[PB[1]], inc=(kc == 7))
                S.op("dve", lambda h: h.tensor_copy(out=lg[:], in_=banks[1][:, 0:8]), xr=[PB[1]], writes=[blg])
                S.op("dve", lambda h: h.tensor_reduce(out=m1[:, 0:1], in_=lg[:], axis=mybir.AxisListType.X, op=ALU.max), writes=[blg])
                S.op("dve", lambda h, t=t: h.tensor_scalar(out=mk1a[:, t, :], in0=lg[:], scalar1=m1[:, 0:1], scalar2=None, op0=ALU.is_equal),
                     reads=[blg], writes=[bsel])
                S.op("dve", lambda h, t=t: h.scalar_tensor_tensor(out=l2[:], in0=mk1a[:, t, :], scalar=-1e30, in1=lg[:], op0=ALU.mult, op1=ALU.add),
                     reads=[bsel], writes=[blg])
                S.op("dve", lambda h: h.tensor_reduce(out=m1[:, 1:2], in_=l2[:], axis=mybir.AxisListType.X, op=ALU.max), writes=[blg])
                S.op("dve", lambda h, t=t: h.tensor_scalar(out=mk2a[:, t, :], in0=l2[:], scalar1=m1[:, 1:2], scalar2=None, op0=ALU.is_equal),
                     reads=[blg], writes=[bsel])
                S.op("dve", lambda h: h.tensor_tensor(out=m1[:, 2:3], in0=m1[:, 0:1], in1=m1[:, 1:2], op=ALU.subtract), writes=[blg])
                S.op("act", lambda h, t=t: h.activation(out=w1_all[:, t:t + 1], in_=m1[:, 2:3], func=AF.Sigmoid), reads=[blg], writes=[bwa])
                S.op("act", lambda h, t=t: h.activation(out=w2_all[:, t:t + 1], in_=m1[:, 2:3], func=AF.Sigmoid, scale=-1.0), reads=[blg], writes=[bwa])
                S.op("dve", lambda h, t=t: h.tensor_tensor(out=sel[:, t, :], in0=mk1a[:, t, :], in1=mk2a[:, t, :], op=ALU.add), writes=[bsel])
            ustr = sb([128, 128], F32, "ustr"); onesm = sb([128, 128], F32, "onesm"); ones1 = sb([128, 1], F32, "ones1"); bu = Buf("u")
            S.op("pool", lambda h: h.memset(ustr[:], 1.0), writes=[bu])
            S.op("pool", lambda h: h.affine_select(out=ustr[:], in_=ustr[:], pattern=[[1, 128]], compare_op=ALU.is_gt, fill=0.0,
                                                   base=0, channel_multiplier=-1), writes=[bu])
            S.op("pool", lambda h: h.memset(onesm[:], 1.0), writes=[bu])
            S.op("pool", lambda h: h.memset(ones1[:], 1.0), writes=[bu])
            pid_i = sb([128, 1], I32, "pid_i"); pid = sb([128, 1], F32, "pid")
            S.op("pool", lambda h: h.iota(pid_i[:], pattern=[[0, 1]], base=0, channel_multiplier=1), writes=[bu])
            S.op("dve", lambda h: h.tensor_copy(out=pid[:], in_=pid_i[:]), writes=[bu])
            selv = sel[:].rearrange("p t e -> p (t e)")
            S.op("pe", lambda h: h.matmul(banks[2][:, 0:256], lhsT=ustr[:], rhs=selv, start=True, stop=True, skip_group_check=True), reads=[bsel, bu], writes=[PB[2]])
            S.op("pe", lambda h: h.matmul(banks[3][:, 0:256], lhsT=onesm[:], rhs=selv, start=True, stop=True, skip_group_check=True), reads=[bsel, bu], writes=[PB[3]])
            tot = sb([128, NT, 8], F32, "tot"); incl = sb([128, NT, 8], F32, "incl"); base = sb([128, NT, 8], F32, "base"); bt = Buf("tot")
            S.op("dve", lambda h: h.tensor_copy(out=tot[:].rearrange("p t e -> p (t e)"), in_=banks[3][:, 0:256]), xr=[PB[3]], writes=[bt])
            for e in range(8):
                S.op("dve", lambda h, e=e: h.tensor_tensor_scan(out=incl[:, :, e], data0=ones1[:, 0:1].to_broadcast([128, NT]), data1=tot[:, :, e],
                                                                initial=0.0, op0=ALU.mult, op1=ALU.add), reads=[bu], writes=[bt])
            S.op("dve", lambda h: h.tensor_tensor(out=base[:], in0=incl[:], in1=tot[:], op=ALU.subtract), writes=[bt])
            ne = incl[:, NT - 1, :]
            ng = sb([128, 8], F32, "ng"); tmp8 = sb([128, 8], F32, "tmp8"); gi = sb([128, 8], F32, "gi"); off = sb([128, 8], F32, "off")
            S.op("dve", lambda h: h.tensor_scalar(out=ng[:], in0=ne, scalar1=0.5, scalar2=None, op0=ALU.is_gt), writes=[bt])
            for k in range(1, 4):
                S.op("dve", lambda h, k=k: h.tensor_scalar(out=tmp8[:], in0=ne, scalar1=k * G + 0.5, scalar2=None, op0=ALU.is_gt), writes=[bt])
                S.op("dve", lambda h: h.tensor_tensor(out=ng[:], in0=ng[:], in1=tmp8[:], op=ALU.add), writes=[bt])
            S.op("dve", lambda h: h.tensor_tensor_scan(out=gi[:], data0=ones1[:, 0:1].to_broadcast([128, 8]), data1=ng[:], initial=0.0,
                                                       op0=ALU.mult, op1=ALU.add), reads=[bu], writes=[bt])
            S.op("dve", lambda h: h.tensor_tensor(out=off[:], in0=gi[:], in1=ng[:], op=ALU.subtract), writes=[bt])
            S.op("dve", lambda h: h.tensor_scalar(out=off[:], in0=off[:], scalar1=float(G), scalar2=None, op0=ALU.mult), writes=[bt])
            for e in range(8):
                S.op("dve", lambda h, e=e: h.tensor_scalar(out=base[:, :, e], in0=base[:, :, e], scalar1=off[:, e:e + 1], scalar2=None, op0=ALU.add), writes=[bt])
            slotf = sb([128, NT, 8], F32, "slotf"); prod = sb([128, NT, 8], F32, "prod"); s12 = sb([128, 2, NT], F32, "s12")
            S.op("dve", lambda h: h.tensor_tensor(out=slotf[:].rearrange("p t e -> p (t e)"), in0=banks[2][:, 0:256],
                                                  in1=base[:].rearrange("p t e -> p (t e)"), op=ALU.add), xr=[PB[2]], writes=[bt])
            for k, mk in enumerate((mk1a, mk2a)):
                S.op("dve", lambda h, mk=mk: h.tensor_tensor(out=prod[:], in0=mk[:], in1=slotf[:], op=ALU.mult), reads=[bsel], writes=[bt])
                S.op("dve", lambda h, k=k: h.tensor_reduce(out=s12[:, k, :], in_=prod[:], axis=mybir.AxisListType.X, op=ALU.add), writes=[bt])
                S.op("dve", lambda h, k=k: h.tensor_copy(out=slot_i[k][:], in_=s12[:, k, :]), reads=[bt], writes=[bslot])
            ge = sb([128, MOE_NG], F32, "ge"); rb1 = sb([128, MOE_NG], F32, "rb1"); rb2 = sb([128, MOE_NG], F32, "rb2")
            for j in range(MOE_NG):
                S.op("dve", lambda h, j=j: h.tensor_scalar(out=tmp8[:], in0=gi[:], scalar1=j + 0.5, scalar2=None, op0=ALU.is_lt, op1=ALU.add,
                                                           accum_out=ge[:, j:j + 1]), writes=[bt])
            S.op("dve", lambda h: h.tensor_scalar(out=ge[:], in0=ge[:], scalar1=7.0, scalar2=None, op0=ALU.min), writes=[bt])
            pid7 = sb([128, 1], F32, "pid7")
            S.op("dve", lambda h: h.tensor_scalar(out=pid7[:], in0=pid[:], scalar1=7.0, scalar2=None, op0=ALU.mult), writes=[bu])
            S.op("dve", lambda h: h.tensor_scalar(out=rb1[:], in0=ge[:], scalar1=7168.0, scalar2=pid7[:, 0:1], op0=ALU.mult, op1=ALU.add), reads=[bu], writes=[bt])
            S.op("dve", lambda h: h.tensor_scalar(out=rb2[:], in0=ge[:], scalar1=3584.0, scalar2=pid[:, 0:1], op0=ALU.mult, op1=ALU.add), reads=[bu], writes=[bt])
            for kc in range(8):
                S.op("dve", lambda h, kc=kc: h.tensor_scalar(out=widx1[:, :, kc], in0=rb1[:], scalar1=float(kc * 896), scalar2=None, op0=ALU.add),
                     reads=[bt], writes=[bwidx])
            for fc in range(28):
                S.op("dve", lambda h, fc=fc: h.tensor_scalar(out=widx2[:, :, fc], in0=rb2[:], scalar1=float(fc * 128), scalar2=None, op0=ALU.add),
                     reads=[bt], writes=[bwidx])
            if getattr(kb, "dbg", None) is not None:
                S.dma("sp", kb.dbg["slot1"], slot_i[0][:], reads=[bslot])
                S.dma("sp", kb.dbg["slot2"], slot_i[1][:], reads=[bslot])
                S.dma("sp", kb.dbg["w1"], w1_all[:], reads=[bwa])
                S.dma("sp", kb.dbg["ge"], ge[:], reads=[bt])
                S.dma("sp", kb.dbg["xn10"], xn_row(10), reads=[bxn])
                S.dma("sp", kb.dbg["xn11"], xn_row(11), reads=[bxn])
            for t in range(NT):
                for k in range(2):
                    S.dma("pool", None, None, reads=[bslot, bxn, bxs0], writes=[bxs],
                          fn=lambda h, t=t, k=k: h.indirect_dma_start(out=xs_d[:, :], out_offset=IOA(ap=slot_i[k][:, t:t + 1], axis=0),
                                                                      in_=xn_row(t), in_offset=None))
            S.barrier()
        with ExitStack() as st:
            sb = lambda shape, dt, name=None: kb.sb(st, shape, dt, name)
            xg = sb([128, 8, 1024], BF16, "xg"); bxg = Buf("xg")
            xgT = sb([128, 8, G], BF16, "xgT"); bxgT = Buf("xgT")
            acc = sb([128, 8, 1024], F32, "acc"); bacc = Buf("acc")
            bufs = swiglu_bufs(kb, st, G)
            pT = banks[0][:].bitcast(BF16)
            for j in range(ngroups):
                S.dma("sp", xg[:], xs_d[j * G:(j + 1) * G, :].rearrange("(s p) d -> p s d", p=128), reads=[bxs], writes=[bxg])
                for sub in range(8):
                    for kc in range(8):
                        S.op("pe", lambda h, kc=kc, sub=sub: h.transpose(out=pT[:, kc * 128:(kc + 1) * 128], in_=xg[:, sub, kc * 128:(kc + 1) * 128],
                                                                         identity=kb.ident[:]), reads=[bxg, kb.bconst], writes=[PB[0]], inc=(kc == 7))
                    S.op("act", lambda h, sub=sub: h.copy(out=xgT[:, :, sub * 128:(sub + 1) * 128], in_=pT.rearrange("p (a b) -> p a b", a=8)),
                         xr=[PB[0]], writes=[bxgT])
                def wload(slot, f0, nf, bw, wg_sb, wu_sb, wd_sb, j=j):
                    for kc in range(8):
                        for (wsb, w2) in ((wg_sb, wg2), (wu_sb, wu2)):
                            S.dma("pool", None, None, reads=[bwidx], writes=[bw],
                                  fn=lambda h, wsb=wsb, w2=w2, kc=kc: h.indirect_dma_start(
                                      out=wsb[slot][:, kc, 0:nf * 128], out_offset=None, in_=w2[:, 0:nf * 128],
                                      in_offset=IOA(ap=widx1[:, j, kc:kc + 1], axis=0), element_offset=f0 * 128))
                    for fc in range(nf):
                        S.dma("pool", None, None, reads=[bwidx], writes=[bw],
                              fn=lambda h, fc=fc: h.indirect_dma_start(
                                  out=wd_sb[slot][:, fc, :], out_offset=None, in_=wd2[:, :],
                                  in_offset=IOA(ap=widx2[:, j, f0 + fc:f0 + fc + 1], axis=0)))

                swiglu_acc(kb, bufs, xgT, bxgT, G, None, None, None, EXPERT_DIM, acc, bacc, lambda sub: None, wload=wload, acc_init=True)
                S.dma("sp", y_d[j * G:(j + 1) * G, :].rearrange("(s p) d -> p s d", p=128), acc[:], reads=[bacc], writes=[by])
            S.barrier()
        with ExitStack() as st:
            sb = lambda shape, dt, name=None: kb.sb(st, shape, dt, name)
            xts_5 = [sb([128, 1024], F32, "xt") for _ in range(2)]; bxts_5 = [Buf("x0"), Buf("x1")]
            y1_5 = [sb([128, 1024], F32, "y1_5") for _ in range(2)]; y2_5 = [sb([128, 1024], F32, "y2_5") for _ in range(2)]
            by1_5 = [Buf("y1a"), Buf("y1b")]; by2_5 = [Buf("y2a"), Buf("y2b")]
            junk_5 = sb([128, 1024], BF16, "junk_5"); bj_5 = Buf("junk_5")
            ss_5 = sb([128, 4], F32, "ss_5"); bss_5 = Buf("ss_5")
            ot_5 = [sb([128, 1024], F32, "ot_5") for _ in range(2)]; bot_5 = [Buf("ot0"), Buf("ot1")]
            for t in range(NT):
                r = t % 2
                xt = xts_5[r]; bx = bxts_5[r]
                S.dma("sp", xt[:], h3[t * 128:(t + 1) * 128, :], writes=[bx])
                for (yy, byy, k) in ((y1_5[r], by1_5[r], 0), (y2_5[r], by2_5[r], 1)):
                    S.dma("pool", None, None, reads=[bslot, by], writes=[byy],
                          fn=lambda h, yy=yy, k=k, t=t: h.indirect_dma_start(out=yy[:, :], out_offset=None, in_=y_d[:, :],
                                                                            in_offset=IOA(ap=slot_i[k][:, t:t + 1], axis=0)))
                S.op("dve", lambda h, xt=xt, r=r, t=t: h.scalar_tensor_tensor(out=xt[:], in0=y1_5[r][:], scalar=w1_all[:, t:t + 1], in1=xt[:],
                                                                            op0=ALU.mult, op1=ALU.add), reads=[by1_5[r], bwa], writes=[bx])
                S.op("dve", lambda h, xt=xt, r=r, t=t: h.scalar_tensor_tensor(out=xt[:], in0=y2_5[r][:], scalar=w2_all[:, t:t + 1], in1=xt[:],
                                                                            op0=ALU.mult, op1=ALU.add), reads=[by2_5[r], bwa], writes=[bx])
                S.op("act", lambda h, xt=xt: h.activation(out=junk_5[:], in_=xt[:], func=AF.Square, accum_out=ss_5[:, 0:1]), reads=[bx], writes=[bj_5, bss_5])
                S.op("act", lambda h: h.activation(out=ss_5[:, 1:2], in_=ss_5[:, 0:1], func=AF.Sqrt, scale=1.0 / D, bias=kb.epst[:, 0:1]),
                     reads=[kb.bconst], writes=[bss_5])
                S.op("dve", lambda h: h.reciprocal(out=ss_5[:, 2:3], in_=ss_5[:, 1:2]), writes=[bss_5])
                S.op("dve", lambda h, xt=xt, r=r: h.scalar_tensor_tensor(out=ot_5[r][:], in0=xt[:], scalar=ss_5[:, 2:3], in1=g_fin[:], op0=ALU.mult, op1=ALU.mult),
                     reads=[bx, bss_5, bg], writes=[bot_5[r]])
                S.dma("sp", out[t * 128:(t + 1) * 128, :], ot_5[r][:], reads=[bot_5[r]])
            S.barrier()

IN_SHAPES = {
    "x": [S_LEN, D], "attn_norm": [2, D], "ffn_norm": [2, D], "kv_norm": [D], "final_norm": [D],
    "gla_w_in": [D, GLA_IN], "gla_w_gate_up": [16, 512], "gla_b_gate": [1, 512], "gla_head_norm": [256],
    "gla_w_out": [D, D], "sb_w_kv": [D, 2048], "sb_w_q": [D, D], "sb_w_out": [D, D],
    "ffn_w_gate": [D, FFN_DIM], "ffn_w_up": [D, FFN_DIM], "ffn_w_down": [FFN_DIM, D],
    "moe_w_router": [D, 8], "moe_w_gate": [8, D, EXPERT_DIM], "moe_w_up": [8, D, EXPERT_DIM], "moe_w_down": [8, EXPERT_DIM, D],
}

ALL_STAGES = ("gla", "ffn", "qkv", "sb", "ao", "moe")
SPARSE_MOE = True


def build(stages=ALL_STAGES, ext=()):
    nc = bass.Bass("TRN2", target_bir_lowering=False)
    I = {k: nc.dram_tensor(k, shp, F32, kind="ExternalInput").ap() for k, shp in IN_SHAPES.items()}

    def scratch(name, shape, dt):
        if name in ext and name != "dbg":
            first = ALL_STAGES.index(stages[0])
            prod = {"h1": 0, "h2": 1, "qT": 2, "kT": 2, "v": 2, "oT": 3, "h3": 4}[name]
            kind = "ExternalInput" if prod < first else "ExternalOutput"
            return nc.dram_tensor(name, shape, dt, kind=kind).ap()
        return nc.dram_tensor(name, shape, dt).ap()

    h1 = scratch("h1", [S_LEN, D], F32)
    h2 = scratch("h2", [S_LEN, D], F32)
    qT_d = scratch("qT", [8, 128, S_LEN], BF16)
    kT_d = scratch("kT", [8, 128, S_LEN], BF16)
    v_d = scratch("v", [S_LEN, D], BF16)
    oT_d = scratch("oT", [8, 128, S_LEN], BF16)
    h3 = scratch("h3", [S_LEN, D], F32)
    out = nc.dram_tensor("out", [S_LEN, D], F32, kind="ExternalOutput").ap()
    with ExitStack() as es:
        kb = KB(nc, es)
        if "dbg" in ext:
            kb.dbg = {"slot1": nc.dram_tensor("dbg_slot1", [128, NT], mybir.dt.int32, kind="ExternalOutput").ap(),
                      "slot2": nc.dram_tensor("dbg_slot2", [128, NT], mybir.dt.int32, kind="ExternalOutput").ap(),
                      "w1": nc.dram_tensor("dbg_w1", [128, NT], F32, kind="ExternalOutput").ap(),
                      "ge": nc.dram_tensor("dbg_ge", [128, MOE_NG], F32, kind="ExternalOutput").ap(),
                      "xn10": nc.dram_tensor("dbg_xn10", [128, 1024], BF16, kind="ExternalOutput").ap(),
                      "xn11": nc.dram_tensor("dbg_xn11", [128, 1024], BF16, kind="ExternalOutput").ap()}
        kb.consts()
        if "gla" in stages:
            stage_gla(kb, I["x"], h1, I["attn_norm"][0], I["gla_w_in"], I["gla_w_gate_up"], I["gla_b_gate"],
                      I["gla_head_norm"], I["gla_w_out"])
        if "ffn" in stages:
            stage_ffn(kb, h1, h2, I["ffn_norm"][0], I["ffn_w_gate"], I["ffn_w_up"], I["ffn_w_down"])
        if "qkv" in stages:
            stage_qkv(kb, h2, qT_d, kT_d, v_d, I["kv_norm"], I["attn_norm"][1], I["sb_w_kv"], I["sb_w_q"])
        if "sb" in stages:
            stage_sb(kb, qT_d, kT_d, v_d, oT_d)
        if "ao" in stages:
            stage_attn_out(kb, oT_d, h2, h3, I["sb_w_out"])
        if "moe" in stages and SPARSE_MOE:
            dk = {"kind": "ExternalOutput"} if "dbg" in ext else {}
            xs_d = nc.dram_tensor("xs_d", [MOE_NG * MOE_G, D], BF16, **dk).ap()
            y_d = nc.dram_tensor("y_d", [MOE_NG * MOE_G, D], F32, **dk).ap()
            stage_moe_sparse(kb, h3, out, xs_d, y_d, I["ffn_norm"][1], I["final_norm"], I["moe_w_router"], I["moe_w_gate"], I["moe_w_up"], I["moe_w_down"])
        elif "moe" in stages:
            stage_moe(kb, h3, out, I["ffn_norm"][1], I["final_norm"], I["moe_w_router"], I["moe_w_gate"], I["moe_w_up"], I["moe_w_down"])
        kb.S.emit()
    return nc


def host_inputs(inputs):
    f = lambda a: np.ascontiguousarray(np.asarray(a, dtype=np.float32))
    shared = {
        "attn_norm": f(inputs["attn_norm"]), "ffn_norm": f(inputs["ffn_norm"]), "kv_norm": f(inputs["kv_norm"]),
        "final_norm": f(inputs["final_norm"]), "gla_w_in": f(inputs["gla_w_in"][0]), "gla_w_gate_up": f(inputs["gla_w_gate_up"][0]),
        "gla_b_gate": f(inputs["gla_b_gate"]).reshape(1, 512), "gla_head_norm": f(inputs["gla_head_norm"][0]),
        "gla_w_out": f(inputs["gla_w_out"][0]), "sb_w_kv": f(inputs["sb_w_kv"]), "sb_w_q": f(inputs["sb_w_q"][0]),
        "sb_w_out": f(inputs["sb_w_out"][0]), "ffn_w_gate": f(inputs["ffn_w_gate"][0]), "ffn_w_up": f(inputs["ffn_w_up"][0]),
        "ffn_w_down": f(inputs["ffn_w_down"][0]), "moe_w_router": f(inputs["moe_w_router"][0]),
        "moe_w_gate": f(inputs["moe_w_gate"][0]), "moe_w_up": f(inputs["moe_w_up"][0]), "moe_w_down": f(inputs["moe_w_down"][0]),
    }
    x = f(inputs["x"])
    maps = []
    for b in range(x.shape[0]):
        m = dict(shared)
        m["x"] = x[b]
        maps.append(m)
    return maps


_NC_CACHE = {}


def kernel(**inputs):
    maps = host_inputs(inputs)
    if "full" not in _NC_CACHE:
        _NC_CACHE["full"] = build()
    nc = _NC_CACHE["full"]
    res = run_bass_kernel_spmd(nc, maps, core_ids=list(range(len(maps))))
    return np.stack([np.asarray(r["out"], dtype=np.float32) for r in res.results], axis=0)
```

```python
import math
import numpy as np
from contextlib import ExitStack
import concourse.bass as bass
import concourse.mybir as mybir
from concourse.bass_utils import run_bass_kernel_spmd

F32 = mybir.dt.float32
BF16 = mybir.dt.bfloat16
AF = mybir.ActivationFunctionType
ALU = mybir.AluOpType

S_LEN = 4096
D = 1024
NT = S_LEN // 128
EPS = 1e-6
GLA_IN = 3088
FFN_DIM = 2816
EXPERT_DIM = 3584
NEXP = 8


class Buf:
    __slots__ = ("w", "r", "name")

    def __init__(self, name=""):
        self.w = []
        self.r = {}
        self.name = name


class _Eng:
    def __init__(self, name, h, sem, sid):
        self.name, self.h, self.sem, self.sid = name, h, sem, sid
        self.cnt = 0
        self.ops = []
        self.seen = {}


class Sched:
    NDMA = 12

    def __init__(self, nc, es):
        self.nc = nc
        self.sems = []
        self.eng = {}
        for name, h in [("pe", nc.tensor), ("act", nc.scalar), ("dve", nc.vector),
                        ("pool", nc.gpsimd), ("sp", nc.sync)]:
            sem = es.enter_context(nc.semaphore("s_" + name))
            self.sems.append(sem)
            self.eng[name] = _Eng(name, h, sem, len(self.sems) - 1)
        self.dq = {}
        for q in ("sp", "act", "pool"):
            lst = []
            for i in range(self.NDMA):
                sem = es.enter_context(nc.semaphore("d_%s%d" % (q, i)))
                self.sems.append(sem)
                lst.append([len(self.sems) - 1, 0])
            self.dq[q] = [lst, 0]

    def _wait(self, e, tok):
        if tok is None:
            return
        sid, v = tok
        if e.name == "pe" and sid == e.sid:
            return
        if e.seen.get(sid, 0) >= v:
            return
        e.seen[sid] = v
        sem = self.sems[sid]
        e.ops.append(lambda h, sem=sem, v=v: h.wait_ge(sem, v))

    def _deps(self, e, reads, writes, xr=(), dma_fill=False):
        for b in reads:
            for tk in b.w:
                self._wait(e, tk)
        for b in xr:
            for tk in b.w:
                self._wait(e, tk)
            for sid, v in list(b.r.items()):
                self._wait(e, (sid, v))
        for b in writes:
            if dma_fill and not b.r and b.w and all(sid >= 5 for sid, _ in b.w):
                continue
            for tk in b.w:
                self._wait(e, tk)
            for sid, v in list(b.r.items()):
                self._wait(e, (sid, v))

    def op(self, eng, fn, reads=(), writes=(), inc=True, xr=()):
        e = self.eng[eng]
        self._deps(e, reads, writes, xr)
        reads = list(reads) + list(xr)
        if inc:
            e.cnt += 1
            v = e.cnt
            sem = e.sem
            e.ops.append(lambda h, fn=fn, sem=sem: fn(h).then_inc(sem, 1))
        else:
            v = e.cnt + 1
            e.ops.append(lambda h, fn=fn: fn(h))
        tok = (e.sid, v)
        for b in writes:
            b.w = [tok]
            b.r = {}
        for b in reads:
            b.r[e.sid] = v

    def dma(self, q, out, in_, reads=(), writes=(), fn=None, **kw):
        e = self.eng[q]
        fill = {id(b): (not b.r and bool(b.w) and all(sid >= 5 for sid, _ in b.w)) for b in writes}
        self._deps(e, reads, writes, dma_fill=True)
        lst, idx = self.dq[q]
        self.dq[q][1] = (idx + 1) % len(lst)
        ent = lst[idx]
        sid = ent[0]
        if ent[1] > 0:
            self._wait(e, (sid, ent[1]))
        ent[1] += 16
        v = ent[1]
        sem = self.sems[sid]
        if fn is None:
            e.ops.append(lambda h, out=out, in_=in_, sem=sem, kw=kw:
                         h.dma_start(out=out, in_=in_, **kw).then_inc(sem, 16))
        else:
            e.ops.append(lambda h, fn=fn, sem=sem: fn(h).then_inc(sem, 16))
        tok = (sid, v)
        for b in writes:
            if fill[id(b)]:
                b.w = [t for t in b.w if t[0] != sid] + [tok]
            else:
                b.w = [tok]
            b.r = {}
        for b in reads:
            b.r[sid] = v

    def barrier(self):
        toks = []
        for e in self.eng.values():
            if e.cnt > 0:
                toks.append((e.sid, e.cnt))
        for q, (lst, _) in self.dq.items():
            for sid, v in lst:
                if v > 0:
                    toks.append((sid, v))
        for e in self.eng.values():
            for t in toks:
                self._wait(e, t)

    def emit(self):
        nc = self.nc
        self.barrier()
        with nc.Block() as block:
            @block.sync
            def _(h):
                for f in self.eng["sp"].ops:
                    f(h)

            @block.tensor
            def _(h):
                for f in self.eng["pe"].ops:
                    f(h)

            @block.scalar
            def _(h):
                for f in self.eng["act"].ops:
                    f(h)

            @block.vector
            def _(h):
                for f in self.eng["dve"].ops:
                    f(h)

            @block.gpsimd
            def _(h):
                for f in self.eng["pool"].ops:
                    f(h)


class KB:
    def __init__(self, nc, es):
        self.nc = nc
        self.es = es
        self.S = Sched(nc, es)
        self.banks = [es.enter_context(nc.psum_tensor("pb%d" % i, [128, 512], F32)) for i in range(8)]
        self.PB = [Buf("pb%d" % i) for i in range(8)]
        self.n = 0

    def sb(self, st, shape, dt, name=None):
        self.n += 1
        return st.enter_context(self.nc.sbuf_tensor("%s_%d" % (name or "t", self.n), shape, dt))

    def consts(self):
        S, es = self.S, self.es
        self.identf = self.sb(es, [128, 128], F32, "identf")
        self.ident = self.sb(es, [128, 128], BF16, "ident")
        self.trif = self.sb(es, [128, 128], F32, "trif")
        self.epst = self.sb(es, [128, 1], F32, "eps")
        self.bconst = Buf("const")
        bc = self.bconst
        identf, ident, trif, epst = self.identf, self.ident, self.trif, self.epst
        S.op("pool", lambda h: h.memset(identf[:], 0.0), writes=[bc])
        S.op("pool", lambda h: h.affine_select(out=identf[:], in_=identf[:], pattern=[[-1, 128]],
                                               compare_op=ALU.not_equal, fill=1.0, base=0, channel_multiplier=1),
             writes=[bc])
        S.op("dve", lambda h: h.tensor_copy(out=ident[:], in_=identf[:]), writes=[bc])
        S.op("pool", lambda h: h.memset(trif[:], 1.0), writes=[bc])
        S.op("pool", lambda h: h.affine_select(out=trif[:], in_=trif[:], pattern=[[1, 128]],
                                               compare_op=ALU.is_ge, fill=0.0, base=0, channel_multiplier=-1),
             writes=[bc])
        S.op("pool", lambda h: h.memset(trif[0:64, 64:128], 0.0), writes=[bc])
        S.op("dve", lambda h: h.memset(epst[:], EPS), writes=[bc])

    def norm_T(self, st_tmp, xt, bx, outs):
        S = self.S
        junk, bj, ss, bss, xn, bxn = st_tmp
        S.op("act", lambda h: h.activation(out=junk[:], in_=xt, func=AF.Square, accum_out=ss[:, 0:1]),
             reads=[bx], writes=[bj, bss])
        S.op("act", lambda h: h.activation(out=ss[:, 1:2], in_=ss[:, 0:1], func=AF.Sqrt, scale=1.0 / D,
                                           bias=self.epst[:, 0:1]), reads=[self.bconst], writes=[bss])
        S.op("dve", lambda h: h.reciprocal(out=ss[:, 2:3], in_=ss[:, 1:2]), writes=[bss])
        pT = self.banks[0][:].bitcast(BF16)
        for (g, bgain, dst, bdst) in outs:
            S.op("dve", lambda h, g=g: h.scalar_tensor_tensor(out=xn[:], in0=xt, scalar=ss[:, 2:3], in1=g,
                                                            op0=ALU.mult, op1=ALU.mult),
                 reads=[bx, bss, bgain, self.bconst], writes=[bxn])
            for kc in range(8):
                S.op("pe", lambda h, kc=kc: h.transpose(out=pT[:, kc * 128:(kc + 1) * 128],
                                                        in_=xn[:, kc * 128:(kc + 1) * 128], identity=self.ident[:]),
                     reads=[bxn, self.bconst], writes=[self.PB[0]], inc=(kc == 7))
            S.op("act", lambda h, dst=dst: h.copy(out=dst, in_=pT.rearrange("p (a b) -> p a b", a=8)),
                 xr=[self.PB[0]], writes=[bdst])

    def norm_tmp(self, st):
        return (self.sb(st, [128, 1024], BF16, "junk"), Buf("junk"), self.sb(st, [128, 4], F32, "ss"), Buf("ss"),
                self.sb(st, [128, 1024], BF16, "xn"), Buf("xn"))

    def load_w_bf16(self, dst, src, bdst, kcs, ncols, c0=0):
        for kc in range(kcs):
            self.S.dma("pool", dst[:, kc, :], src[kc * 128:(kc + 1) * 128, c0:c0 + ncols], writes=[bdst])


def stage_gla(kb, x, h1, attn_norm0, w_in, w_gu, b_gate, g_head, w_out, ntiles=NT):
    nc, S, banks, PB = kb.nc, kb.S, kb.banks, kb.PB
    with ExitStack() as st:
        sb = lambda shape, dt, name=None: kb.sb(st, shape, dt, name)
        w_in_sb = sb([128, 8, GLA_IN], BF16, "w_in")
        w_out_sb = sb([128, 8, 1024], BF16, "w_out")
        bw = Buf("w")
        kb.load_w_bf16(w_in_sb, w_in, bw, 8, GLA_IN)
        kb.load_w_bf16(w_out_sb, w_out, bw, 8, 1024)
        wgu = sb([17, 512], F32, "wgu")
        S.dma("sp", wgu[0:16, :], w_gu, writes=[bw])
        S.dma("sp", wgu[16:17, :], b_gate, writes=[bw])
        g_attn = sb([128, 1024], F32, "g_attn")
        S.dma("sp", g_attn[:], attn_norm0.partition_broadcast(128), writes=[bw])
        g_hd = sb([128, 256], F32, "g_hd")
        S.dma("sp", g_hd[:], g_head.partition_broadcast(128), writes=[bw])
        ntmp = kb.norm_tmp(st)
        xts = [sb([128, 1024], F32, "xt") for _ in range(2)]
        bxts = [Buf("xt") for _ in range(2)]
        xnT = sb([128, 8, 128], BF16, "xnT"); bxnT = Buf("xnT")
        v_bf = sb([128, 1024], BF16, "v"); bv = Buf("v")
        sr = sb([128, 1024], F32, "sr"); bsr = Buf("sr")
        glrT = sb([17, 128], F32, "glrT"); bglr = Buf("glrT")
        S.op("dve", lambda h: h.memset(glrT[:], 1.0), writes=[bglr])
        e1 = sb([128, 512], F32, "e1"); be1 = Buf("e1")
        lap = sb([128, 512], F32, "lap"); blap = Buf("lap")
        ek_tm = sb([128, 512], F32, "ek_tm"); bek = Buf("ek_tm")
        kinv_tm = sb([128, 512], BF16, "kinv_tm"); bkinv = Buf("kinv_tm")
        eq = sb([128, 512], F32, "eq"); beq = Buf("eq")
        ekT = sb([128, 512], F32, "ekT"); bekT = Buf("ekT")
        dec = sb([128, 4, 2], F32, "dec"); bdec = Buf("dec")
        q_even = sb([128, 4, 128], BF16, "q_even"); q_odd = sb([128, 4, 128], BF16, "q_odd"); bq = Buf("q")
        S.op("dve", lambda h: h.memset(q_even[:], 0.0), writes=[bq])
        S.op("dve", lambda h: h.memset(q_odd[:], 0.0), writes=[bq])
        kinvT = sb([128, 4, 128], BF16, "kinvT"); bkT = Buf("kinvT")
        attnT = sb([128, 4, 128], BF16, "attnT"); battn = Buf("attnT")
        Sf = sb([128, 4, 256], F32, "Sf"); bSf = Buf("Sf")
        Sb = sb([128, 4, 256], BF16, "Sb"); bSb = Buf("Sb")
        Sm = sb([128, 4, 256], BF16, "Sm"); bSm = Buf("Sm")
        t1 = sb([128, 256], F32, "t1"); bt1 = Buf("t1")
        S.op("dve", lambda h: h.memset(Sf[:], 0.0), writes=[bSf])
        S.op("dve", lambda h: h.memset(Sb[:], 0.0), writes=[bSb])
        hs = sb([128, 8], F32, "hs"); bhs = Buf("hs")
        og = sb([128, 256], F32, "og"); bog = Buf("og")
        og_bf = sb([128, 1024], BF16, "og_bf"); bogb = Buf("og_bf")
        ogT = sb([128, 8, 128], BF16, "ogT"); bogT = Buf("ogT")
        h1t = sb([128, 1024], F32, "h1t"); bh1 = Buf("h1t")
        junk2 = sb([128, 256], BF16, "junk2"); bj2 = Buf("junk2")
        pTb = banks[0][:].bitcast(BF16)
        qscale = math.log(128.0 ** -0.5)
        lnq = sb([128, 1], F32, "lnq")
        S.op("dve", lambda h: h.memset(lnq[:], qscale), writes=[kb.bconst])

        for t in range(ntiles):
            xt = xts[t % 2]; bx = bxts[t % 2]
            S.dma("sp", xt[:], x[t * 128:(t + 1) * 128, :], writes=[bx])
            kb.norm_T(ntmp, xt[:], bx, [(g_attn[:], bw, xnT[:], bxnT)])

            def proj_tm(bank, c0, n=512):
                for kc in range(8):
                    S.op("pe", lambda h, kc=kc: h.matmul(banks[bank][:, 0:n], lhsT=xnT[:, kc, :], rhs=w_in_sb[:, kc, c0:c0 + n],
                                                         start=(kc == 0), stop=(kc == 7)),
                         reads=[bxnT, bw], writes=[PB[bank]], inc=(kc == 7))

            def proj_fm(bank, c0, m, col0):
                for kc in range(8):
                    S.op("pe", lambda h, kc=kc: h.matmul(banks[bank][0:m, col0:col0 + 128], lhsT=w_in_sb[:, kc, c0:c0 + m],
                                                         rhs=xnT[:, kc, :], start=(kc == 0), stop=(kc == 7)),
                         reads=[bxnT, bw], writes=[PB[bank]], inc=(kc == 7))

            proj_tm(1, 512)
            proj_tm(2, 1024); proj_tm(3, 1536)
            S.op("act", lambda h: h.copy(out=v_bf[:, 0:512], in_=banks[2][:]), xr=[PB[2]], writes=[bv])
            S.op("act", lambda h: h.copy(out=v_bf[:, 512:1024], in_=banks[3][:]), xr=[PB[3]], writes=[bv])
            proj_tm(2, 2064); proj_tm(3, 2576)
            S.op("act", lambda h: h.activation(out=sr[:, 0:512], in_=banks[2][:], func=AF.Silu), xr=[PB[2]], writes=[bsr])
            S.op("act", lambda h: h.activation(out=sr[:, 512:1024], in_=banks[3][:], func=AF.Silu), xr=[PB[3]], writes=[bsr])
            proj_fm(6, 2048, 16, 0)
            S.op("dve", lambda h: h.tensor_copy(out=glrT[0:16, :], in_=banks[6][0:16, 0:128]), xr=[PB[6]], writes=[bglr])
            for hh in range(4):
                proj_fm(4, hh * 128, 128, hh * 128)
            for hh in range(4):
                proj_fm(5, 512 + hh * 128, 128, hh * 128)
            S.op("pe", lambda h: h.matmul(banks[6][:], lhsT=glrT[:, :], rhs=wgu[:, :], start=True, stop=True, skip_group_check=True),
                 reads=[bglr, bw], writes=[PB[6]])
            S.op("act", lambda h: h.activation(out=e1[:], in_=banks[6][:], func=AF.Exp, scale=-1.0), xr=[PB[6]], writes=[be1])
            S.op("act", lambda h: h.activation(out=lap[:], in_=e1[:], func=AF.Ln, bias=1.0), reads=[be1], writes=[blap])
            S.op("pe", lambda h: h.matmul(banks[6][:], lhsT=kb.trif[:], rhs=lap[:], start=True, stop=True, skip_group_check=True),
                 reads=[blap, kb.bconst], writes=[PB[6]])
            S.op("act", lambda h: h.activation(out=ek_tm[:], in_=banks[6][:], func=AF.Exp, scale=1.0 / 16), xr=[PB[6]], writes=[bek])
            S.op("dve", lambda h: h.tensor_tensor(out=kinv_tm[:], in0=banks[1][:], in1=ek_tm[:], op=ALU.mult),
                 xr=[PB[1]], reads=[bek], writes=[bkinv])
            for hh in range(4):
                S.op("pe", lambda h, hh=hh: h.matmul(banks[7][:, hh * 128:(hh + 1) * 128], lhsT=lap[:, hh * 128:(hh + 1) * 128],
                                                     rhs=kb.trif[:], start=True, stop=True, skip_group_check=True),
                     reads=[blap, kb.bconst], writes=[PB[7]], inc=(hh == 3))
            S.op("act", lambda h: h.activation(out=eq[:], in_=banks[7][:], func=AF.Exp, scale=-1.0 / 16, bias=lnq[:, 0:1]),
                 xr=[PB[7]], reads=[kb.bconst], writes=[beq])
            S.op("act", lambda h: h.activation(out=ekT[:], in_=banks[7][:], func=AF.Exp, scale=1.0 / 16), xr=[PB[7]], writes=[bekT])
            cbv = banks[7][:].rearrange("p (h c i) -> p h c i", h=4, c=2)
            S.op("act", lambda h: h.activation(out=dec[:], in_=cbv[:, :, :, 63], func=AF.Exp, scale=-1.0 / 16),
                 xr=[PB[7]], writes=[bdec])
            qv = banks[4][:].rearrange("p (h i) -> p h i", h=4)
            eqv = eq[:].rearrange("p (h i) -> p h i", h=4)
            S.op("dve", lambda h: h.tensor_tensor(out=q_even[:, :, 0:64], in0=qv[:, :, 0:64], in1=eqv[:, :, 0:64], op=ALU.mult),
                 xr=[PB[4]], reads=[beq], writes=[bq])
            S.op("dve", lambda h: h.tensor_tensor(out=q_odd[:, :, 64:128], in0=qv[:, :, 64:128], in1=eqv[:, :, 64:128], op=ALU.mult),
                 xr=[PB[4]], reads=[beq], writes=[bq])
            S.op("dve", lambda h: h.tensor_tensor(out=kinvT[:].rearrange("p h i -> p (h i)"), in0=banks[5][:], in1=ekT[:], op=ALU.mult),
                 xr=[PB[5]], reads=[bekT], writes=[bkT])
            for hh in range(4):
                S.op("pe", lambda h, hh=hh: h.matmul(banks[7][:, hh * 128:(hh + 1) * 128], lhsT=kinvT[:, hh, :], rhs=q_even[:, hh, :],
                                                     start=True, stop=False), reads=[bkT, bq], writes=[PB[7]], inc=False)
                S.op("pe", lambda h, hh=hh: h.matmul(banks[7][:, hh * 128:(hh + 1) * 128], lhsT=kinvT[:, hh, :], rhs=q_odd[:, hh, :],
                                                     start=False, stop=True), reads=[bkT, bq], writes=[PB[7]], inc=(hh == 3))
            for hh in range(4):
                S.op("dve", lambda h, hh=hh: h.tensor_tensor(out=attnT[:, hh, :], in0=banks[7][:, hh * 128:(hh + 1) * 128],
                                                             in1=kb.trif[:], op=ALU.mult),
                     xr=[PB[7]], reads=[kb.bconst], writes=[battn])
            for hh in range(4):
                ob = 2 + hh // 2
                oc = (hh % 2) * 256
                vs = v_bf[:, hh * 256:(hh + 1) * 256]
                S.op("pe", lambda h, hh=hh, ob=ob, oc=oc: h.matmul(banks[ob][:, oc:oc + 256], lhsT=q_even[:, hh, :], rhs=Sb[:, hh, :],
                                                                   start=True, stop=False),
                     reads=[bq, bSb], writes=[PB[ob]], inc=False)
                S.op("pe", lambda h, hh=hh, ob=ob, oc=oc, vs=vs: h.matmul(banks[ob][:, oc:oc + 256], lhsT=attnT[:, hh, :], rhs=vs,
                                                                          start=False, stop=False),
                     reads=[battn, bv], writes=[PB[ob]], inc=False)
                S.op("pe", lambda h, hh=hh: h.matmul(banks[6][:, 0:256], lhsT=kinv_tm[0:64, hh * 128:(hh + 1) * 128],
                                                     rhs=v_bf[0:64, hh * 256:(hh + 1) * 256], start=True, stop=True),
                     reads=[bkinv, bv], writes=[PB[6]])
                S.op("dve", lambda h, hh=hh: h.tensor_tensor(out=t1[:], in0=banks[6][:, 0:256], in1=Sf[:, hh, :], op=ALU.add),
                     xr=[PB[6]], reads=[bSf], writes=[bt1])
                S.op("dve", lambda h, hh=hh: h.tensor_scalar(out=Sf[:, hh, :], in0=t1[:], scalar1=dec[:, hh, 0:1], scalar2=None, op0=ALU.mult),
                     reads=[bt1, bdec], writes=[bSf])
                S.op("act", lambda h, hh=hh: h.copy(out=Sm[:, hh, :], in_=Sf[:, hh, :]), reads=[bSf], writes=[bSm])
                S.op("pe", lambda h, hh=hh, ob=ob, oc=oc: h.matmul(banks[ob][:, oc:oc + 256], lhsT=q_odd[:, hh, :], rhs=Sm[:, hh, :],
                                                                   start=False, stop=True),
                     reads=[bq, bSm], writes=[PB[ob]])
                S.op("pe", lambda h, hh=hh: h.matmul(banks[6][:, 256:512], lhsT=kinv_tm[64:128, hh * 128:(hh + 1) * 128],
                                                     rhs=v_bf[64:128, hh * 256:(hh + 1) * 256], start=True, stop=True),
                     reads=[bkinv, bv], writes=[PB[6]])
                S.op("dve", lambda h, hh=hh: h.tensor_tensor(out=t1[:], in0=banks[6][:, 256:512], in1=Sf[:, hh, :], op=ALU.add),
                     xr=[PB[6]], reads=[bSf], writes=[bt1])
                S.op("dve", lambda h, hh=hh: h.tensor_scalar(out=Sf[:, hh, :], in0=t1[:], scalar1=dec[:, hh, 1:2], scalar2=None, op0=ALU.mult),
                     reads=[bt1, bdec], writes=[bSf])
                S.op("act", lambda h, hh=hh: h.copy(out=Sb[:, hh, :], in_=Sf[:, hh, :]), reads=[bSf], writes=[bSb])
                S.op("act", lambda h, hh=hh, ob=ob, oc=oc: h.activation(out=junk2[:], in_=banks[ob][:, oc:oc + 256], func=AF.Square,
                                                                        accum_out=hs[:, 0:1]), xr=[PB[ob]], writes=[bj2, bhs])
                S.op("act", lambda h: h.activation(out=hs[:, 1:2], in_=hs[:, 0:1], func=AF.Sqrt, scale=1.0 / 256, bias=kb.epst[:, 0:1]),
                     reads=[kb.bconst], writes=[bhs])
                S.op("dve", lambda h: h.reciprocal(out=hs[:, 2:3], in_=hs[:, 1:2]), writes=[bhs])
                S.op("dve", lambda h, ob=ob, oc=oc: h.scalar_tensor_tensor(out=og[:], in0=banks[ob][:, oc:oc + 256], scalar=hs[:, 2:3], in1=g_hd[:],
                                                                           op0=ALU.mult, op1=ALU.mult),
                     xr=[PB[ob]], reads=[bhs, bw], writes=[bog])
                S.op("dve", lambda h, hh=hh: h.tensor_tensor(out=og_bf[:, hh * 256:(hh + 1) * 256], in0=og[:], in1=sr[:, hh * 256:(hh + 1) * 256], op=ALU.mult),
                     reads=[bog, bsr], writes=[bogb])
            for kc in range(8):
                S.op("pe", lambda h, kc=kc: h.transpose(out=pTb[:, kc * 128:(kc + 1) * 128], in_=og_bf[:, kc * 128:(kc + 1) * 128],
                                                        identity=kb.ident[:]), reads=[bogb, kb.bconst], writes=[PB[0]], inc=(kc == 7))
            S.op("act", lambda h: h.copy(out=ogT[:], in_=pTb.rearrange("p (a b) -> p a b", a=8)), xr=[PB[0]], writes=[bogT])
            for half in range(2):
                bank = 4 + half
                for kc in range(8):
                    S.op("pe", lambda h, kc=kc, half=half, bank=bank: h.matmul(banks[bank][:], lhsT=ogT[:, kc, :],
                                                                               rhs=w_out_sb[:, kc, half * 512:(half + 1) * 512],
                                                                               start=(kc == 0), stop=(kc == 7)),
                         reads=[bogT, bw], writes=[PB[bank]], inc=(kc == 7))
                S.op("dve", lambda h, half=half, bank=bank, xt=xt: h.tensor_tensor(out=h1t[:, half * 512:(half + 1) * 512], in0=banks[bank][:],
                                                                                  in1=xt[:, half * 512:(half + 1) * 512], op=ALU.add),
                     xr=[PB[bank]], reads=[bx], writes=[bh1])
            S.dma("sp", h1[t * 128:(t + 1) * 128, :], h1t[:], reads=[bh1])
        S.barrier()


def swiglu_acc(kb, st_bufs, xnT, bxnT, ntok, wg, wu, wd, F, acc, bacc, gate_ap_fn, bgate=None, wload=None, acc_init=False, mid_hook=None):
    S, banks, PB = kb.S, kb.banks, kb.PB
    (wg_sb, wu_sb, wd_sb, bws, aT, baT, sg, bsg) = st_bufs
    nfc = F // 128
    GS = 4
    ngr = (nfc + GS - 1) // GS
    ntt = ntok // 512
    nsub = ntok // 128
    cnt = getattr(kb, "_swg_cnt", 0)
    for gi in range(ngr):
        f0 = gi * GS
        nf = min(GS, nfc - f0)
        slot = cnt % 2
        cnt += 1
        bw = bws[slot]
        if wload is not None:
            wload(slot, f0, nf, bw, wg_sb, wu_sb, wd_sb)
        else:
            for kc in range(8):
                S.dma("pool", wg_sb[slot][:, kc, 0:nf * 128], wg[kc * 128:(kc + 1) * 128, f0 * 128:(f0 + nf) * 128], writes=[bw])
                S.dma("pool", wu_sb[slot][:, kc, 0:nf * 128], wu[kc * 128:(kc + 1) * 128, f0 * 128:(f0 + nf) * 128], writes=[bw])
            for fc in range(nf):
                S.dma("pool", wd_sb[slot][:, fc, :], wd[(f0 + fc) * 128:(f0 + fc + 1) * 128, :], writes=[bw])
        for fc in range(nf):
            for tt in range(ntt):
                pg = 2 + (tt % 2) * 2
                pu = pg + 1
                for kc in range(8):
                    S.op("pe", lambda h, kc=kc, fc=fc, tt=tt, pg=pg, slot=slot: h.matmul(
                        banks[pg][:], lhsT=wg_sb[slot][:, kc, fc * 128:(fc + 1) * 128], rhs=xnT[:, kc, tt * 512:(tt + 1) * 512],
                        start=(kc == 0), stop=(kc == 7)), reads=[bw, bxnT], writes=[PB[pg]], inc=(kc == 7))
                for kc in range(8):
                    S.op("pe", lambda h, kc=kc, fc=fc, tt=tt, pu=pu, slot=slot: h.matmul(
                        banks[pu][:], lhsT=wu_sb[slot][:, kc, fc * 128:(fc + 1) * 128], rhs=xnT[:, kc, tt * 512:(tt + 1) * 512],
                        start=(kc == 0), stop=(kc == 7)), reads=[bw, bxnT], writes=[PB[pu]], inc=(kc == 7))
                sgt = sg[tt % 2]
                S.op("act", lambda h, pg=pg, sgt=sgt: h.activation(out=sgt[:], in_=banks[pg][:], func=AF.Silu),
                     xr=[PB[pg]], writes=[bsg[tt % 2]])
                S.op("dve", lambda h, pu=pu, sgt=sgt, fc=fc, tt=tt, slot=slot: h.tensor_tensor(
                    out=aT[slot][:, fc, tt * 512:(tt + 1) * 512], in0=banks[pu][:], in1=sgt[:], op=ALU.mult),
                    xr=[PB[pu]], reads=[bsg[tt % 2]], writes=[baT[slot]])
        if mid_hook is not None and gi == ngr - 1:
            mid_hook()
        for sub in range(nsub):
            for half in range(2):
                pd = (6, 7, 0, 1)[(sub * 2 + half) % 4]
                for fc in range(nf):
                    S.op("pe", lambda h, fc=fc, sub=sub, half=half, pd=pd, slot=slot, nf=nf: h.matmul(
                        banks[pd][:], lhsT=aT[slot][:, fc, sub * 128:(sub + 1) * 128], rhs=wd_sb[slot][:, fc, half * 512:(half + 1) * 512],
                        start=(fc == 0), stop=(fc == nf - 1)), reads=[baT[slot], bw], writes=[PB[pd]], inc=(fc == nf - 1))
                g = gate_ap_fn(sub)
                if acc_init and gi == 0:
                    S.op("dve", lambda h, sub=sub, half=half, pd=pd: h.tensor_copy(out=acc[:, sub, half * 512:(half + 1) * 512], in_=banks[pd][:]),
                         xr=[PB[pd]], writes=[bacc])
                    continue
                S.op("dve", lambda h, sub=sub, half=half, pd=pd, g=g: h.scalar_tensor_tensor(
                    out=acc[:, sub, half * 512:(half + 1) * 512], in0=banks[pd][:], scalar=(1.0 if g is None else g),
                    in1=acc[:, sub, half * 512:(half + 1) * 512], op0=ALU.mult, op1=ALU.add),
                    xr=[PB[pd]], reads=([bacc] if bgate is None else [bacc, bgate]), writes=[bacc])
    kb._swg_cnt = cnt


def swiglu_bufs(kb, st, ntok):
    sb = lambda shape, dt, name=None: kb.sb(st, shape, dt, name)
    wg_sb = [sb([128, 8, 512], BF16, "wg") for _ in range(2)]
    wu_sb = [sb([128, 8, 512], BF16, "wu") for _ in range(2)]
    wd_sb = [sb([128, 4, 1024], BF16, "wd") for _ in range(2)]
    bws = [Buf("w0"), Buf("w1")]
    aT0 = sb([128, 4, ntok], BF16, "aT")
    aT = [aT0, aT0]
    baT0 = Buf("aT0")
    baT = [baT0, baT0]
    sg = [sb([128, 512], F32, "sg") for _ in range(2)]
    bsg = [Buf("sg0"), Buf("sg1")]
    return (wg_sb, wu_sb, wd_sb, bws, aT, baT, sg, bsg)


def stage_ffn(kb, h1, h2, ffn_norm0, wg, wu, wd, TB=2048):
    S = kb.S
    with ExitStack() as st:
        sb = lambda shape, dt, name=None: kb.sb(st, shape, dt, name)
        g_bc = sb([128, 1024], F32, "g_ffn")
        bg = Buf("g")
        S.dma("sp", g_bc[:], ffn_norm0.partition_broadcast(128), writes=[bg])
        ntmp = kb.norm_tmp(st)
        xnT = sb([128, 8, TB], BF16, "xnT"); bxnT = Buf("xnT")
        acc = sb([128, TB // 128, 1024], F32, "acc"); bacc = Buf("acc")
        bufs = swiglu_bufs(kb, st, TB)
        for blk in range(S_LEN // TB):
            for sub in range(TB // 128):
                r0 = blk * TB + sub * 128
                S.dma("sp", acc[:, sub, :], h1[r0:r0 + 128, :], writes=[bacc])
                kb.norm_T(ntmp, acc[:, sub, :], bacc, [(g_bc[:], bg, xnT[:, :, sub * 128:(sub + 1) * 128], bxnT)])
            swiglu_acc(kb, bufs, xnT, bxnT, TB, wg, wu, wd, FFN_DIM, acc, bacc, lambda sub: None)
            for sub in range(TB // 128):
                r0 = blk * TB + sub * 128
                S.dma("sp", h2[r0:r0 + 128, :], acc[:, sub, :], reads=[bacc])
        S.barrier()


def stage_qkv(kb, h2, qT_d, kT_d, v_d, kv_norm, attn_norm1, w_kv, w_q):
    S, banks, PB = kb.S, kb.banks, kb.PB
    with ExitStack() as st:
        sb = lambda shape, dt, name=None: kb.sb(st, shape, dt, name)
        bw = Buf("w")
        wq_sb = sb([128, 8, 1024], BF16, "wq")
        wkv_sb = sb([128, 8, 2048], BF16, "wkv")
        kb.load_w_bf16(wq_sb, w_q, bw, 8, 1024)
        kb.load_w_bf16(wkv_sb, w_kv, bw, 8, 2048)
        g_kv = sb([128, 1024], F32, "g_kv")
        g_q = sb([128, 1024], F32, "g_q")
        S.dma("sp", g_kv[:], kv_norm.partition_broadcast(128), writes=[bw])
        S.dma("sp", g_q[:], attn_norm1.partition_broadcast(128), writes=[bw])
        ntmp = kb.norm_tmp(st)
        TB = 512
        xt = [sb([128, 1024], F32, "xt") for _ in range(2)]
        bxt = [Buf("xt0"), Buf("xt1")]
        xkvT = sb([128, 8, TB], BF16, "xkvT"); bxkv = Buf("xkvT")
        xqT = sb([128, 8, TB], BF16, "xqT"); bxq = Buf("xqT")
        stg = [sb([128, 512], BF16, "stg") for _ in range(3)]
        bstg = [Buf("stg%d" % i) for i in range(3)]
        sc = 128.0 ** -0.5
        n = 0
        for blk in range(S_LEN // TB):
            for sub in range(TB // 128):
                r0 = blk * TB + sub * 128
                x_ = xt[sub % 2]; bx_ = bxt[sub % 2]
                S.dma("sp", x_[:], h2[r0:r0 + 128, :], writes=[bx_])
                kb.norm_T(ntmp, x_[:], bx_, [(g_kv[:], bw, xkvT[:, :, sub * 128:(sub + 1) * 128], bxkv),
                                            (g_q[:], bw, xqT[:, :, sub * 128:(sub + 1) * 128], bxq)])
            t0 = blk * TB
            for hh in range(8):
                for (wsb, c0, xT_, bx2, dst, scale) in ((wq_sb, hh * 128, xqT, bxq, qT_d, sc), (wkv_sb, hh * 128, xkvT, bxkv, kT_d, 1.0)):
                    bank = 1 + (n % 3); sidx = n % 3; n += 1
                    for kc in range(8):
                        S.op("pe", lambda h, kc=kc, wsb=wsb, c0=c0, xT_=xT_, bank=bank: h.matmul(
                            banks[bank][:], lhsT=wsb[:, kc, c0:c0 + 128], rhs=xT_[:, kc, :], start=(kc == 0), stop=(kc == 7)),
                            reads=[bw, bx2], writes=[PB[bank]], inc=(kc == 7))
                    S.op("act", lambda h, bank=bank, sidx=sidx, scale=scale: h.activation(out=stg[sidx][:], in_=banks[bank][:], func=AF.Copy, scale=scale),
                         xr=[PB[bank]], writes=[bstg[sidx]])
                    S.dma("sp", dst[hh, :, t0:t0 + TB], stg[sidx][:], reads=[bstg[sidx]])
            for sub in range(TB // 128):
                for half in range(2):
                    bank = 1 + (n % 3); sidx = n % 3; n += 1
                    for kc in range(8):
                        S.op("pe", lambda h, kc=kc, sub=sub, half=half, bank=bank: h.matmul(
                            banks[bank][:], lhsT=xkvT[:, kc, sub * 128:(sub + 1) * 128], rhs=wkv_sb[:, kc, 1024 + half * 512:1024 + (half + 1) * 512],
                            start=(kc == 0), stop=(kc == 7)), reads=[bw, bxkv], writes=[PB[bank]], inc=(kc == 7))
                    S.op("act", lambda h, bank=bank, sidx=sidx: h.copy(out=stg[sidx][:], in_=banks[bank][:]), xr=[PB[bank]], writes=[bstg[sidx]])
                    S.dma("sp", v_d[t0 + sub * 128:t0 + (sub + 1) * 128, half * 512:(half + 1) * 512], stg[sidx][:], reads=[bstg[sidx]])
        S.barrier()


def stage_sb(kb, qT_d, kT_d, v_d, oT_d, nheads=8, nqb=NT):
    S, banks, PB = kb.S, kb.banks, kb.PB
    with ExitStack() as st:
        sb = lambda shape, dt, name=None: kb.sb(st, shape, dt, name)
        qT = [sb([128, S_LEN], BF16, "qT") for _ in range(2)]
        kT = [sb([128, S_LEN], BF16, "kT") for _ in range(2)]
        vh = [sb([128, NT, 128], BF16, "vh") for _ in range(2)]
        bqkv = [Buf("qkv0"), Buf("qkv1")]
        ones = sb([128, 1], F32, "ones")
        cmask = sb([128, 128], F32, "cmask")
        bc = Buf("c")
        S.op("dve", lambda h: h.memset(ones[:], 1.0), writes=[bc])
        S.op("pool", lambda h: h.memset(cmask[:], 1.0), writes=[bc])
        S.op("pool", lambda h: h.affine_select(out=cmask[:], in_=cmask[:], pattern=[[-1, 128]], compare_op=ALU.is_gt,
                                               fill=0.0, base=0, channel_multiplier=1), writes=[bc])
        E = [sb([128, 512], F32, "E") for _ in range(2)]; bE = [Buf("E0"), Buf("E1")]
        SP = [sb([128, S_LEN + 1], F32, "SP") for _ in range(2)]; bSP = [Buf("SP0"), Buf("SP1")]
        for i in range(2):
            S.op("dve", lambda h, i=i: h.memset(SP[i][:, 0:1], 0.0), writes=[bSP[i]])
        G = sb([128, 512], F32, "G"); bG = Buf("G")
        ARG = [sb([128, S_LEN], F32, "ARG") for _ in range(2)]; bARG = [Buf("ARG0"), Buf("ARG1")]
        nt = sb([128, 2], F32, "nt"); bnts = [Buf("nt0"), Buf("nt1")]
        W = sb([128, S_LEN], BF16, "W"); bW = Buf("W")
        WT = sb([128, NT, 128], BF16, "WT"); bWT = Buf("WT")
        oTs = [sb([128, 128], BF16, "oTs") for _ in range(2)]; boT = [Buf("oT0"), Buf("oT1")]
        items = [(hh, tb) for hh in range(nheads) for tb in range(nqb)]
        N = len(items)
        cneg = sb([128, 128], F32, "cneg")
        S.op("dve", lambda h: h.tensor_scalar(out=cneg[:], in0=cmask[:], scalar1=-1.0, scalar2=1e30, op0=ALU.add, op1=ALU.mult),
             writes=[bc])
        TRB = (0, 3, 4, 5)

        bWc = [Buf("W%d" % c) for c in range(4)]
        bWTc = [Buf("WT%d" % c) for c in range(4)]

        def act_extras(j):
            ex = []
            if 0 <= j - 3 < N:
                def f(i=j - 3):
                    hh, tb = items[i]
                    r = i % 2
                    S.op("act", lambda h: h.copy(out=oTs[r][:], in_=banks[7][:, 0:128]), xr=[PB[7]], writes=[boT[r]])
                    S.dma("sp", oT_d[hh, :, tb * 128:(tb + 1) * 128], oTs[r][:], reads=[boT[r]])
                ex.append(f)
            if 0 <= j - 2 < N:
                hh, tb = items[j - 2]
                nb = tb + 1
                for c in range((nb + 7) // 8):
                    def f(c=c, nb=nb):
                        b0 = c * 8
                        nbb = min(8, nb - b0)
                        pT = banks[TRB[c]][:].bitcast(BF16)
                        S.op("act", lambda h: h.copy(out=WT[:, b0:b0 + nbb, :].rearrange("p a b -> p (a b)"), in_=pT[:, 0:nbb * 128]),
                             xr=[PB[TRB[c]]], writes=[bWTc[c]])
                    ex.append(f)
            if 0 <= j - 1 < N:
                hh, tb = items[j - 1]
                ns = (tb + 1) * 128
                r = (j - 1) % 2
                for c in range((ns + 1023) // 1024):
                    def f(c=c, ns=ns, r=r):
                        c0 = c * 1024
                        c1 = min(ns, c0 + 1024)
                        S.op("act", lambda h: h.activation(out=W[:, c0:c1], in_=ARG[r][:, c0:c1], func=AF.Exp, bias=nt[:, r:r + 1]),
                             reads=[bARG[r], bnts[r]], writes=[bWc[c]])
                    ex.append(f)
            return ex

        def pe_extras(j):
            ex = []
            if 0 <= j - 2 < N:
                hh, tb = items[j - 2]
                sl = hh % 2
                nb = tb + 1
                for c in range((nb + 7) // 8):
                    def f(c=c, nb=nb, sl=sl):
                        for b in range(c * 8, min(nb, c * 8 + 8)):
                            S.op("pe", lambda h, b=b: h.matmul(banks[7][:, 0:128], lhsT=vh[sl][:, b, :], rhs=WT[:, b, :], start=(b == 0), stop=(b == nb - 1)),
                                 reads=[bqkv[sl], bWTc[c]], writes=[PB[7]], inc=(b == min(nb, c * 8 + 8) - 1))
                    ex.append(f)
            if 0 <= j - 1 < N:
                hh, tb = items[j - 1]
                nb = tb + 1
                for c in range((nb + 7) // 8):
                    def f(c=c, nb=nb):
                        b0 = c * 8
                        nbb = min(8, nb - b0)
                        pT = banks[TRB[c]][:].bitcast(BF16)
                        for jj in range(nbb):
                            S.op("pe", lambda h, jj=jj: h.transpose(out=pT[:, jj * 128:(jj + 1) * 128], in_=W[:, (b0 + jj) * 128:(b0 + jj + 1) * 128],
                                                                    identity=kb.ident[:]), reads=[bWc[c], kb.bconst], writes=[PB[TRB[c]]], inc=(jj == nbb - 1))
                    ex.append(f)
            return ex

        def pump(aex, pex, cnt):
            if aex:
                aex.pop(0)()
                cnt[0] += 1
            if pex and (cnt[0] >= cnt[1] + 2 or not aex):
                pex.pop(0)()
                cnt[1] += 1

        def s1(i, aex, pex, cnt):
            hh, tb = items[i]
            sl = hh % 2
            r = i % 2
            if tb == 0:
                S.dma("sp", qT[sl][:], qT_d[hh], writes=[bqkv[sl]])
                S.dma("sp", kT[sl][:], kT_d[hh], writes=[bqkv[sl]])
                S.dma("sp", vh[sl][:], v_d[:, hh * 128:(hh + 1) * 128].rearrange("(b p) d -> p b d", p=128), writes=[bqkv[sl]])
            ns = (tb + 1) * 128
            nkt = (ns + 511) // 512
            ZB = (1, 2, 6)

            def zmm(kt):
                s0 = kt * 512
                w = min(512, ns - s0)
                zb = ZB[kt % 3]
                S.op("pe", lambda h: h.matmul(banks[zb][:, 0:w], lhsT=qT[sl][:, tb * 128:(tb + 1) * 128],
                                              rhs=kT[sl][:, s0:s0 + w], start=True, stop=True),
                     reads=[bqkv[sl]], writes=[PB[zb]])

            zmm(0)
            for kt in range(nkt):
                s0 = kt * 512
                w = min(512, ns - s0)
                zb = ZB[kt % 3]
                if kt + 1 < nkt:
                    zmm(kt + 1)
                e_ = E[kt % 2]; be_ = bE[kt % 2]
                S.op("act", lambda h, zb=zb, w=w, e_=e_: h.activation(out=e_[:, 0:w], in_=banks[zb][:, 0:w], func=AF.Exp),
                     xr=[PB[zb]], writes=[be_])
                S.op("act", lambda h, w=w, e_=e_, r=r, s0=s0: h.activation(out=SP[r][:, 1 + s0:1 + s0 + w], in_=e_[:, 0:w], func=AF.Ln, bias=1.0),
                     reads=[be_], writes=[bSP[r]])
                pump(aex, pex, cnt)
                if kt == nkt - 1:
                    d0 = 1 + ns - 128
                    S.op("dve", lambda h, r=r, d0=d0: h.tensor_tensor(out=SP[r][:, d0:d0 + 128], in0=SP[r][:, d0:d0 + 128], in1=cmask[:], op=ALU.mult),
                         reads=[bc], writes=[bSP[r]])
                init = 0.0 if kt == 0 else G[:, 511:512]
                S.op("dve", lambda h, r=r, s0=s0, w=w, init=init: h.tensor_tensor_scan(
                    out=G[:, 0:w], data0=ones[:, 0:1].to_broadcast([128, w]), data1=SP[r][:, s0:s0 + w], initial=init,
                    op0=ALU.mult, op1=ALU.add), reads=[bSP[r], bc], writes=[bG])
                S.op("dve", lambda h, r=r, s0=s0, w=w, zb=zb: h.tensor_tensor(out=ARG[r][:, s0:s0 + w], in0=banks[zb][:, 0:w], in1=G[:, 0:w], op=ALU.add),
                     xr=[PB[zb]], reads=[bG], writes=[bARG[r]])
            lw = ns - (nkt - 1) * 512
            S.op("dve", lambda h, lw=lw, r=r: h.tensor_scalar(out=nt[:, r:r + 1], in0=G[:, lw - 1:lw], scalar1=-1.0, scalar2=None, op0=ALU.mult),
                 reads=[bG], writes=[bnts[r]])
            S.op("dve", lambda h, r=r, ns=ns: h.tensor_tensor(out=ARG[r][:, ns - 128:ns], in0=ARG[r][:, ns - 128:ns], in1=cneg[:], op=ALU.add),
                 reads=[bc], writes=[bARG[r]])

        for j in range(N + 3):
            aex = act_extras(j)
            pex = pe_extras(j)
            cnt = [0, 0]
            if j < N:
                s1(j, aex, pex, cnt)
            while aex or pex:
                pump(aex, pex, cnt)
        S.barrier()


def stage_attn_out(kb, oT_d, h2, h3, w_out):
    S, banks, PB = kb.S, kb.banks, kb.PB
    with ExitStack() as st:
        sb = lambda shape, dt, name=None: kb.sb(st, shape, dt, name)
        bw = Buf("w")
        wo_sb = sb([128, 8, 1024], BF16, "wo")
        kb.load_w_bf16(wo_sb, w_out, bw, 8, 1024)
        TB = 512
        oT = [sb([128, 8, TB], BF16, "oT") for _ in range(2)]; boT = [Buf("oT0"), Buf("oT1")]
        xt = [sb([128, 1024], F32, "xt") for _ in range(2)]; bxt = [Buf("x0"), Buf("x1")]
        n = 0
        for blk in range(S_LEN // TB):
            sl = blk % 2
            for hh in range(8):
                S.dma("sp", oT[sl][:, hh, :], oT_d[hh, :, blk * TB:(blk + 1) * TB], writes=[boT[sl]])
            for sub in range(TB // 128):
                r0 = blk * TB + sub * 128
                x_ = xt[n % 2]; bx_ = bxt[n % 2]; n += 1
                S.dma("sp", x_[:], h2[r0:r0 + 128, :], writes=[bx_])
                for half in range(2):
                    bank = 1 + half
                    for kc in range(8):
                        S.op("pe", lambda h, kc=kc, sub=sub, half=half, bank=bank, sl=sl: h.matmul(
                            banks[bank][:], lhsT=oT[sl][:, kc, sub * 128:(sub + 1) * 128], rhs=wo_sb[:, kc, half * 512:(half + 1) * 512],
                            start=(kc == 0), stop=(kc == 7)), reads=[boT[sl], bw], writes=[PB[bank]], inc=(kc == 7))
                    S.op("dve", lambda h, half=half, bank=bank, x_=x_: h.tensor_tensor(out=x_[:, half * 512:(half + 1) * 512], in0=banks[bank][:],
                                                                                      in1=x_[:, half * 512:(half + 1) * 512], op=ALU.add),
                         xr=[PB[bank]], reads=[bx_], writes=[bx_])
                S.dma("sp", h3[r0:r0 + 128, :], x_[:], reads=[bx_])
        S.barrier()


def stage_moe(kb, h3, out, ffn_norm1, final_norm, w_router, wg, wu, wd, TB=2048, nexp=NEXP):
    S, banks, PB = kb.S, kb.banks, kb.PB
    with ExitStack() as st:
        sb = lambda shape, dt, name=None: kb.sb(st, shape, dt, name)
        bg = Buf("g")
        g_bc = sb([128, 1024], F32, "g_ffn1")
        g_fin = sb([128, 1024], F32, "g_fin")
        S.dma("sp", g_bc[:], ffn_norm1.partition_broadcast(128), writes=[bg])
        S.dma("sp", g_fin[:], final_norm.partition_broadcast(128), writes=[bg])
        wr_sb = sb([128, 8, 8], BF16, "wr")
        for kc in range(8):
            S.dma("pool", wr_sb[:, kc, :], w_router[kc * 128:(kc + 1) * 128, :], writes=[bg])
        ntmp = kb.norm_tmp(st)
        xnT = sb([128, 8, TB], BF16, "xnT"); bxnT = Buf("xnT")
        acc = sb([128, TB // 128, 1024], F32, "acc"); bacc = Buf("acc")
        nsub = TB // 128
        gates = sb([128, nsub, 8], F32, "gates"); bgates = Buf("gates")
        lg = sb([128, 8], F32, "lg"); blg = Buf("lg")
        m1 = sb([128, 8], F32, "m1"); mk1 = sb([128, 8], F32, "mk1"); mk2 = sb([128, 8], F32, "mk2"); l2 = sb([128, 8], F32, "l2")
        ot = sb([128, 1024], F32, "ot"); bot = Buf("ot")
        bufs = swiglu_bufs(kb, st, TB)
        for blk in range(S_LEN // TB):
            for sub in range(nsub):
                r0 = blk * TB + sub * 128
                S.dma("sp", acc[:, sub, :], h3[r0:r0 + 128, :], writes=[bacc])
                kb.norm_T(ntmp, acc[:, sub, :], bacc, [(g_bc[:], bg, xnT[:, :, sub * 128:(sub + 1) * 128], bxnT)])
                for kc in range(8):
                    S.op("pe", lambda h, kc=kc, sub=sub: h.matmul(banks[1][:, 0:8], lhsT=xnT[:, kc, sub * 128:(sub + 1) * 128], rhs=wr_sb[:, kc, :],
                                                                  start=(kc == 0), stop=(kc == 7)), reads=[bxnT, bg], writes=[PB[1]], inc=(kc == 7))
                S.op("dve", lambda h: h.tensor_copy(out=lg[:], in_=banks[1][:, 0:8]), xr=[PB[1]], writes=[blg])
                S.op("dve", lambda h: h.tensor_reduce(out=m1[:, 0:1], in_=lg[:], axis=mybir.AxisListType.X, op=ALU.max), writes=[blg])
                S.op("dve", lambda h: h.tensor_scalar(out=mk1[:], in0=lg[:], scalar1=m1[:, 0:1], scalar2=None, op0=ALU.is_equal), writes=[blg])
                S.op("dve", lambda h: h.scalar_tensor_tensor(out=l2[:], in0=mk1[:], scalar=-1e30, in1=lg[:], op0=ALU.mult, op1=ALU.add), writes=[blg])
                S.op("dve", lambda h: h.tensor_reduce(out=m1[:, 1:2], in_=l2[:], axis=mybir.AxisListType.X, op=ALU.max), writes=[blg])
                S.op("dve", lambda h: h.tensor_scalar(out=mk2[:], in0=l2[:], scalar1=m1[:, 1:2], scalar2=None, op0=ALU.is_equal), writes=[blg])
                S.op("dve", lambda h: h.tensor_tensor(out=m1[:, 2:3], in0=m1[:, 0:1], in1=m1[:, 1:2], op=ALU.subtract), writes=[blg])
                S.op("act", lambda h: h.activation(out=m1[:, 3:4], in_=m1[:, 2:3], func=AF.Sigmoid), writes=[blg])
                S.op("act", lambda h: h.activation(out=m1[:, 4:5], in_=m1[:, 2:3], func=AF.Sigmoid, scale=-1.0), writes=[blg])
                S.op("dve", lambda h: h.tensor_scalar(out=mk1[:], in0=mk1[:], scalar1=m1[:, 3:4], scalar2=None, op0=ALU.mult), writes=[blg])
                S.op("dve", lambda h, sub=sub: h.scalar_tensor_tensor(out=gates[:, sub, :], in0=mk2[:], scalar=m1[:, 4:5], in1=mk1[:], op0=ALU.mult, op1=ALU.add),
                     reads=[blg], writes=[bgates])
            for e in range(nexp):
                swiglu_acc(kb, bufs, xnT, bxnT, TB, wg[e], wu[e], wd[e], EXPERT_DIM, acc, bacc,
                           lambda sub, e=e: gates[:, sub, e:e + 1], bgates)
            junk, bj, ss, bss, xn, bxn = ntmp
            for sub in range(nsub):
                r0 = blk * TB + sub * 128
                S.op("act", lambda h, sub=sub: h.activation(out=junk[:], in_=acc[:, sub, :], func=AF.Square, accum_out=ss[:, 0:1]),
                     reads=[bacc, bgates], writes=[bj, bss])
                S.op("act", lambda h: h.activation(out=ss[:, 1:2], in_=ss[:, 0:1], func=AF.Sqrt, scale=1.0 / D, bias=kb.epst[:, 0:1]),
                     reads=[kb.bconst], writes=[bss])
                S.op("dve", lambda h: h.reciprocal(out=ss[:, 2:3], in_=ss[:, 1:2]), writes=[bss])
                S.op("dve", lambda h, sub=sub: h.scalar_tensor_tensor(out=ot[:], in0=acc[:, sub, :], scalar=ss[:, 2:3], in1=g_fin[:], op0=ALU.mult, op1=ALU.mult),
                     reads=[bacc, bss, bg], writes=[bot])
                S.dma("sp", out[r0:r0 + 128, :], ot[:], reads=[bot])
        S.barrier()


I32 = mybir.dt.int32
MOE_G = 1024
MOE_NG = 15


def stage_moe_sparse(kb, h3, out, xs_d, y_d, ffn_norm1, final_norm, w_router, wg, wu, wd, ngroups=MOE_NG):
    S, banks, PB, nc = kb.S, kb.banks, kb.PB, kb.nc
    G = MOE_G
    wg2 = wg.rearrange("e k f -> (e k) f")
    wu2 = wu.rearrange("e k f -> (e k) f")
    wd2 = wd.rearrange("e f d -> (e f) d")
    IOA = bass.IndirectOffsetOnAxis
    with ExitStack() as st0:
        sb0 = lambda shape, dt, name=None: kb.sb(st0, shape, dt, name)
        bg = Buf("g")
        g_bc = sb0([128, 1024], F32, "g_ffn1")
        g_fin = sb0([128, 1024], F32, "g_fin")
        S.dma("sp", g_bc[:], ffn_norm1.partition_broadcast(128), writes=[bg])
        S.dma("sp", g_fin[:], final_norm.partition_broadcast(128), writes=[bg])
        w1_all = sb0([128, NT], F32, "w1_all"); w2_all = sb0([128, NT], F32, "w2_all"); bwa = Buf("w_all")
        slot_i = [sb0([128, NT], I32, "slot1_i"), sb0([128, NT], I32, "slot2_i")]; bslot = Buf("slot")
        widx1 = sb0([128, MOE_NG, 8], I32, "widx1"); widx2 = sb0([128, MOE_NG, 28], I32, "widx2"); bwidx = Buf("widx")
        bxs = Buf("xs_d"); by = Buf("y_d"); bxs0 = Buf("xs_zero")
        with ExitStack() as st:
            sb = lambda shape, dt, name=None: kb.sb(st, shape, dt, name)
            wr_sb = sb([128, 8, 8], BF16, "wr")
            for kc in range(8):
                S.dma("pool", wr_sb[:, kc, :], w_router[kc * 128:(kc + 1) * 128, :], writes=[bg])
            zt = sb([128, 1024], BF16, "zt"); bz = Buf("zt")
            S.op("pool", lambda h: h.memset(zt[:], 0.0), writes=[bz])
            for blk in range(MOE_NG * G // 128):
                S.dma("sp", xs_d[blk * 128:(blk + 1) * 128, :], zt[:], reads=[bz], writes=[bxs0])
            xn_parts = [sb([128, 8, 1024], BF16, "xn_all%d" % i) for i in range(4)]; bxn = Buf("xn_all")
            xn_row = lambda t: xn_parts[t // 8][:, t % 8, :]
            xts = [sb([128, 1024], F32, "xt") for _ in range(2)]; bxts = [Buf("x0"), Buf("x1")]
            junk = sb([128, 1024], BF16, "junk"); bj = Buf("junk")
            ss = sb([128, 4], F32, "ss"); bss = Buf("ss")
            xnT = sb([128, 8, 128], BF16, "xnT"); bxnT = Buf("xnT")
            sel = sb([128, NT, 8], F32, "sel"); mk1a = sb([128, NT, 8], F32, "mk1a"); mk2a = sb([128, NT, 8], F32, "mk2a"); bsel = Buf("sel")
            lg = sb([128, 8], F32, "lg"); blg = Buf("lg")
            m1 = sb([128, 8], F32, "m1"); l2 = sb([128, 8], F32, "l2")
            pT = banks[0][:].bitcast(BF16)
            for t in range(NT):
                xt = xts[t % 2]; bx = bxts[t % 2]
                S.dma("sp", xt[:], h3[t * 128:(t + 1) * 128, :], writes=[bx])
                S.op("act", lambda h, xt=xt: h.activation(out=junk[:], in_=xt[:], func=AF.Square, accum_out=ss[:, 0:1]),
                     reads=[bx], writes=[bj, bss])
                S.op("act", lambda h: h.activation(out=ss[:, 1:2], in_=ss[:, 0:1], func=AF.Sqrt, scale=1.0 / D, bias=kb.epst[:, 0:1]),
                     reads=[kb.bconst], writes=[bss])
                S.op("dve", lambda h: h.reciprocal(out=ss[:, 2:3], in_=ss[:, 1:2]), writes=[bss])
                S.op("dve", lambda h, xt=xt, t=t: h.scalar_tensor_tensor(out=xn_row(t), in0=xt[:], scalar=ss[:, 2:3], in1=g_bc[:],
                                                                         op0=ALU.mult, op1=ALU.mult), reads=[bx, bss, bg], writes=[bxn])
                for kc in range(8):
                    S.op("pe", lambda h, kc=kc, t=t: h.transpose(out=pT[:, kc * 128:(kc + 1) * 128], in_=xn_row(t)[:, kc * 128:(kc + 1) * 128],
                                                                 identity=kb.ident[:]), reads=[bxn, kb.bconst], writes=[PB[0]], inc=(kc == 7))
                S.op("act", lambda h: h.copy(out=xnT[:], in_=pT.rearrange("p (a b) -> p a b", a=8)), xr=[PB[0]], writes=[bxnT])
                for kc in range(8):
                    S.op("pe", lambda h, kc=kc: h.matmul(banks[1][:, 0:8], lhsT=xnT[:, kc, :], rhs=wr_sb[:, kc, :],
                                                         start=(kc == 0), stop=(kc == 7)), reads=[bxnT, bg], writes=[PB[1]], inc=(kc == 7))
                S.op("dve", lambda h: h.tensor_copy(out=lg[:], in_=banks[1][:, 0:8]), xr=[PB[1]], writes=[blg])
                S.op("dve", lambda h: h.tensor_reduce(out=m1[:, 0:1], in_=lg[:], axis=mybir.AxisListType.X, op=ALU.max), writes=[blg])
                S.op("dve", lambda h, t=t: h.tensor_scalar(out=mk1a[:, t, :], in0=lg[:], scalar1=m1[:, 0:1], scalar2=None, op0=ALU.is_equal),
                     reads=[blg], writes=[bsel])
                S.op("dve", lambda h, t=t: h.scalar_tensor_tensor(out=l2[:], in0=mk1a[:, t, :], scalar=-1e30, in1=lg[:], op0=ALU.mult, op1=ALU.add),
                     reads=[bsel], writes=[blg])
                S.op("dve", lambda h: h.tensor_reduce(out=m1[:, 1:2], in_=l2[:], axis=mybir.AxisListType.X, op=ALU.max), writes=[blg])
                S.op("dve", lambda h, t=t: h.tensor_scalar(out=mk2a[:, t, :], in0=l2[:], scalar1=m1[:, 1:2], scalar2=None, op0=ALU.is_equal),
                     reads=[blg], writes=[bsel])
                S.op("dve", lambda h: h.tensor_tensor(out=m1[:, 2:3], in0=m1[:, 0:1], in1=m1[:, 1:2], op=ALU.subtract), writes=[blg])
                S.op("act", lambda h, t=t: h.activation(out=w1_all[:, t:t + 1], in_=m1[:, 2:3], func=AF.Sigmoid), reads=[blg], writes=[bwa])
                S.op("act", lambda h, t=t: h.activation(out=w2_all[:, t:t + 1], in_=m1[:, 2:3], func=AF.Sigmoid, scale=-1.0), reads=[blg], writes=[bwa])
                S.op("dve", lambda h, t=t: h.tensor_tensor(out=sel[:, t, :], in0=mk1a[:, t, :], in1=mk2a[:, t, :], op=ALU.add), writes=[bsel])
            ustr = sb([128, 128], F32, "ustr"); onesm = sb([128, 128], F32, "onesm"); ones1 = sb([128, 1], F32, "ones1"); bu = Buf("u")
            S.op("pool", lambda h: h.memset(ustr[:], 1.0), writes=[bu])
            S.op("pool", lambda h: h.affine_select(out=ustr[:], in_=ustr[:], pattern=[[1, 128]], compare_op=ALU.is_gt, fill=0.0,
                                                   base=0, channel_multiplier=-1), writes=[bu])
            S.op("pool", lambda h: h.memset(onesm[:], 1.0), writes=[bu])
            S.op("pool", lambda h: h.memset(ones1[:], 1.0), writes=[bu])
            pid_i = sb([128, 1], I32, "pid_i"); pid = sb([128, 1], F32, "pid")
            S.op("pool", lambda h: h.iota(pid_i[:], pattern=[[0, 1]], base=0, channel_multiplier=1), writes=[bu])
            S.op("dve", lambda h: h.tensor_copy(out=pid[:], in_=pid_i[:]), writes=[bu])
            selv = sel[:].rearrange("p t e -> p (t e)")
            S.op("pe", lambda h: h.matmul(banks[2][:, 0:256], lhsT=ustr[:], rhs=selv, start=True, stop=True, skip_group_check=True), reads=[bsel, bu], writes=[PB[2]])
            S.op("pe", lambda h: h.matmul(banks[3][:, 0:256], lhsT=onesm[:], rhs=selv, start=True, stop=True, skip_group_check=True), reads=[bsel, bu], writes=[PB[3]])
            tot = sb([128, NT, 8], F32, "tot"); incl = sb([128, NT, 8], F32, "incl"); base = sb([128, NT, 8], F32, "base"); bt = Buf("tot")
            S.op("dve", lambda h: h.tensor_copy(out=tot[:].rearrange("p t e -> p (t e)"), in_=banks[3][:, 0:256]), xr=[PB[3]], writes=[bt])
            for e in range(8):
                S.op("dve", lambda h, e=e: h.tensor_tensor_scan(out=incl[:, :, e], data0=ones1[:, 0:1].to_broadcast([128, NT]), data1=tot[:, :, e],
                                                                initial=0.0, op0=ALU.mult, op1=ALU.add), reads=[bu], writes=[bt])
            S.op("dve", lambda h: h.tensor_tensor(out=base[:], in0=incl[:], in1=tot[:], op=ALU.subtract), writes=[bt])
            ne = incl[:, NT - 1, :]
            ng = sb([128, 8], F32, "ng"); tmp8 = sb([128, 8], F32, "tmp8"); gi = sb([128, 8], F32, "gi"); off = sb([128, 8], F32, "off")
            S.op("dve", lambda h: h.tensor_scalar(out=ng[:], in0=ne, scalar1=0.5, scalar2=None, op0=ALU.is_gt), writes=[bt])
            for k in range(1, 4):
                S.op("dve", lambda h, k=k: h.tensor_scalar(out=tmp8[:], in0=ne, scalar1=k * G + 0.5, scalar2=None, op0=ALU.is_gt), writes=[bt])
                S.op("dve", lambda h: h.tensor_tensor(out=ng[:], in0=ng[:], in1=tmp8[:], op=ALU.add), writes=[bt])
            S.op("dve", lambda h: h.tensor_tensor_scan(out=gi[:], data0=ones1[:, 0:1].to_broadcast([128, 8]), data1=ng[:], initial=0.0,
                                                       op0=ALU.mult, op1=ALU.add), reads=[bu], writes=[bt])
            S.op("dve", lambda h: h.tensor_tensor(out=off[:], in0=gi[:], in1=ng[:], op=ALU.subtract), writes=[bt])
            S.op("dve", lambda h: h.tensor_scalar(out=off[:], in0=off[:], scalar1=float(G), scalar2=None, op0=ALU.mult), writes=[bt])
            for e in range(8):
                S.op("dve", lambda h, e=e: h.tensor_scalar(out=base[:, :, e], in0=base[:, :, e], scalar1=off[:, e:e + 1], scalar2=None, op0=ALU.add), writes=[bt])
            slotf = sb([128, NT, 8], F32, "slotf"); prod = sb([128, NT, 8], F32, "prod"); s12 = sb([128, 2, NT], F32, "s12")
            S.op("dve", lambda h: h.tensor_tensor(out=slotf[:].rearrange("p t e -> p (t e)"), in0=banks[2][:, 0:256],
                                                  in1=base[:].rearrange("p t e -> p (t e)"), op=ALU.add), xr=[PB[2]], writes=[bt])
            for k, mk in enumerate((mk1a, mk2a)):
                S.op("dve", lambda h, mk=mk: h.tensor_tensor(out=prod[:], in0=mk[:], in1=slotf[:], op=ALU.mult), reads=[bsel], writes=[bt])
                S.op("dve", lambda h, k=k: h.tensor_reduce(out=s12[:, k, :], in_=prod[:], axis=mybir.AxisListType.X, op=ALU.add), writes=[bt])
                S.op("dve", lambda h, k=k: h.tensor_copy(out=slot_i[k][:], in_=s12[:, k, :]), reads=[bt], writes=[bslot])
            ge = sb([128, MOE_NG], F32, "ge"); rb1 = sb([128, MOE_NG], F32, "rb1"); rb2 = sb([128, MOE_NG], F32, "rb2")
            for j in range(MOE_NG):
                S.op("dve", lambda h, j=j: h.tensor_scalar(out=tmp8[:], in0=gi[:], scalar1=j + 0.5, scalar2=None, op0=ALU.is_lt, op1=ALU.add,
                                                           accum_out=ge[:, j:j + 1]), writes=[bt])
            S.op("dve", lambda h: h.tensor_scalar(out=ge[:], in0=ge[:], scalar1=7.0, scalar2=None, op0=ALU.min), writes=[bt])
            pid7 = sb([128, 1], F32, "pid7")
            S.op("dve", lambda h: h.tensor_scalar(out=pid7[:], in0=pid[:], scalar1=7.0, scalar2=None, op0=ALU.mult), writes=[bu])
            S.op("dve", lambda h: h.tensor_scalar(out=rb1[:], in0=ge[:], scalar1=7168.0, scalar2=pid7[:, 0:1], op0=ALU.mult, op1=ALU.add), reads=[bu], writes=[bt])
            S.op("dve", lambda h: h.tensor_scalar(out=rb2[:], in0=ge[:], scalar1=3584.0, scalar2=pid[:, 0:1], op0=ALU.mult, op1=ALU.add), reads=[bu], writes=[bt])
            for kc in range(8):
                S.op("dve", lambda h, kc=kc: h.tensor_scalar(out=widx1[:, :, kc], in0=rb1[:], scalar1=float(kc * 896), scalar2=None, op0=ALU.add),
                     reads=[bt], writes=[bwidx])
            for fc in range(28):
                S.op("dve", lambda h, fc=fc: h.tensor_scalar(out=widx2[:, :, fc], in0=rb2[:], scalar1=float(fc * 128), scalar2=None, op0=ALU.add),
                     reads=[bt], writes=[bwidx])
            if getattr(kb, "dbg", None) is not None:
                S.dma("sp", kb.dbg["slot1"], slot_i[0][:], reads=[bslot])
                S.dma("sp", kb.dbg["slot2"], slot_i[1][:], reads=[bslot])
                S.dma("sp", kb.dbg["w1"], w1_all[:], reads=[bwa])
                S.dma("sp", kb.dbg["ge"], ge[:], reads=[bt])
                S.dma("sp", kb.dbg["xn10"], xn_row(10), reads=[bxn])
                S.dma("sp", kb.dbg["xn11"], xn_row(11), reads=[bxn])
            for t in range(NT):
                for k in range(2):
                    S.dma("pool", None, None, reads=[bslot, bxn, bxs0], writes=[bxs],
                          fn=lambda h, t=t, k=k: h.indirect_dma_start(out=xs_d[:, :], out_offset=IOA(ap=slot_i[k][:, t:t + 1], axis=0),
                                                                      in_=xn_row(t), in_offset=None))
            S.barrier()
        with ExitStack() as st:
            sb = lambda shape, dt, name=None: kb.sb(st, shape, dt, name)
            xg = sb([128, 8, 1024], BF16, "xg"); bxg = Buf("xg")
            xgT = sb([128, 8, G], BF16, "xgT"); bxgT = Buf("xgT")
            acc = sb([128, 8, 1024], F32, "acc"); bacc = Buf("acc")
            bufs = swiglu_bufs(kb, st, G)
            pT = banks[0][:].bitcast(BF16)
            def gload(j):
                S.dma("sp", xg[:], xs_d[j * G:(j + 1) * G, :].rearrange("(s p) d -> p s d", p=128), reads=[bxs], writes=[bxg])

            def gprep(j):
                for sub in range(8):
                    for kc in range(8):
                        S.op("pe", lambda h, kc=kc, sub=sub: h.transpose(out=pT[:, kc * 128:(kc + 1) * 128], in_=xg[:, sub, kc * 128:(kc + 1) * 128],
                                                                         identity=kb.ident[:]), reads=[bxg, kb.bconst], writes=[PB[0]], inc=(kc == 7))
                    S.op("act", lambda h, sub=sub: h.copy(out=xgT[:, :, sub * 128:(sub + 1) * 128], in_=pT.rearrange("p (a b) -> p a b", a=8)),
                         xr=[PB[0]], writes=[bxgT])
                if j + 1 < ngroups:
                    gload(j + 1)

            gload(0)
            gprep(0)
            for j in range(ngroups):
                hook = (lambda j=j: gprep(j + 1)) if j + 1 < ngroups else None
                def wload(slot, f0, nf, bw, wg_sb, wu_sb, wd_sb, j=j):
                    for kc in range(8):
                        for (wsb, w2) in ((wg_sb, wg2), (wu_sb, wu2)):
                            S.dma("pool", None, None, reads=[bwidx], writes=[bw],
                                  fn=lambda h, wsb=wsb, w2=w2, kc=kc: h.indirect_dma_start(
                                      out=wsb[slot][:, kc, 0:nf * 128], out_offset=None, in_=w2[:, 0:nf * 128],
                                      in_offset=IOA(ap=widx1[:, j, kc:kc + 1], axis=0), element_offset=f0 * 128))
                    for fc in range(nf):
                        S.dma("pool", None, None, reads=[bwidx], writes=[bw],
                              fn=lambda h, fc=fc: h.indirect_dma_start(
                                  out=wd_sb[slot][:, fc, :], out_offset=None, in_=wd2[:, :],
                                  in_offset=IOA(ap=widx2[:, j, f0 + fc:f0 + fc + 1], axis=0)))

                swiglu_acc(kb, bufs, xgT, bxgT, G, None, None, None, EXPERT_DIM, acc, bacc, lambda sub: None, wload=wload, acc_init=True, mid_hook=hook)
                S.dma("sp", y_d[j * G:(j + 1) * G, :].rearrange("(s p) d -> p s d", p=128), acc[:], reads=[bacc], writes=[by])
            S.barrier()
        with ExitStack() as st:
            sb = lambda shape, dt, name=None: kb.sb(st, shape, dt, name)
            xts_5 = [sb([128, 1024], F32, "xt") for _ in range(2)]; bxts_5 = [Buf("x0"), Buf("x1")]
            y1_5 = [sb([128, 1024], F32, "y1_5") for _ in range(2)]; y2_5 = [sb([128, 1024], F32, "y2_5") for _ in range(2)]
            by1_5 = [Buf("y1a"), Buf("y1b")]; by2_5 = [Buf("y2a"), Buf("y2b")]
            junk_5 = sb([128, 1024], BF16, "junk_5"); bj_5 = Buf("junk_5")
            ss_5 = sb([128, 4], F32, "ss_5"); bss_5 = Buf("ss_5")
            ot_5 = [sb([128, 1024], F32, "ot_5") for _ in range(2)]; bot_5 = [Buf("ot0"), Buf("ot1")]
            for t in range(NT):
                r = t % 2
                xt = xts_5[r]; bx = bxts_5[r]
                S.dma("sp", xt[:], h3[t * 128:(t + 1) * 128, :], writes=[bx])
                for (yy, byy, k) in ((y1_5[r], by1_5[r], 0), (y2_5[r], by2_5[r], 1)):
                    S.dma("pool", None, None, reads=[bslot, by], writes=[byy],
                          fn=lambda h, yy=yy, k=k, t=t: h.indirect_dma_start(out=yy[:, :], out_offset=None, in_=y_d[:, :],
                                                                            in_offset=IOA(ap=slot_i[k][:, t:t + 1], axis=0)))
                S.op("dve", lambda h, xt=xt, r=r, t=t: h.scalar_tensor_tensor(out=xt[:], in0=y1_5[r][:], scalar=w1_all[:, t:t + 1], in1=xt[:],
                                                                            op0=ALU.mult, op1=ALU.add), reads=[by1_5[r], bwa], writes=[bx])
                S.op("dve", lambda h, xt=xt, r=r, t=t: h.scalar_tensor_tensor(out=xt[:], in0=y2_5[r][:], scalar=w2_all[:, t:t + 1], in1=xt[:],
                                                                            op0=ALU.mult, op1=ALU.add), reads=[by2_5[r], bwa], writes=[bx])
                S.op("act", lambda h, xt=xt: h.activation(out=junk_5[:], in_=xt[:], func=AF.Square, accum_out=ss_5[:, 0:1]), reads=[bx], writes=[bj_5, bss_5])
                S.op("act", lambda h: h.activation(out=ss_5[:, 1:2], in_=ss_5[:, 0:1], func=AF.Sqrt, scale=1.0 / D, bias=kb.epst[:, 0:1]),
                     reads=[kb.bconst], writes=[bss_5])
                S.op("dve", lambda h: h.reciprocal(out=ss_5[:, 2:3], in_=ss_5[:, 1:2]), writes=[bss_5])
                S.op("dve", lambda h, xt=xt, r=r: h.scalar_tensor_tensor(out=ot_5[r][:], in0=xt[:], scalar=ss_5[:, 2:3], in1=g_fin[:], op0=ALU.mult, op1=ALU.mult),
                     reads=[bx, bss_5, bg], writes=[bot_5[r]])
                S.dma("sp", out[t * 128:(t + 1) * 128, :], ot_5[r][:], reads=[bot_5[r]])
            S.barrier()

IN_SHAPES = {
    "x": [S_LEN, D], "attn_norm": [2, D], "ffn_norm": [2, D], "kv_norm": [D], "final_norm": [D],
    "gla_w_in": [D, GLA_IN], "gla_w_gate_up": [16, 512], "gla_b_gate": [1, 512], "gla_head_norm": [256],
    "gla_w_out": [D, D], "sb_w_kv": [D, 2048], "sb_w_q": [D, D], "sb_w_out": [D, D],
    "ffn_w_gate": [D, FFN_DIM], "ffn_w_up": [D, FFN_DIM], "ffn_w_down": [FFN_DIM, D],
    "moe_w_router": [D, 8], "moe_w_gate": [8, D, EXPERT_DIM], "moe_w_up": [8, D, EXPERT_DIM], "moe_w_down": [8, EXPERT_DIM, D],
}

ALL_STAGES = ("gla", "ffn", "qkv", "sb", "ao", "moe")
SPARSE_MOE = True


def build(stages=ALL_STAGES, ext=()):
    nc = bass.Bass("TRN2", target_bir_lowering=False)
    I = {k: nc.dram_tensor(k, shp, F32, kind="ExternalInput").ap() for k, shp in IN_SHAPES.items()}

    def scratch(name, shape, dt):
        if name in ext and name != "dbg":
            first = ALL_STAGES.index(stages[0])
            prod = {"h1": 0, "h2": 1, "qT": 2, "kT": 2, "v": 2, "oT": 3, "h3": 4}[name]
            kind = "ExternalInput" if prod < first else "ExternalOutput"
            return nc.dram_tensor(name, shape, dt, kind=kind).ap()
        return nc.dram_tensor(name, shape, dt).ap()

    h1 = scratch("h1", [S_LEN, D], F32)
    h2 = scratch("h2", [S_LEN, D], F32)
    qT_d = scratch("qT", [8, 128, S_LEN], BF16)
    kT_d = scratch("kT", [8, 128, S_LEN], BF16)
    v_d = scratch("v", [S_LEN, D], BF16)
    oT_d = scratch("oT", [8, 128, S_LEN], BF16)
    h3 = scratch("h3", [S_LEN, D], F32)
    out = nc.dram_tensor("out", [S_LEN, D], F32, kind="ExternalOutput").ap()
    with ExitStack() as es:
        kb = KB(nc, es)
        if "dbg" in ext:
            kb.dbg = {"slot1": nc.dram_tensor("dbg_slot1", [128, NT], mybir.dt.int32, kind="ExternalOutput").ap(),
                      "slot2": nc.dram_tensor("dbg_slot2", [128, NT], mybir.dt.int32, kind="ExternalOutput").ap(),
                      "w1": nc.dram_tensor("dbg_w1", [128, NT], F32, kind="ExternalOutput").ap(),
                      "ge": nc.dram_tensor("dbg_ge", [128, MOE_NG], F32, kind="ExternalOutput").ap(),
                      "xn10": nc.dram_tensor("dbg_xn10", [128, 1024], BF16, kind="ExternalOutput").ap(),
                      "xn11": nc.dram_tensor("dbg_xn11", [128, 1024], BF16, kind="ExternalOutput").ap()}
        kb.consts()
        if "gla" in stages:
            stage_gla(kb, I["x"], h1, I["attn_norm"][0], I["gla_w_in"], I["gla_w_gate_up"], I["gla_b_gate"],
                      I["gla_head_norm"], I["gla_w_out"])
        if "ffn" in stages:
            stage_ffn(kb, h1, h2, I["ffn_norm"][0], I["ffn_w_gate"], I["ffn_w_up"], I["ffn_w_down"])
        if "qkv" in stages:
            stage_qkv(kb, h2, qT_d, kT_d, v_d, I["kv_norm"], I["attn_norm"][1], I["sb_w_kv"], I["sb_w_q"])
        if "sb" in stages:
            stage_sb(kb, qT_d, kT_d, v_d, oT_d)
        if "ao" in stages:
            stage_attn_out(kb, oT_d, h2, h3, I["sb_w_out"])
        if "moe" in stages and SPARSE_MOE:
            dk = {"kind": "ExternalOutput"} if "dbg" in ext else {}
            xs_d = nc.dram_tensor("xs_d", [MOE_NG * MOE_G, D], BF16, **dk).ap()
            y_d = nc.dram_tensor("y_d", [MOE_NG * MOE_G, D], F32, **dk).ap()
            stage_moe_sparse(kb, h3, out, xs_d, y_d, I["ffn_norm"][1], I["final_norm"], I["moe_w_router"], I["moe_w_gate"], I["moe_w_up"], I["moe_w_down"])
        elif "moe" in stages:
            stage_moe(kb, h3, out, I["ffn_norm"][1], I["final_norm"], I["moe_w_router"], I["moe_w_gate"], I["moe_w_up"], I["moe_w_down"])
        kb.S.emit()
    return nc


def host_inputs(inputs):
    f = lambda a: np.ascontiguousarray(np.asarray(a, dtype=np.float32))
    shared = {
        "attn_norm": f(inputs["attn_norm"]), "ffn_norm": f(inputs["ffn_norm"]), "kv_norm": f(inputs["kv_norm"]),
        "final_norm": f(inputs["final_norm"]), "gla_w_in": f(inputs["gla_w_in"][0]), "gla_w_gate_up": f(inputs["gla_w_gate_up"][0]),
        "gla_b_gate": f(inputs["gla_b_gate"]).reshape(1, 512), "gla_head_norm": f(inputs["gla_head_norm"][0]),
        "gla_w_out": f(inputs["gla_w_out"][0]), "sb_w_kv": f(inputs["sb_w_kv"]), "sb_w_q": f(inputs["sb_w_q"][0]),
        "sb_w_out": f(inputs["sb_w_out"][0]), "ffn_w_gate": f(inputs["ffn_w_gate"][0]), "ffn_w_up": f(inputs["ffn_w_up"][0]),
        "ffn_w_down": f(inputs["ffn_w_down"][0]), "moe_w_router": f(inputs["moe_w_router"][0]),
        "moe_w_gate": f(inputs["moe_w_gate"][0]), "moe_w_up": f(inputs["moe_w_up"][0]), "moe_w_down": f(inputs["moe_w_down"][0]),
    }
    x = f(inputs["x"])
    maps = []
    for b in range(x.shape[0]):
        m = dict(shared)
        m["x"] = x[b]
        maps.append(m)
    return maps


_NC_CACHE = {}


def kernel(**inputs):
    maps = host_inputs(inputs)
    if "full" not in _NC_CACHE:
        _NC_CACHE["full"] = build()
    nc = _NC_CACHE["full"]
    res = run_bass_kernel_spmd(nc, maps, core_ids=list(range(len(maps))))
    return np.stack([np.asarray(r["out"], dtype=np.float32) for r in res.results], axis=0)
```

```python
import math
import numpy as np
from contextlib import ExitStack
import concourse.bass as bass
import concourse.mybir as mybir
from concourse.bass_utils import run_bass_kernel_spmd

F32 = mybir.dt.float32
BF16 = mybir.dt.bfloat16
AF = mybir.ActivationFunctionType
ALU = mybir.AluOpType

S_LEN = 4096
D = 1024
NT = S_LEN // 128
EPS = 1e-6
GLA_IN = 3088
FFN_DIM = 2816
EXPERT_DIM = 3584
NEXP = 8


class Buf:
    __slots__ = ("w", "r", "name")

    def __init__(self, name=""):
        self.w = []
        self.r = {}
        self.name = name


class _Eng:
    def __init__(self, name, h, sem, sid):
        self.name, self.h, self.sem, self.sid = name, h, sem, sid
        self.cnt = 0
        self.ops = []
        self.seen = {}


class Sched:
    NDMA = 12

    def __init__(self, nc, es):
        self.nc = nc
        self.sems = []
        self.eng = {}
        for name, h in [("pe", nc.tensor), ("act", nc.scalar), ("dve", nc.vector),
                        ("pool", nc.gpsimd), ("sp", nc.sync)]:
            sem = es.enter_context(nc.semaphore("s_" + name))
            self.sems.append(sem)
            self.eng[name] = _Eng(name, h, sem, len(self.sems) - 1)
        self.dq = {}
        for q in ("sp", "act", "pool"):
            lst = []
            for i in range(self.NDMA):
                sem = es.enter_context(nc.semaphore("d_%s%d" % (q, i)))
                self.sems.append(sem)
                lst.append([len(self.sems) - 1, 0])
            self.dq[q] = [lst, 0]

    def _wait(self, e, tok):
        if tok is None:
            return
        sid, v = tok
        if e.name == "pe" and sid == e.sid:
            return
        if e.seen.get(sid, 0) >= v:
            return
        e.seen[sid] = v
        sem = self.sems[sid]
        e.ops.append(lambda h, sem=sem, v=v: h.wait_ge(sem, v))

    def _deps(self, e, reads, writes, xr=(), dma_fill=False):
        for b in reads:
            for tk in b.w:
                self._wait(e, tk)
        for b in xr:
            for tk in b.w:
                self._wait(e, tk)
            for sid, v in list(b.r.items()):
                self._wait(e, (sid, v))
        for b in writes:
            if dma_fill and not b.r and b.w and all(sid >= 5 for sid, _ in b.w):
                continue
            for tk in b.w:
                self._wait(e, tk)
            for sid, v in list(b.r.items()):
                self._wait(e, (sid, v))

    def op(self, eng, fn, reads=(), writes=(), inc=True, xr=()):
        e = self.eng[eng]
        self._deps(e, reads, writes, xr)
        reads = list(reads) + list(xr)
        if inc:
            e.cnt += 1
            v = e.cnt
            sem = e.sem
            e.ops.append(lambda h, fn=fn, sem=sem: fn(h).then_inc(sem, 1))
        else:
            v = e.cnt + 1
            e.ops.append(lambda h, fn=fn: fn(h))
        tok = (e.sid, v)
        for b in writes:
            b.w = [tok]
            b.r = {}
        for b in reads:
            b.r[e.sid] = v

    def dma(self, q, out, in_, reads=(), writes=(), fn=None, **kw):
        e = self.eng[q]
        fill = {id(b): (not b.r and bool(b.w) and all(sid >= 5 for sid, _ in b.w)) for b in writes}
        self._deps(e, reads, writes, dma_fill=True)
        lst, idx = self.dq[q]
        self.dq[q][1] = (idx + 1) % len(lst)
        ent = lst[idx]
        sid = ent[0]
        if ent[1] > 0:
            self._wait(e, (sid, ent[1]))
        ent[1] += 16
        v = ent[1]
        sem = self.sems[sid]
        if fn is None:
            e.ops.append(lambda h, out=out, in_=in_, sem=sem, kw=kw:
                         h.dma_start(out=out, in_=in_, **kw).then_inc(sem, 16))
        else:
            e.ops.append(lambda h, fn=fn, sem=sem: fn(h).then_inc(sem, 16))
        tok = (sid, v)
        for b in writes:
            if fill[id(b)]:
                b.w = [t for t in b.w if t[0] != sid] + [tok]
            else:
                b.w = [tok]
            b.r = {}
        for b in reads:
            b.r[sid] = v

    def barrier(self):
        toks = []
        for e in self.eng.values():
            if e.cnt > 0:
                toks.append((e.sid, e.cnt))
        for q, (lst, _) in self.dq.items():
            for sid, v in lst:
                if v > 0:
                    toks.append((sid, v))
        for e in self.eng.values():
            for t in toks:
                self._wait(e, t)

    def emit(self):
        nc = self.nc
        self.barrier()
        with nc.Block() as block:
            @block.sync
            def _(h):
                for f in self.eng["sp"].ops:
                    f(h)

            @block.tensor
            def _(h):
                for f in self.eng["pe"].ops:
                    f(h)

            @block.scalar
            def _(h):
                for f in self.eng["act"].ops:
                    f(h)

            @block.vector
            def _(h):
                for f in self.eng["dve"].ops:
                    f(h)

            @block.gpsimd
            def _(h):
                for f in self.eng["pool"].ops:
                    f(h)


class KB:
    def __init__(self, nc, es):
        self.nc = nc
        self.es = es
        self.S = Sched(nc, es)
        self.banks = [es.enter_context(nc.psum_tensor("pb%d" % i, [128, 512], F32)) for i in range(8)]
        self.PB = [Buf("pb%d" % i) for i in range(8)]
        self.n = 0

    def sb(self, st, shape, dt, name=None):
        self.n += 1
        return st.enter_context(self.nc.sbuf_tensor("%s_%d" % (name or "t", self.n), shape, dt))

    def consts(self):
        S, es = self.S, self.es
        self.identf = self.sb(es, [128, 128], F32, "identf")
        self.ident = self.sb(es, [128, 128], BF16, "ident")
        self.trif = self.sb(es, [128, 128], F32, "trif")
        self.epst = self.sb(es, [128, 1], F32, "eps")
        self.bconst = Buf("const")
        bc = self.bconst
        identf, ident, trif, epst = self.identf, self.ident, self.trif, self.epst
        S.op("pool", lambda h: h.memset(identf[:], 0.0), writes=[bc])
        S.op("pool", lambda h: h.affine_select(out=identf[:], in_=identf[:], pattern=[[-1, 128]],
                                               compare_op=ALU.not_equal, fill=1.0, base=0, channel_multiplier=1),
             writes=[bc])
        S.op("dve", lambda h: h.tensor_copy(out=ident[:], in_=identf[:]), writes=[bc])
        S.op("pool", lambda h: h.memset(trif[:], 1.0), writes=[bc])
        S.op("pool", lambda h: h.affine_select(out=trif[:], in_=trif[:], pattern=[[1, 128]],
                                               compare_op=ALU.is_ge, fill=0.0, base=0, channel_multiplier=-1),
             writes=[bc])
        S.op("pool", lambda h: h.memset(trif[0:64, 64:128], 0.0), writes=[bc])
        S.op("dve", lambda h: h.memset(epst[:], EPS), writes=[bc])

    def norm_T(self, st_tmp, xt, bx, outs):
        S = self.S
        junk, bj, ss, bss, xn, bxn = st_tmp
        S.op("act", lambda h: h.activation(out=junk[:], in_=xt, func=AF.Square, accum_out=ss[:, 0:1]),
             reads=[bx], writes=[bj, bss])
        S.op("act", lambda h: h.activation(out=ss[:, 1:2], in_=ss[:, 0:1], func=AF.Sqrt, scale=1.0 / D,
                                           bias=self.epst[:, 0:1]), reads=[self.bconst], writes=[bss])
        S.op("dve", lambda h: h.reciprocal(out=ss[:, 2:3], in_=ss[:, 1:2]), writes=[bss])
        pT = self.banks[0][:].bitcast(BF16)
        for (g, bgain, dst, bdst) in outs:
            S.op("dve", lambda h, g=g: h.scalar_tensor_tensor(out=xn[:], in0=xt, scalar=ss[:, 2:3], in1=g,
                                                            op0=ALU.mult, op1=ALU.mult),
                 reads=[bx, bss, bgain, self.bconst], writes=[bxn])
            for kc in range(8):
                S.op("pe", lambda h, kc=kc: h.transpose(out=pT[:, kc * 128:(kc + 1) * 128],
                                                        in_=xn[:, kc * 128:(kc + 1) * 128], identity=self.ident[:]),
                     reads=[bxn, self.bconst], writes=[self.PB[0]], inc=(kc == 7))
            S.op("act", lambda h, dst=dst: h.copy(out=dst, in_=pT.rearrange("p (a b) -> p a b", a=8)),
                 xr=[self.PB[0]], writes=[bdst])

    def norm_tmp(self, st):
        return (self.sb(st, [128, 1024], BF16, "junk"), Buf("junk"), self.sb(st, [128, 4], F32, "ss"), Buf("ss"),
                self.sb(st, [128, 1024], BF16, "xn"), Buf("xn"))

    def load_w_bf16(self, dst, src, bdst, kcs, ncols, c0=0):
        for kc in range(kcs):
            self.S.dma("pool", dst[:, kc, :], src[kc * 128:(kc + 1) * 128, c0:c0 + ncols], writes=[bdst])


def stage_gla(kb, x, h1, attn_norm0, w_in, w_gu, b_gate, g_head, w_out, ntiles=NT):
    nc, S, banks, PB = kb.nc, kb.S, kb.banks, kb.PB
    with ExitStack() as st:
        sb = lambda shape, dt, name=None: kb.sb(st, shape, dt, name)
        w_in_sb = sb([128, 8, GLA_IN], BF16, "w_in")
        w_out_sb = sb([128, 8, 1024], BF16, "w_out")
        bw = Buf("w")
        kb.load_w_bf16(w_in_sb, w_in, bw, 8, GLA_IN)
        kb.load_w_bf16(w_out_sb, w_out, bw, 8, 1024)
        wgu = sb([17, 512], F32, "wgu")
        S.dma("sp", wgu[0:16, :], w_gu, writes=[bw])
        S.dma("sp", wgu[16:17, :], b_gate, writes=[bw])
        g_attn = sb([128, 1024], F32, "g_attn")
        S.dma("sp", g_attn[:], attn_norm0.partition_broadcast(128), writes=[bw])
        g_hd = sb([128, 256], F32, "g_hd")
        S.dma("sp", g_hd[:], g_head.partition_broadcast(128), writes=[bw])
        ntmp = kb.norm_tmp(st)
        xts = [sb([128, 1024], F32, "xt") for _ in range(2)]
        bxts = [Buf("xt") for _ in range(2)]
        xnT = sb([128, 8, 128], BF16, "xnT"); bxnT = Buf("xnT")
        v_bf = sb([128, 1024], BF16, "v"); bv = Buf("v")
        sr = sb([128, 1024], F32, "sr"); bsr = Buf("sr")
        glrT = sb([17, 128], F32, "glrT"); bglr = Buf("glrT")
        S.op("dve", lambda h: h.memset(glrT[:], 1.0), writes=[bglr])
        e1 = sb([128, 512], F32, "e1"); be1 = Buf("e1")
        lap = sb([128, 512], F32, "lap"); blap = Buf("lap")
        ek_tm = sb([128, 512], F32, "ek_tm"); bek = Buf("ek_tm")
        kinv_tm = sb([128, 512], BF16, "kinv_tm"); bkinv = Buf("kinv_tm")
        eq = sb([128, 512], F32, "eq"); beq = Buf("eq")
        ekT = sb([128, 512], F32, "ekT"); bekT = Buf("ekT")
        dec = sb([128, 4, 2], F32, "dec"); bdec = Buf("dec")
        q_even = sb([128, 4, 128], BF16, "q_even"); q_odd = sb([128, 4, 128], BF16, "q_odd"); bq = Buf("q")
        S.op("dve", lambda h: h.memset(q_even[:], 0.0), writes=[bq])
        S.op("dve", lambda h: h.memset(q_odd[:], 0.0), writes=[bq])
        kinvT = sb([128, 4, 128], BF16, "kinvT"); bkT = Buf("kinvT")
        attnT = sb([128, 4, 128], BF16, "attnT"); battn = Buf("attnT")
        Sf = sb([128, 4, 256], F32, "Sf"); bSfh = [Buf("Sf%d" % i) for i in range(4)]
        Sb = sb([128, 4, 256], BF16, "Sb"); bSbh = [Buf("Sb%d" % i) for i in range(4)]
        Sm = sb([128, 4, 256], BF16, "Sm"); bSmh = [Buf("Sm%d" % i) for i in range(4)]
        t1 = sb([128, 4, 256], F32, "t1"); bt1h = [Buf("t1%d" % i) for i in range(4)]
        S.op("dve", lambda h: h.memset(Sf[:], 0.0), writes=bSfh)
        S.op("dve", lambda h: h.memset(Sb[:], 0.0), writes=bSbh)
        hs4 = sb([128, 4, 4], F32, "hs4"); bhs = Buf("hs")
        og4 = sb([128, 4, 256], F32, "og4"); bog = Buf("og")
        og_bf = sb([128, 1024], BF16, "og_bf"); bogb = Buf("og_bf")
        ogT = sb([128, 8, 128], BF16, "ogT"); bogT = Buf("ogT")
        h1t = sb([128, 1024], F32, "h1t"); bh1 = Buf("h1t")
        junk2 = sb([128, 256], BF16, "junk2"); bj2 = Buf("junk2")
        pTb = banks[0][:].bitcast(BF16)
        qscale = math.log(128.0 ** -0.5)
        lnq = sb([128, 1], F32, "lnq")
        S.op("dve", lambda h: h.memset(lnq[:], qscale), writes=[kb.bconst])

        for t in range(ntiles):
            xt = xts[t % 2]; bx = bxts[t % 2]
            S.dma("sp", xt[:], x[t * 128:(t + 1) * 128, :], writes=[bx])
            kb.norm_T(ntmp, xt[:], bx, [(g_attn[:], bw, xnT[:], bxnT)])

            def proj_tm(bank, c0, n=512):
                for kc in range(8):
                    S.op("pe", lambda h, kc=kc: h.matmul(banks[bank][:, 0:n], lhsT=xnT[:, kc, :], rhs=w_in_sb[:, kc, c0:c0 + n],
                                                         start=(kc == 0), stop=(kc == 7)),
                         reads=[bxnT, bw], writes=[PB[bank]], inc=(kc == 7))

            def proj_fm(bank, c0, m, col0):
                for kc in range(8):
                    S.op("pe", lambda h, kc=kc: h.matmul(banks[bank][0:m, col0:col0 + 128], lhsT=w_in_sb[:, kc, c0:c0 + m],
                                                         rhs=xnT[:, kc, :], start=(kc == 0), stop=(kc == 7)),
                         reads=[bxnT, bw], writes=[PB[bank]], inc=(kc == 7))

            proj_tm(1, 512)
            proj_tm(2, 1024); proj_tm(3, 1536)
            S.op("act", lambda h: h.copy(out=v_bf[:, 0:512], in_=banks[2][:]), xr=[PB[2]], writes=[bv])
            S.op("act", lambda h: h.copy(out=v_bf[:, 512:1024], in_=banks[3][:]), xr=[PB[3]], writes=[bv])
            proj_tm(2, 2064); proj_tm(3, 2576)
            S.op("act", lambda h: h.activation(out=sr[:, 0:512], in_=banks[2][:], func=AF.Silu), xr=[PB[2]], writes=[bsr])
            S.op("act", lambda h: h.activation(out=sr[:, 512:1024], in_=banks[3][:], func=AF.Silu), xr=[PB[3]], writes=[bsr])
            proj_fm(6, 2048, 16, 0)
            S.op("dve", lambda h: h.tensor_copy(out=glrT[0:16, :], in_=banks[6][0:16, 0:128]), xr=[PB[6]], writes=[bglr])
            for hh in range(4):
                proj_fm(4, hh * 128, 128, hh * 128)
            for hh in range(4):
                proj_fm(5, 512 + hh * 128, 128, hh * 128)
            S.op("pe", lambda h: h.matmul(banks[6][:], lhsT=glrT[:, :], rhs=wgu[:, :], start=True, stop=True, skip_group_check=True),
                 reads=[bglr, bw], writes=[PB[6]])
            S.op("act", lambda h: h.activation(out=e1[:], in_=banks[6][:], func=AF.Exp, scale=-1.0), xr=[PB[6]], writes=[be1])
            S.op("act", lambda h: h.activation(out=lap[:], in_=e1[:], func=AF.Ln, bias=1.0), reads=[be1], writes=[blap])
            S.op("pe", lambda h: h.matmul(banks[6][:], lhsT=kb.trif[:], rhs=lap[:], start=True, stop=True, skip_group_check=True),
                 reads=[blap, kb.bconst], writes=[PB[6]])
            S.op("act", lambda h: h.activation(out=ek_tm[:], in_=banks[6][:], func=AF.Exp, scale=1.0 / 16), xr=[PB[6]], writes=[bek])
            S.op("dve", lambda h: h.tensor_tensor(out=kinv_tm[:], in0=banks[1][:], in1=ek_tm[:], op=ALU.mult),
                 xr=[PB[1]], reads=[bek], writes=[bkinv])
            for hh in range(4):
                S.op("pe", lambda h, hh=hh: h.matmul(banks[7][:, hh * 128:(hh + 1) * 128], lhsT=lap[:, hh * 128:(hh + 1) * 128],
                                                     rhs=kb.trif[:], start=True, stop=True, skip_group_check=True),
                     reads=[blap, kb.bconst], writes=[PB[7]], inc=(hh == 3))
            S.op("act", lambda h: h.activation(out=eq[:], in_=banks[7][:], func=AF.Exp, scale=-1.0 / 16, bias=lnq[:, 0:1]),
                 xr=[PB[7]], reads=[kb.bconst], writes=[beq])
            S.op("act", lambda h: h.activation(out=ekT[:], in_=banks[7][:], func=AF.Exp, scale=1.0 / 16), xr=[PB[7]], writes=[bekT])
            cbv = banks[7][:].rearrange("p (h c i) -> p h c i", h=4, c=2)
            S.op("act", lambda h: h.activation(out=dec[:], in_=cbv[:, :, :, 63], func=AF.Exp, scale=-1.0 / 16),
                 xr=[PB[7]], writes=[bdec])
            qv = banks[4][:].rearrange("p (h i) -> p h i", h=4)
            eqv = eq[:].rearrange("p (h i) -> p h i", h=4)
            S.op("dve", lambda h: h.tensor_tensor(out=q_even[:, :, 0:64], in0=qv[:, :, 0:64], in1=eqv[:, :, 0:64], op=ALU.mult),
                 xr=[PB[4]], reads=[beq], writes=[bq])
            S.op("dve", lambda h: h.tensor_tensor(out=q_odd[:, :, 64:128], in0=qv[:, :, 64:128], in1=eqv[:, :, 64:128], op=ALU.mult),
                 xr=[PB[4]], reads=[beq], writes=[bq])
            S.op("dve", lambda h: h.tensor_tensor(out=kinvT[:].rearrange("p h i -> p (h i)"), in0=banks[5][:], in1=ekT[:], op=ALU.mult),
                 xr=[PB[5]], reads=[bekT], writes=[bkT])
            for hh in range(4):
                S.op("pe", lambda h, hh=hh: h.matmul(banks[7][:, hh * 128:(hh + 1) * 128], lhsT=kinvT[:, hh, :], rhs=q_even[:, hh, :],
                                                     start=True, stop=False), reads=[bkT, bq], writes=[PB[7]], inc=False)
                S.op("pe", lambda h, hh=hh: h.matmul(banks[7][:, hh * 128:(hh + 1) * 128], lhsT=kinvT[:, hh, :], rhs=q_odd[:, hh, :],
                                                     start=False, stop=True), reads=[bkT, bq], writes=[PB[7]], inc=(hh == 3))
            for hh in range(4):
                S.op("dve", lambda h, hh=hh: h.tensor_tensor(out=attnT[:, hh, :], in0=banks[7][:, hh * 128:(hh + 1) * 128],
                                                             in1=kb.trif[:], op=ALU.mult),
                     xr=[PB[7]], reads=[kb.bconst], writes=[battn])
            OB = (2, 3, 4, 5)
            UBK = (6, 6, 1, 1)
            for hh in range(4):
                ob = OB[hh]
                vs = v_bf[:, hh * 256:(hh + 1) * 256]
                S.op("pe", lambda h, hh=hh, ob=ob: h.matmul(banks[ob][:, 0:256], lhsT=q_even[:, hh, :], rhs=Sb[:, hh, :], start=True, stop=False),
                     reads=[bq, bSbh[hh]], writes=[PB[ob]], inc=False)
                S.op("pe", lambda h, hh=hh, ob=ob, vs=vs: h.matmul(banks[ob][:, 0:256], lhsT=attnT[:, hh, :], rhs=vs, start=False, stop=False),
                     reads=[battn, bv], writes=[PB[ob]], inc=False)
            for ck in range(2):
                r0 = ck * 64
                for hh in range(4):
                    ub = UBK[hh]; uc = (hh % 2) * 256
                    S.op("pe", lambda h, hh=hh, ub=ub, uc=uc, r0=r0: h.matmul(banks[ub][:, uc:uc + 256], lhsT=kinv_tm[r0:r0 + 64, hh * 128:(hh + 1) * 128],
                                                                              rhs=v_bf[r0:r0 + 64, hh * 256:(hh + 1) * 256], start=True, stop=True),
                         reads=[bkinv, bv], writes=[PB[ub]])
                for hh in range(4):
                    ub = UBK[hh]; uc = (hh % 2) * 256
                    S.op("dve", lambda h, hh=hh, ub=ub, uc=uc: h.tensor_tensor(out=t1[:, hh, :], in0=banks[ub][:, uc:uc + 256], in1=Sf[:, hh, :], op=ALU.add),
                         xr=[PB[ub]], reads=[bSfh[hh]], writes=[bt1h[hh]])
                for hh in range(4):
                    S.op("dve", lambda h, hh=hh, ck=ck: h.tensor_scalar(out=Sf[:, hh, :], in0=t1[:, hh, :], scalar1=dec[:, hh, ck:ck + 1], scalar2=None, op0=ALU.mult),
                         reads=[bt1h[hh], bdec], writes=[bSfh[hh]])
                for hh in range(4):
                    if ck == 0:
                        S.op("act", lambda h, hh=hh: h.copy(out=Sm[:, hh, :], in_=Sf[:, hh, :]), reads=[bSfh[hh]], writes=[bSmh[hh]])
                    else:
                        S.op("act", lambda h, hh=hh: h.copy(out=Sb[:, hh, :], in_=Sf[:, hh, :]), reads=[bSfh[hh]], writes=[bSbh[hh]])
                if ck == 0:
                    for hh in range(4):
                        ob = OB[hh]
                        S.op("pe", lambda h, hh=hh, ob=ob: h.matmul(banks[ob][:, 0:256], lhsT=q_odd[:, hh, :], rhs=Sm[:, hh, :], start=False, stop=True),
                             reads=[bq, bSmh[hh]], writes=[PB[ob]])
            for hh in range(4):
                ob = OB[hh]
                S.op("act", lambda h, hh=hh, ob=ob: h.activation(out=junk2[:], in_=banks[ob][:, 0:256], func=AF.Square,
                                                                 accum_out=hs4[:, hh, 0:1]), xr=[PB[ob]], writes=[bj2, bhs])
            S.op("act", lambda h: h.activation(out=hs4[:, :, 1], in_=hs4[:, :, 0], func=AF.Sqrt, scale=1.0 / 256, bias=kb.epst[:, 0:1]),
                 reads=[kb.bconst], writes=[bhs])
            S.op("dve", lambda h: h.reciprocal(out=hs4[:, :, 2], in_=hs4[:, :, 1]), writes=[bhs])
            for hh in range(4):
                ob = OB[hh]
                S.op("dve", lambda h, hh=hh, ob=ob: h.scalar_tensor_tensor(out=og4[:, hh, :], in0=banks[ob][:, 0:256], scalar=hs4[:, hh, 2:3], in1=g_hd[:],
                                                                           op0=ALU.mult, op1=ALU.mult),
                     xr=[PB[ob]], reads=[bhs, bw], writes=[bog])
            S.op("dve", lambda h: h.tensor_tensor(out=og_bf[:], in0=og4[:].rearrange("p h v -> p (h v)"), in1=sr[:], op=ALU.mult),
                 reads=[bog, bsr], writes=[bogb])
            for kc in range(8):
                S.op("pe", lambda h, kc=kc: h.transpose(out=pTb[:, kc * 128:(kc + 1) * 128], in_=og_bf[:, kc * 128:(kc + 1) * 128],
                                                        identity=kb.ident[:]), reads=[bogb, kb.bconst], writes=[PB[0]], inc=(kc == 7))
            S.op("act", lambda h: h.copy(out=ogT[:], in_=pTb.rearrange("p (a b) -> p a b", a=8)), xr=[PB[0]], writes=[bogT])
            for half in range(2):
                bank = 4 + half
                for kc in range(8):
                    S.op("pe", lambda h, kc=kc, half=half, bank=bank: h.matmul(banks[bank][:], lhsT=ogT[:, kc, :],
                                                                               rhs=w_out_sb[:, kc, half * 512:(half + 1) * 512],
                                                                               start=(kc == 0), stop=(kc == 7)),
                         reads=[bogT, bw], writes=[PB[bank]], inc=(kc == 7))
                S.op("dve", lambda h, half=half, bank=bank, xt=xt: h.tensor_tensor(out=h1t[:, half * 512:(half + 1) * 512], in0=banks[bank][:],
                                                                                  in1=xt[:, half * 512:(half + 1) * 512], op=ALU.add),
                     xr=[PB[bank]], reads=[bx], writes=[bh1])
            S.dma("sp", h1[t * 128:(t + 1) * 128, :], h1t[:], reads=[bh1])
        S.barrier()


def swiglu_acc(kb, st_bufs, xnT, bxnT, ntok, wg, wu, wd, F, acc, bacc, gate_ap_fn, bgate=None, wload=None, acc_init=False, mid_hook=None):
    S, banks, PB = kb.S, kb.banks, kb.PB
    (wg_sb, wu_sb, wd_sb, bws, aT, baT, sg, bsg) = st_bufs
    nfc = F // 128
    GS = 4
    ngr = (nfc + GS - 1) // GS
    ntt = ntok // 512
    nsub = ntok // 128
    cnt = getattr(kb, "_swg_cnt", 0)
    for gi in range(ngr):
        f0 = gi * GS
        nf = min(GS, nfc - f0)
        slot = cnt % 2
        cnt += 1
        bw = bws[slot]
        if wload is not None:
            wload(slot, f0, nf, bw, wg_sb, wu_sb, wd_sb)
        else:
            for kc in range(8):
                S.dma("pool", wg_sb[slot][:, kc, 0:nf * 128], wg[kc * 128:(kc + 1) * 128, f0 * 128:(f0 + nf) * 128], writes=[bw])
                S.dma("pool", wu_sb[slot][:, kc, 0:nf * 128], wu[kc * 128:(kc + 1) * 128, f0 * 128:(f0 + nf) * 128], writes=[bw])
            for fc in range(nf):
                S.dma("pool", wd_sb[slot][:, fc, :], wd[(f0 + fc) * 128:(f0 + fc + 1) * 128, :], writes=[bw])
        for fc in range(nf):
            for tt in range(ntt):
                pg = 2 + (tt % 2) * 2
                pu = pg + 1
                for kc in range(8):
                    S.op("pe", lambda h, kc=kc, fc=fc, tt=tt, pg=pg, slot=slot: h.matmul(
                        banks[pg][:], lhsT=wg_sb[slot][:, kc, fc * 128:(fc + 1) * 128], rhs=xnT[:, kc, tt * 512:(tt + 1) * 512],
                        start=(kc == 0), stop=(kc == 7)), reads=[bw, bxnT], writes=[PB[pg]], inc=(kc == 7))
                for kc in range(8):
                    S.op("pe", lambda h, kc=kc, fc=fc, tt=tt, pu=pu, slot=slot: h.matmul(
                        banks[pu][:], lhsT=wu_sb[slot][:, kc, fc * 128:(fc + 1) * 128], rhs=xnT[:, kc, tt * 512:(tt + 1) * 512],
                        start=(kc == 0), stop=(kc == 7)), reads=[bw, bxnT], writes=[PB[pu]], inc=(kc == 7))
                sgt = sg[tt % 2]
                S.op("act", lambda h, pg=pg, sgt=sgt: h.activation(out=sgt[:], in_=banks[pg][:], func=AF.Silu),
                     xr=[PB[pg]], writes=[bsg[tt % 2]])
                S.op("dve", lambda h, pu=pu, sgt=sgt, fc=fc, tt=tt, slot=slot: h.tensor_tensor(
                    out=aT[slot][:, fc, tt * 512:(tt + 1) * 512], in0=banks[pu][:], in1=sgt[:], op=ALU.mult),
                    xr=[PB[pu]], reads=[bsg[tt % 2]], writes=[baT[slot]])
        if mid_hook is not None and gi == ngr - 1:
            mid_hook()
        for sub in range(nsub):
            for half in range(2):
                pd = (6, 7, 0, 1)[(sub * 2 + half) % 4]
                for fc in range(nf):
                    S.op("pe", lambda h, fc=fc, sub=sub, half=half, pd=pd, slot=slot, nf=nf: h.matmul(
                        banks[pd][:], lhsT=aT[slot][:, fc, sub * 128:(sub + 1) * 128], rhs=wd_sb[slot][:, fc, half * 512:(half + 1) * 512],
                        start=(fc == 0), stop=(fc == nf - 1)), reads=[baT[slot], bw], writes=[PB[pd]], inc=(fc == nf - 1))
                g = gate_ap_fn(sub)
                if acc_init and gi == 0:
                    S.op("dve", lambda h, sub=sub, half=half, pd=pd: h.tensor_copy(out=acc[:, sub, half * 512:(half + 1) * 512], in_=banks[pd][:]),
                         xr=[PB[pd]], writes=[bacc])
                    continue
                S.op("dve", lambda h, sub=sub, half=half, pd=pd, g=g: h.scalar_tensor_tensor(
                    out=acc[:, sub, half * 512:(half + 1) * 512], in0=banks[pd][:], scalar=(1.0 if g is None else g),
                    in1=acc[:, sub, half * 512:(half + 1) * 512], op0=ALU.mult, op1=ALU.add),
                    xr=[PB[pd]], reads=([bacc] if bgate is None else [bacc, bgate]), writes=[bacc])
    kb._swg_cnt = cnt


def swiglu_bufs(kb, st, ntok):
    sb = lambda shape, dt, name=None: kb.sb(st, shape, dt, name)
    wg_sb = [sb([128, 8, 512], BF16, "wg") for _ in range(2)]
    wu_sb = [sb([128, 8, 512], BF16, "wu") for _ in range(2)]
    wd_sb = [sb([128, 4, 1024], BF16, "wd") for _ in range(2)]
    bws = [Buf("w0"), Buf("w1")]
    aT0 = sb([128, 4, ntok], BF16, "aT")
    aT = [aT0, aT0]
    baT0 = Buf("aT0")
    baT = [baT0, baT0]
    sg = [sb([128, 512], F32, "sg") for _ in range(2)]
    bsg = [Buf("sg0"), Buf("sg1")]
    return (wg_sb, wu_sb, wd_sb, bws, aT, baT, sg, bsg)


def stage_ffn(kb, h1, h2, ffn_norm0, wg, wu, wd, TB=2048):
    S = kb.S
    with ExitStack() as st:
        sb = lambda shape, dt, name=None: kb.sb(st, shape, dt, name)
        g_bc = sb([128, 1024], F32, "g_ffn")
        bg = Buf("g")
        S.dma("sp", g_bc[:], ffn_norm0.partition_broadcast(128), writes=[bg])
        ntmp = kb.norm_tmp(st)
        xnT = sb([128, 8, TB], BF16, "xnT"); bxnT = Buf("xnT")
        acc = sb([128, TB // 128, 1024], F32, "acc"); bacc = Buf("acc")
        bufs = swiglu_bufs(kb, st, TB)
        for blk in range(S_LEN // TB):
            for sub in range(TB // 128):
                r0 = blk * TB + sub * 128
                S.dma("sp", acc[:, sub, :], h1[r0:r0 + 128, :], writes=[bacc])
                kb.norm_T(ntmp, acc[:, sub, :], bacc, [(g_bc[:], bg, xnT[:, :, sub * 128:(sub + 1) * 128], bxnT)])
            swiglu_acc(kb, bufs, xnT, bxnT, TB, wg, wu, wd, FFN_DIM, acc, bacc, lambda sub: None)
            for sub in range(TB // 128):
                r0 = blk * TB + sub * 128
                S.dma("sp", h2[r0:r0 + 128, :], acc[:, sub, :], reads=[bacc])
        S.barrier()


def stage_qkv(kb, h2, qT_d, kT_d, v_d, kv_norm, attn_norm1, w_kv, w_q):
    S, banks, PB = kb.S, kb.banks, kb.PB
    with ExitStack() as st:
        sb = lambda shape, dt, name=None: kb.sb(st, shape, dt, name)
        bw = Buf("w")
        wq_sb = sb([128, 8, 1024], BF16, "wq")
        wkv_sb = sb([128, 8, 2048], BF16, "wkv")
        kb.load_w_bf16(wq_sb, w_q, bw, 8, 1024)
        kb.load_w_bf16(wkv_sb, w_kv, bw, 8, 2048)
        g_kv = sb([128, 1024], F32, "g_kv")
        g_q = sb([128, 1024], F32, "g_q")
        S.dma("sp", g_kv[:], kv_norm.partition_broadcast(128), writes=[bw])
        S.dma("sp", g_q[:], attn_norm1.partition_broadcast(128), writes=[bw])
        ntmp = kb.norm_tmp(st)
        TB = 512
        xt = [sb([128, 1024], F32, "xt") for _ in range(2)]
        bxt = [Buf("xt0"), Buf("xt1")]
        xkvT = sb([128, 8, TB], BF16, "xkvT"); bxkv = Buf("xkvT")
        xqT = sb([128, 8, TB], BF16, "xqT"); bxq = Buf("xqT")
        stg = [sb([128, 512], BF16, "stg") for _ in range(3)]
        bstg = [Buf("stg%d" % i) for i in range(3)]
        sc = 128.0 ** -0.5
        n = 0
        for blk in range(S_LEN // TB):
            for sub in range(TB // 128):
                r0 = blk * TB + sub * 128
                x_ = xt[sub % 2]; bx_ = bxt[sub % 2]
                S.dma("sp", x_[:], h2[r0:r0 + 128, :], writes=[bx_])
                kb.norm_T(ntmp, x_[:], bx_, [(g_kv[:], bw, xkvT[:, :, sub * 128:(sub + 1) * 128], bxkv),
                                            (g_q[:], bw, xqT[:, :, sub * 128:(sub + 1) * 128], bxq)])
            t0 = blk * TB
            for hh in range(8):
                for (wsb, c0, xT_, bx2, dst, scale) in ((wq_sb, hh * 128, xqT, bxq, qT_d, sc), (wkv_sb, hh * 128, xkvT, bxkv, kT_d, 1.0)):
                    bank = 1 + (n % 3); sidx = n % 3; n += 1
                    for kc in range(8):
                        S.op("pe", lambda h, kc=kc, wsb=wsb, c0=c0, xT_=xT_, bank=bank: h.matmul(
                            banks[bank][:], lhsT=wsb[:, kc, c0:c0 + 128], rhs=xT_[:, kc, :], start=(kc == 0), stop=(kc == 7)),
                            reads=[bw, bx2], writes=[PB[bank]], inc=(kc == 7))
                    S.op("act", lambda h, bank=bank, sidx=sidx, scale=scale: h.activation(out=stg[sidx][:], in_=banks[bank][:], func=AF.Copy, scale=scale),
                         xr=[PB[bank]], writes=[bstg[sidx]])
                    S.dma("sp", dst[hh, :, t0:t0 + TB], stg[sidx][:], reads=[bstg[sidx]])
            for sub in range(TB // 128):
                for half in range(2):
                    bank = 1 + (n % 3); sidx = n % 3; n += 1
                    for kc in range(8):
                        S.op("pe", lambda h, kc=kc, sub=sub, half=half, bank=bank: h.matmul(
                            banks[bank][:], lhsT=xkvT[:, kc, sub * 128:(sub + 1) * 128], rhs=wkv_sb[:, kc, 1024 + half * 512:1024 + (half + 1) * 512],
                            start=(kc == 0), stop=(kc == 7)), reads=[bw, bxkv], writes=[PB[bank]], inc=(kc == 7))
                    S.op("act", lambda h, bank=bank, sidx=sidx: h.copy(out=stg[sidx][:], in_=banks[bank][:]), xr=[PB[bank]], writes=[bstg[sidx]])
                    S.dma("sp", v_d[t0 + sub * 128:t0 + (sub + 1) * 128, half * 512:(half + 1) * 512], stg[sidx][:], reads=[bstg[sidx]])
        S.barrier()


def stage_sb(kb, qT_d, kT_d, v_d, oT_d, nheads=8, nqb=NT):
    S, banks, PB = kb.S, kb.banks, kb.PB
    with ExitStack() as st:
        sb = lambda shape, dt, name=None: kb.sb(st, shape, dt, name)
        qT = [sb([128, S_LEN], BF16, "qT") for _ in range(2)]
        kT = [sb([128, S_LEN], BF16, "kT") for _ in range(2)]
        vh = [sb([128, NT, 128], BF16, "vh") for _ in range(2)]
        bqkv = [Buf("qkv0"), Buf("qkv1")]
        ones = sb([128, 1], F32, "ones")
        cmask = sb([128, 128], F32, "cmask")
        bc = Buf("c")
        S.op("dve", lambda h: h.memset(ones[:], 1.0), writes=[bc])
        S.op("pool", lambda h: h.memset(cmask[:], 1.0), writes=[bc])
        S.op("pool", lambda h: h.affine_select(out=cmask[:], in_=cmask[:], pattern=[[-1, 128]], compare_op=ALU.is_gt,
                                               fill=0.0, base=0, channel_multiplier=1), writes=[bc])
        E = [sb([128, 512], F32, "E") for _ in range(2)]; bE = [Buf("E0"), Buf("E1")]
        SP = [sb([128, S_LEN + 1], F32, "SP") for _ in range(2)]; bSP = [Buf("SP0"), Buf("SP1")]
        for i in range(2):
            S.op("dve", lambda h, i=i: h.memset(SP[i][:, 0:1], 0.0), writes=[bSP[i]])
        G = sb([128, 512], F32, "G"); bG = Buf("G")
        ARG = [sb([128, S_LEN], F32, "ARG") for _ in range(2)]; bARG = [Buf("ARG0"), Buf("ARG1")]
        nt = sb([128, 2], F32, "nt"); bnts = [Buf("nt0"), Buf("nt1")]
        W = sb([128, S_LEN], BF16, "W"); bW = Buf("W")
        WT = sb([128, NT, 128], BF16, "WT"); bWT = Buf("WT")
        oTs = [sb([128, 128], BF16, "oTs") for _ in range(2)]; boT = [Buf("oT0"), Buf("oT1")]
        items = [(hh, tb) for hh in range(nheads) for tb in range(nqb)]
        N = len(items)
        cneg = sb([128, 128], F32, "cneg")
        S.op("dve", lambda h: h.tensor_scalar(out=cneg[:], in0=cmask[:], scalar1=-1.0, scalar2=1e30, op0=ALU.add, op1=ALU.mult),
             writes=[bc])
        TRB = (0, 3, 4, 5)

        bWc = [Buf("W%d" % c) for c in range(4)]
        bWTc = [Buf("WT%d" % c) for c in range(4)]

        def act_extras(j):
            ex = []
            if 0 <= j - 3 < N:
                def f(i=j - 3):
                    hh, tb = items[i]
                    r = i % 2
                    S.op("act", lambda h: h.copy(out=oTs[r][:], in_=banks[7][:, 0:128]), xr=[PB[7]], writes=[boT[r]])
                    S.dma("sp", oT_d[hh, :, tb * 128:(tb + 1) * 128], oTs[r][:], reads=[boT[r]])
                ex.append(f)
            if 0 <= j - 2 < N:
                hh, tb = items[j - 2]
                nb = tb + 1
                for c in range((nb + 7) // 8):
                    def f(c=c, nb=nb):
                        b0 = c * 8
                        nbb = min(8, nb - b0)
                        pT = banks[TRB[c]][:].bitcast(BF16)
                        S.op("act", lambda h: h.copy(out=WT[:, b0:b0 + nbb, :].rearrange("p a b -> p (a b)"), in_=pT[:, 0:nbb * 128]),
                             xr=[PB[TRB[c]]], writes=[bWTc[c]])
                    ex.append(f)
            if 0 <= j - 1 < N:
                hh, tb = items[j - 1]
                ns = (tb + 1) * 128
                r = (j - 1) % 2
                for c in range((ns + 1023) // 1024):
                    def f(c=c, ns=ns, r=r):
                        c0 = c * 1024
                        c1 = min(ns, c0 + 1024)
                        S.op("act", lambda h: h.activation(out=W[:, c0:c1], in_=ARG[r][:, c0:c1], func=AF.Exp, bias=nt[:, r:r + 1]),
                             reads=[bARG[r], bnts[r]], writes=[bWc[c]])
                    ex.append(f)
            return ex

        def pe_extras(j):
            ex = []
            if 0 <= j - 2 < N:
                hh, tb = items[j - 2]
                sl = hh % 2
                nb = tb + 1
                for c in range((nb + 7) // 8):
                    def f(c=c, nb=nb, sl=sl):
                        for b in range(c * 8, min(nb, c * 8 + 8)):
                            S.op("pe", lambda h, b=b: h.matmul(banks[7][:, 0:128], lhsT=vh[sl][:, b, :], rhs=WT[:, b, :], start=(b == 0), stop=(b == nb - 1)),
                                 reads=[bqkv[sl], bWTc[c]], writes=[PB[7]], inc=(b == min(nb, c * 8 + 8) - 1))
                    ex.append(f)
            if 0 <= j - 1 < N:
                hh, tb = items[j - 1]
                nb = tb + 1
                for c in range((nb + 7) // 8):
                    def f(c=c, nb=nb):
                        b0 = c * 8
                        nbb = min(8, nb - b0)
                        pT = banks[TRB[c]][:].bitcast(BF16)
                        for jj in range(nbb):
                            S.op("pe", lambda h, jj=jj: h.transpose(out=pT[:, jj * 128:(jj + 1) * 128], in_=W[:, (b0 + jj) * 128:(b0 + jj + 1) * 128],
                                                                    identity=kb.ident[:]), reads=[bWc[c], kb.bconst], writes=[PB[TRB[c]]], inc=(jj == nbb - 1))
                    ex.append(f)
            return ex

        def pump(aex, pex, cnt):
            if aex:
                aex.pop(0)()
                cnt[0] += 1
            if pex and (cnt[0] >= cnt[1] + 2 or not aex):
                pex.pop(0)()
                cnt[1] += 1

        def s1(i, aex, pex, cnt):
            hh, tb = items[i]
            sl = hh % 2
            r = i % 2
            if tb == 0:
                S.dma("sp", qT[sl][:], qT_d[hh], writes=[bqkv[sl]])
                S.dma("sp", kT[sl][:], kT_d[hh], writes=[bqkv[sl]])
                S.dma("sp", vh[sl][:], v_d[:, hh * 128:(hh + 1) * 128].rearrange("(b p) d -> p b d", p=128), writes=[bqkv[sl]])
            ns = (tb + 1) * 128
            nkt = (ns + 511) // 512
            ZB = (1, 2, 6)

            def zmm(kt):
                s0 = kt * 512
                w = min(512, ns - s0)
                zb = ZB[kt % 3]
                S.op("pe", lambda h: h.matmul(banks[zb][:, 0:w], lhsT=qT[sl][:, tb * 128:(tb + 1) * 128],
                                              rhs=kT[sl][:, s0:s0 + w], start=True, stop=True),
                     reads=[bqkv[sl]], writes=[PB[zb]])

            zmm(0)
            for kt in range(nkt):
                s0 = kt * 512
                w = min(512, ns - s0)
                zb = ZB[kt % 3]
                if kt + 1 < nkt:
                    zmm(kt + 1)
                e_ = E[kt % 2]; be_ = bE[kt % 2]
                S.op("act", lambda h, zb=zb, w=w, e_=e_: h.activation(out=e_[:, 0:w], in_=banks[zb][:, 0:w], func=AF.Exp),
                     xr=[PB[zb]], writes=[be_])
                S.op("act", lambda h, w=w, e_=e_, r=r, s0=s0: h.activation(out=SP[r][:, 1 + s0:1 + s0 + w], in_=e_[:, 0:w], func=AF.Ln, bias=1.0),
                     reads=[be_], writes=[bSP[r]])
                pump(aex, pex, cnt)
                if kt == nkt - 1:
                    d0 = 1 + ns - 128
                    S.op("dve", lambda h, r=r, d0=d0: h.tensor_tensor(out=SP[r][:, d0:d0 + 128], in0=SP[r][:, d0:d0 + 128], in1=cmask[:], op=ALU.mult),
                         reads=[bc], writes=[bSP[r]])
                init = 0.0 if kt == 0 else G[:, 511:512]
                S.op("dve", lambda h, r=r, s0=s0, w=w, init=init: h.tensor_tensor_scan(
                    out=G[:, 0:w], data0=ones[:, 0:1].to_broadcast([128, w]), data1=SP[r][:, s0:s0 + w], initial=init,
                    op0=ALU.mult, op1=ALU.add), reads=[bSP[r], bc], writes=[bG])
                S.op("dve", lambda h, r=r, s0=s0, w=w, zb=zb: h.tensor_tensor(out=ARG[r][:, s0:s0 + w], in0=banks[zb][:, 0:w], in1=G[:, 0:w], op=ALU.add),
                     xr=[PB[zb]], reads=[bG], writes=[bARG[r]])
            lw = ns - (nkt - 1) * 512
            S.op("dve", lambda h, lw=lw, r=r: h.tensor_scalar(out=nt[:, r:r + 1], in0=G[:, lw - 1:lw], scalar1=-1.0, scalar2=None, op0=ALU.mult),
                 reads=[bG], writes=[bnts[r]])
            S.op("dve", lambda h, r=r, ns=ns: h.tensor_tensor(out=ARG[r][:, ns - 128:ns], in0=ARG[r][:, ns - 128:ns], in1=cneg[:], op=ALU.add),
                 reads=[bc], writes=[bARG[r]])

        for j in range(N + 3):
            aex = act_extras(j)
            pex = pe_extras(j)
            cnt = [0, 0]
            if j < N:
                s1(j, aex, pex, cnt)
            while aex or pex:
                pump(aex, pex, cnt)
        S.barrier()


def stage_attn_out(kb, oT_d, h2, h3, w_out):
    S, banks, PB = kb.S, kb.banks, kb.PB
    with ExitStack() as st:
        sb = lambda shape, dt, name=None: kb.sb(st, shape, dt, name)
        bw = Buf("w")
        wo_sb = sb([128, 8, 1024], BF16, "wo")
        kb.load_w_bf16(wo_sb, w_out, bw, 8, 1024)
        TB = 512
        oT = [sb([128, 8, TB], BF16, "oT") for _ in range(2)]; boT = [Buf("oT0"), Buf("oT1")]
        xt = [sb([128, 1024], F32, "xt") for _ in range(2)]; bxt = [Buf("x0"), Buf("x1")]
        n = 0
        for blk in range(S_LEN // TB):
            sl = blk % 2
            for hh in range(8):
                S.dma("sp", oT[sl][:, hh, :], oT_d[hh, :, blk * TB:(blk + 1) * TB], writes=[boT[sl]])
            for sub in range(TB // 128):
                r0 = blk * TB + sub * 128
                x_ = xt[n % 2]; bx_ = bxt[n % 2]; n += 1
                S.dma("sp", x_[:], h2[r0:r0 + 128, :], writes=[bx_])
                for half in range(2):
                    bank = 1 + half
                    for kc in range(8):
                        S.op("pe", lambda h, kc=kc, sub=sub, half=half, bank=bank, sl=sl: h.matmul(
                            banks[bank][:], lhsT=oT[sl][:, kc, sub * 128:(sub + 1) * 128], rhs=wo_sb[:, kc, half * 512:(half + 1) * 512],
                            start=(kc == 0), stop=(kc == 7)), reads=[boT[sl], bw], writes=[PB[bank]], inc=(kc == 7))
                    S.op("dve", lambda h, half=half, bank=bank, x_=x_: h.tensor_tensor(out=x_[:, half * 512:(half + 1) * 512], in0=banks[bank][:],
                                                                                      in1=x_[:, half * 512:(half + 1) * 512], op=ALU.add),
                         xr=[PB[bank]], reads=[bx_], writes=[bx_])
                S.dma("sp", h3[r0:r0 + 128, :], x_[:], reads=[bx_])
        S.barrier()


def stage_moe(kb, h3, out, ffn_norm1, final_norm, w_router, wg, wu, wd, TB=2048, nexp=NEXP):
    S, banks, PB = kb.S, kb.banks, kb.PB
    with ExitStack() as st:
        sb = lambda shape, dt, name=None: kb.sb(st, shape, dt, name)
        bg = Buf("g")
        g_bc = sb([128, 1024], F32, "g_ffn1")
        g_fin = sb([128, 1024], F32, "g_fin")
        S.dma("sp", g_bc[:], ffn_norm1.partition_broadcast(128), writes=[bg])
        S.dma("sp", g_fin[:], final_norm.partition_broadcast(128), writes=[bg])
        wr_sb = sb([128, 8, 8], BF16, "wr")
        for kc in range(8):
            S.dma("pool", wr_sb[:, kc, :], w_router[kc * 128:(kc + 1) * 128, :], writes=[bg])
        ntmp = kb.norm_tmp(st)
        xnT = sb([128, 8, TB], BF16, "xnT"); bxnT = Buf("xnT")
        acc = sb([128, TB // 128, 1024], F32, "acc"); bacc = Buf("acc")
        nsub = TB // 128
        gates = sb([128, nsub, 8], F32, "gates"); bgates = Buf("gates")
        lg = sb([128, 8], F32, "lg"); blg = Buf("lg")
        m1 = sb([128, 8], F32, "m1"); mk1 = sb([128, 8], F32, "mk1"); mk2 = sb([128, 8], F32, "mk2"); l2 = sb([128, 8], F32, "l2")
        ot = sb([128, 1024], F32, "ot"); bot = Buf("ot")
        bufs = swiglu_bufs(kb, st, TB)
        for blk in range(S_LEN // TB):
            for sub in range(nsub):
                r0 = blk * TB + sub * 128
                S.dma("sp", acc[:, sub, :], h3[r0:r0 + 128, :], writes=[bacc])
                kb.norm_T(ntmp, acc[:, sub, :], bacc, [(g_bc[:], bg, xnT[:, :, sub * 128:(sub + 1) * 128], bxnT)])
                for kc in range(8):
                    S.op("pe", lambda h, kc=kc, sub=sub: h.matmul(banks[1][:, 0:8], lhsT=xnT[:, kc, sub * 128:(sub + 1) * 128], rhs=wr_sb[:, kc, :],
                                                                  start=(kc == 0), stop=(kc == 7)), reads=[bxnT, bg], writes=[PB[1]], inc=(kc == 7))
                S.op("dve", lambda h: h.tensor_copy(out=lg[:], in_=banks[1][:, 0:8]), xr=[PB[1]], writes=[blg])
                S.op("dve", lambda h: h.tensor_reduce(out=m1[:, 0:1], in_=lg[:], axis=mybir.AxisListType.X, op=ALU.max), writes=[blg])
                S.op("dve", lambda h: h.tensor_scalar(out=mk1[:], in0=lg[:], scalar1=m1[:, 0:1], scalar2=None, op0=ALU.is_equal), writes=[blg])
                S.op("dve", lambda h: h.scalar_tensor_tensor(out=l2[:], in0=mk1[:], scalar=-1e30, in1=lg[:], op0=ALU.mult, op1=ALU.add), writes=[blg])
                S.op("dve", lambda h: h.tensor_reduce(out=m1[:, 1:2], in_=l2[:], axis=mybir.AxisListType.X, op=ALU.max), writes=[blg])
                S.op("dve", lambda h: h.tensor_scalar(out=mk2[:], in0=l2[:], scalar1=m1[:, 1:2], scalar2=None, op0=ALU.is_equal), writes=[blg])
                S.op("dve", lambda h: h.tensor_tensor(out=m1[:, 2:3], in0=m1[:, 0:1], in1=m1[:, 1:2], op=ALU.subtract), writes=[blg])
                S.op("act", lambda h: h.activation(out=m1[:, 3:4], in_=m1[:, 2:3], func=AF.Sigmoid), writes=[blg])
                S.op("act", lambda h: h.activation(out=m1[:, 4:5], in_=m1[:, 2:3], func=AF.Sigmoid, scale=-1.0), writes=[blg])
                S.op("dve", lambda h: h.tensor_scalar(out=mk1[:], in0=mk1[:], scalar1=m1[:, 3:4], scalar2=None, op0=ALU.mult), writes=[blg])
                S.op("dve", lambda h, sub=sub: h.scalar_tensor_tensor(out=gates[:, sub, :], in0=mk2[:], scalar=m1[:, 4:5], in1=mk1[:], op0=ALU.mult, op1=ALU.add),
                     reads=[blg], writes=[bgates])
            for e in range(nexp):
                swiglu_acc(kb, bufs, xnT, bxnT, TB, wg[e], wu[e], wd[e], EXPERT_DIM, acc, bacc,
                           lambda sub, e=e: gates[:, sub, e:e + 1], bgates)
            junk, bj, ss, bss, xn, bxn = ntmp
            for sub in range(nsub):
                r0 = blk * TB + sub * 128
                S.op("act", lambda h, sub=sub: h.activation(out=junk[:], in_=acc[:, sub, :], func=AF.Square, accum_out=ss[:, 0:1]),
                     reads=[bacc, bgates], writes=[bj, bss])
                S.op("act", lambda h: h.activation(out=ss[:, 1:2], in_=ss[:, 0:1], func=AF.Sqrt, scale=1.0 / D, bias=kb.epst[:, 0:1]),
                     reads=[kb.bconst], writes=[bss])
                S.op("dve", lambda h: h.reciprocal(out=ss[:, 2:3], in_=ss[:, 1:2]), writes=[bss])
                S.op("dve", lambda h, sub=sub: h.scalar_tensor_tensor(out=ot[:], in0=acc[:, sub, :], scalar=ss[:, 2:3], in1=g_fin[:], op0=ALU.mult, op1=ALU.mult),
                     reads=[bacc, bss, bg], writes=[bot])
                S.dma("sp", out[r0:r0 + 128, :], ot[:], reads=[bot])
        S.barrier()


I32 = mybir.dt.int32
MOE_G = 1024
MOE_NG = 15


def stage_moe_sparse(kb, h3, out, xs_d, y_d, ffn_norm1, final_norm, w_router, wg, wu, wd, ngroups=MOE_NG):
    S, banks, PB, nc = kb.S, kb.banks, kb.PB, kb.nc
    G = MOE_G
    wg2 = wg.rearrange("e k f -> (e k) f")
    wu2 = wu.rearrange("e k f -> (e k) f")
    wd2 = wd.rearrange("e f d -> (e f) d")
    IOA = bass.IndirectOffsetOnAxis
    with ExitStack() as st0:
        sb0 = lambda shape, dt, name=None: kb.sb(st0, shape, dt, name)
        bg = Buf("g")
        g_bc = sb0([128, 1024], F32, "g_ffn1")
        g_fin = sb0([128, 1024], F32, "g_fin")
        S.dma("sp", g_bc[:], ffn_norm1.partition_broadcast(128), writes=[bg])
        S.dma("sp", g_fin[:], final_norm.partition_broadcast(128), writes=[bg])
        w1_all = sb0([128, NT], F32, "w1_all"); w2_all = sb0([128, NT], F32, "w2_all"); bwa = Buf("w_all")
        slot_i = [sb0([128, NT], I32, "slot1_i"), sb0([128, NT], I32, "slot2_i")]; bslot = Buf("slot")
        widx1 = sb0([128, MOE_NG, 8], I32, "widx1"); widx2 = sb0([128, MOE_NG, 28], I32, "widx2"); bwidx = Buf("widx")
        bxs = Buf("xs_d"); by = Buf("y_d"); bxs0 = Buf("xs_zero")
        with ExitStack() as st:
            sb = lambda shape, dt, name=None: kb.sb(st, shape, dt, name)
            wr_sb = sb([128, 8, 8], BF16, "wr")
            for kc in range(8):
                S.dma("pool", wr_sb[:, kc, :], w_router[kc * 128:(kc + 1) * 128, :], writes=[bg])
            zt = sb([128, 1024], BF16, "zt"); bz = Buf("zt")
            S.op("pool", lambda h: h.memset(zt[:], 0.0), writes=[bz])
            for blk in range(MOE_NG * G // 128):
                S.dma("sp", xs_d[blk * 128:(blk + 1) * 128, :], zt[:], reads=[bz], writes=[bxs0])
            xn_parts = [sb([128, 8, 1024], BF16, "xn_all%d" % i) for i in range(4)]; bxn = Buf("xn_all")
            xn_row = lambda t: xn_parts[t // 8][:, t % 8, :]
            xts = [sb([128, 1024], F32, "xt") for _ in range(2)]; bxts = [Buf("x0"), Buf("x1")]
            junk = sb([128, 1024], BF16, "junk"); bj = Buf("junk")
            ss = sb([128, 4], F32, "ss"); bss = Buf("ss")
            xnT = sb([128, 8, 128], BF16, "xnT"); bxnT = Buf("xnT")
            sel = sb([128, NT, 8], F32, "sel"); mk1a = sb([128, NT, 8], F32, "mk1a"); mk2a = sb([128, NT, 8], F32, "mk2a"); bsel = Buf("sel")
            lg = sb([128, 8], F32, "lg"); blg = Buf("lg")
            m1 = sb([128, 8], F32, "m1"); l2 = sb([128, 8], F32, "l2")
            pT = banks[0][:].bitcast(BF16)
            for t in range(NT):
                xt = xts[t % 2]; bx = bxts[t % 2]
                S.dma("sp", xt[:], h3[t * 128:(t + 1) * 128, :], writes=[bx])
                S.op("act", lambda h, xt=xt: h.activation(out=junk[:], in_=xt[:], func=AF.Square, accum_out=ss[:, 0:1]),
                     reads=[bx], writes=[bj, bss])
                S.op("act", lambda h: h.activation(out=ss[:, 1:2], in_=ss[:, 0:1], func=AF.Sqrt, scale=1.0 / D, bias=kb.epst[:, 0:1]),
                     reads=[kb.bconst], writes=[bss])
                S.op("dve", lambda h: h.reciprocal(out=ss[:, 2:3], in_=ss[:, 1:2]), writes=[bss])
                S.op("dve", lambda h, xt=xt, t=t: h.scalar_tensor_tensor(out=xn_row(t), in0=xt[:], scalar=ss[:, 2:3], in1=g_bc[:],
                                                                         op0=ALU.mult, op1=ALU.mult), reads=[bx, bss, bg], writes=[bxn])
                for kc in range(8):
                    S.op("pe", lambda h, kc=kc, t=t: h.transpose(out=pT[:, kc * 128:(kc + 1) * 128], in_=xn_row(t)[:, kc * 128:(kc + 1) * 128],
                                                                 identity=kb.ident[:]), reads=[bxn, kb.bconst], writes=[PB[0]], inc=(kc == 7))
                S.op("act", lambda h: h.copy(out=xnT[:], in_=pT.rearrange("p (a b) -> p a b", a=8)), xr=[PB[0]], writes=[bxnT])
                for kc in range(8):
                    S.op("pe", lambda h, kc=kc: h.matmul(banks[1][:, 0:8], lhsT=xnT[:, kc, :], rhs=wr_sb[:, kc, :],
                                                         start=(kc == 0), stop=(kc == 7)), reads=[bxnT, bg], writes=[PB[1]], inc=(kc == 7))
                S.op("dve", lambda h: h.tensor_copy(out=lg[:], in_=banks[1][:, 0:8]), xr=[PB[1]], writes=[blg])
                S.op("dve", lambda h: h.tensor_reduce(out=m1[:, 0:1], in_=lg[:], axis=mybir.AxisListType.X, op=ALU.max), writes=[blg])
                S.op("dve", lambda h, t=t: h.tensor_scalar(out=mk1a[:, t, :], in0=lg[:], scalar1=m1[:, 0:1], scalar2=None, op0=ALU.is_equal),
                     reads=[blg], writes=[bsel])
                S.op("dve", lambda h, t=t: h.scalar_tensor_tensor(out=l2[:], in0=mk1a[:, t, :], scalar=-1e30, in1=lg[:], op0=ALU.mult, op1=ALU.add),
                     reads=[bsel], writes=[blg])
                S.op("dve", lambda h: h.tensor_reduce(out=m1[:, 1:2], in_=l2[:], axis=mybir.AxisListType.X, op=ALU.max), writes=[blg])
                S.op("dve", lambda h, t=t: h.tensor_scalar(out=mk2a[:, t, :], in0=l2[:], scalar1=m1[:, 1:2], scalar2=None, op0=ALU.is_equal),
                     reads=[blg], writes=[bsel])
                S.op("dve", lambda h: h.tensor_tensor(out=m1[:, 2:3], in0=m1[:, 0:1], in1=m1[:, 1:2], op=ALU.subtract), writes=[blg])
                S.op("act", lambda h, t=t: h.activation(out=w1_all[:, t:t + 1], in_=m1[:, 2:3], func=AF.Sigmoid), reads=[blg], writes=[bwa])
                S.op("act", lambda h, t=t: h.activation(out=w2_all[:, t:t + 1], in_=m1[:, 2:3], func=AF.Sigmoid, scale=-1.0), reads=[blg], writes=[bwa])
                S.op("dve", lambda h, t=t: h.tensor_tensor(out=sel[:, t, :], in0=mk1a[:, t, :], in1=mk2a[:, t, :], op=ALU.add), writes=[bsel])
            ustr = sb([128, 128], F32, "ustr"); onesm = sb([128, 128], F32, "onesm"); ones1 = sb([128, 1], F32, "ones1"); bu = Buf("u")
            S.op("pool", lambda h: h.memset(ustr[:], 1.0), writes=[bu])
            S.op("pool", lambda h: h.affine_select(out=ustr[:], in_=ustr[:], pattern=[[1, 128]], compare_op=ALU.is_gt, fill=0.0,
                                                   base=0, channel_multiplier=-1), writes=[bu])
            S.op("pool", lambda h: h.memset(onesm[:], 1.0), writes=[bu])
            S.op("pool", lambda h: h.memset(ones1[:], 1.0), writes=[bu])
            pid_i = sb([128, 1], I32, "pid_i"); pid = sb([128, 1], F32, "pid")
            S.op("pool", lambda h: h.iota(pid_i[:], pattern=[[0, 1]], base=0, channel_multiplier=1), writes=[bu])
            S.op("dve", lambda h: h.tensor_copy(out=pid[:], in_=pid_i[:]), writes=[bu])
            selv = sel[:].rearrange("p t e -> p (t e)")
            S.op("pe", lambda h: h.matmul(banks[2][:, 0:256], lhsT=ustr[:], rhs=selv, start=True, stop=True, skip_group_check=True), reads=[bsel, bu], writes=[PB[2]])
            S.op("pe", lambda h: h.matmul(banks[3][:, 0:256], lhsT=onesm[:], rhs=selv, start=True, stop=True, skip_group_check=True), reads=[bsel, bu], writes=[PB[3]])
            tot = sb([128, NT, 8], F32, "tot"); incl = sb([128, NT, 8], F32, "incl"); base = sb([128, NT, 8], F32, "base"); bt = Buf("tot")
            S.op("dve", lambda h: h.tensor_copy(out=tot[:].rearrange("p t e -> p (t e)"), in_=banks[3][:, 0:256]), xr=[PB[3]], writes=[bt])
            for e in range(8):
                S.op("dve", lambda h, e=e: h.tensor_tensor_scan(out=incl[:, :, e], data0=ones1[:, 0:1].to_broadcast([128, NT]), data1=tot[:, :, e],
                                                                initial=0.0, op0=ALU.mult, op1=ALU.add), reads=[bu], writes=[bt])
            S.op("dve", lambda h: h.tensor_tensor(out=base[:], in0=incl[:], in1=tot[:], op=ALU.subtract), writes=[bt])
            ne = incl[:, NT - 1, :]
            ng = sb([128, 8], F32, "ng"); tmp8 = sb([128, 8], F32, "tmp8"); gi = sb([128, 8], F32, "gi"); off = sb([128, 8], F32, "off")
            S.op("dve", lambda h: h.tensor_scalar(out=ng[:], in0=ne, scalar1=0.5, scalar2=None, op0=ALU.is_gt), writes=[bt])
            for k in range(1, 4):
                S.op("dve", lambda h, k=k: h.tensor_scalar(out=tmp8[:], in0=ne, scalar1=k * G + 0.5, scalar2=None, op0=ALU.is_gt), writes=[bt])
                S.op("dve", lambda h: h.tensor_tensor(out=ng[:], in0=ng[:], in1=tmp8[:], op=ALU.add), writes=[bt])
            S.op("dve", lambda h: h.tensor_tensor_scan(out=gi[:], data0=ones1[:, 0:1].to_broadcast([128, 8]), data1=ng[:], initial=0.0,
                                                       op0=ALU.mult, op1=ALU.add), reads=[bu], writes=[bt])
            S.op("dve", lambda h: h.tensor_tensor(out=off[:], in0=gi[:], in1=ng[:], op=ALU.subtract), writes=[bt])
            S.op("dve", lambda h: h.tensor_scalar(out=off[:], in0=off[:], scalar1=float(G), scalar2=None, op0=ALU.mult), writes=[bt])
            for e in range(8):
                S.op("dve", lambda h, e=e: h.tensor_scalar(out=base[:, :, e], in0=base[:, :, e], scalar1=off[:, e:e + 1], scalar2=None, op0=ALU.add), writes=[bt])
            slotf = sb([128, NT, 8], F32, "slotf"); prod = sb([128, NT, 8], F32, "prod"); s12 = sb([128, 2, NT], F32, "s12")
            S.op("dve", lambda h: h.tensor_tensor(out=slotf[:].rearrange("p t e -> p (t e)"), in0=banks[2][:, 0:256],
                                                  in1=base[:].rearrange("p t e -> p (t e)"), op=ALU.add), xr=[PB[2]], writes=[bt])
            for k, mk in enumerate((mk1a, mk2a)):
                S.op("dve", lambda h, mk=mk: h.tensor_tensor(out=prod[:], in0=mk[:], in1=slotf[:], op=ALU.mult), reads=[bsel], writes=[bt])
                S.op("dve", lambda h, k=k: h.tensor_reduce(out=s12[:, k, :], in_=prod[:], axis=mybir.AxisListType.X, op=ALU.add), writes=[bt])
                S.op("dve", lambda h, k=k: h.tensor_copy(out=slot_i[k][:], in_=s12[:, k, :]), reads=[bt], writes=[bslot])
            ge = sb([128, MOE_NG], F32, "ge"); rb1 = sb([128, MOE_NG], F32, "rb1"); rb2 = sb([128, MOE_NG], F32, "rb2")
            for j in range(MOE_NG):
                S.op("dve", lambda h, j=j: h.tensor_scalar(out=tmp8[:], in0=gi[:], scalar1=j + 0.5, scalar2=None, op0=ALU.is_lt, op1=ALU.add,
                                                           accum_out=ge[:, j:j + 1]), writes=[bt])
            S.op("dve", lambda h: h.tensor_scalar(out=ge[:], in0=ge[:], scalar1=7.0, scalar2=None, op0=ALU.min), writes=[bt])
            pid7 = sb([128, 1], F32, "pid7")
            S.op("dve", lambda h: h.tensor_scalar(out=pid7[:], in0=pid[:], scalar1=7.0, scalar2=None, op0=ALU.mult), writes=[bu])
            S.op("dve", lambda h: h.tensor_scalar(out=rb1[:], in0=ge[:], scalar1=7168.0, scalar2=pid7[:, 0:1], op0=ALU.mult, op1=ALU.add), reads=[bu], writes=[bt])
            S.op("dve", lambda h: h.tensor_scalar(out=rb2[:], in0=ge[:], scalar1=3584.0, scalar2=pid[:, 0:1], op0=ALU.mult, op1=ALU.add), reads=[bu], writes=[bt])
            for kc in range(8):
                S.op("dve", lambda h, kc=kc: h.tensor_scalar(out=widx1[:, :, kc], in0=rb1[:], scalar1=float(kc * 896), scalar2=None, op0=ALU.add),
                     reads=[bt], writes=[bwidx])
            for fc in range(28):
                S.op("dve", lambda h, fc=fc: h.tensor_scalar(out=widx2[:, :, fc], in0=rb2[:], scalar1=float(fc * 128), scalar2=None, op0=ALU.add),
                     reads=[bt], writes=[bwidx])
            if getattr(kb, "dbg", None) is not None:
                S.dma("sp", kb.dbg["slot1"], slot_i[0][:], reads=[bslot])
                S.dma("sp", kb.dbg["slot2"], slot_i[1][:], reads=[bslot])
                S.dma("sp", kb.dbg["w1"], w1_all[:], reads=[bwa])
                S.dma("sp", kb.dbg["ge"], ge[:], reads=[bt])
                S.dma("sp", kb.dbg["xn10"], xn_row(10), reads=[bxn])
                S.dma("sp", kb.dbg["xn11"], xn_row(11), reads=[bxn])
            for t in range(NT):
                for k in range(2):
                    S.dma("pool", None, None, reads=[bslot, bxn, bxs0], writes=[bxs],
                          fn=lambda h, t=t, k=k: h.indirect_dma_start(out=xs_d[:, :], out_offset=IOA(ap=slot_i[k][:, t:t + 1], axis=0),
                                                                      in_=xn_row(t), in_offset=None))
            S.barrier()
        with ExitStack() as st:
            sb = lambda shape, dt, name=None: kb.sb(st, shape, dt, name)
            xg = sb([128, 8, 1024], BF16, "xg"); bxg = Buf("xg")
            xgT = sb([128, 8, G], BF16, "xgT"); bxgT = Buf("xgT")
            acc = sb([128, 8, 1024], F32, "acc"); bacc = Buf("acc")
            bufs = swiglu_bufs(kb, st, G)
            pT = banks[0][:].bitcast(BF16)
            def gload(j):
                S.dma("sp", xg[:], xs_d[j * G:(j + 1) * G, :].rearrange("(s p) d -> p s d", p=128), reads=[bxs], writes=[bxg])

            def gprep(j):
                for sub in range(8):
                    for kc in range(8):
                        S.op("pe", lambda h, kc=kc, sub=sub: h.transpose(out=pT[:, kc * 128:(kc + 1) * 128], in_=xg[:, sub, kc * 128:(kc + 1) * 128],
                                                                         identity=kb.ident[:]), reads=[bxg, kb.bconst], writes=[PB[0]], inc=(kc == 7))
                    S.op("act", lambda h, sub=sub: h.copy(out=xgT[:, :, sub * 128:(sub + 1) * 128], in_=pT.rearrange("p (a b) -> p a b", a=8)),
                         xr=[PB[0]], writes=[bxgT])
                if j + 1 < ngroups:
                    gload(j + 1)

            gload(0)
            gprep(0)
            for j in range(ngroups):
                hook = (lambda j=j: gprep(j + 1)) if j + 1 < ngroups else None
                def wload(slot, f0, nf, bw, wg_sb, wu_sb, wd_sb, j=j):
                    for kc in range(8):
                        for (wsb, w2) in ((wg_sb, wg2), (wu_sb, wu2)):
                            S.dma("pool", None, None, reads=[bwidx], writes=[bw],
                                  fn=lambda h, wsb=wsb, w2=w2, kc=kc: h.indirect_dma_start(
                                      out=wsb[slot][:, kc, 0:nf * 128], out_offset=None, in_=w2[:, 0:nf * 128],
                                      in_offset=IOA(ap=widx1[:, j, kc:kc + 1], axis=0), element_offset=f0 * 128))
                    for fc in range(nf):
                        S.dma("pool", None, None, reads=[bwidx], writes=[bw],
                              fn=lambda h, fc=fc: h.indirect_dma_start(
                                  out=wd_sb[slot][:, fc, :], out_offset=None, in_=wd2[:, :],
                                  in_offset=IOA(ap=widx2[:, j, f0 + fc:f0 + fc + 1], axis=0)))

                swiglu_acc(kb, bufs, xgT, bxgT, G, None, None, None, EXPERT_DIM, acc, bacc, lambda sub: None, wload=wload, acc_init=True, mid_hook=hook)
                S.dma("sp", y_d[j * G:(j + 1) * G, :].rearrange("(s p) d -> p s d", p=128), acc[:], reads=[bacc], writes=[by])
            S.barrier()
        with ExitStack() as st:
            sb = lambda shape, dt, name=None: kb.sb(st, shape, dt, name)
            xts_5 = [sb([128, 1024], F32, "xt") for _ in range(2)]; bxts_5 = [Buf("x0"), Buf("x1")]
            y1_5 = [sb([128, 1024], F32, "y1_5") for _ in range(2)]; y2_5 = [sb([128, 1024], F32, "y2_5") for _ in range(2)]
            by1_5 = [Buf("y1a"), Buf("y1b")]; by2_5 = [Buf("y2a"), Buf("y2b")]
            junk_5 = sb([128, 1024], BF16, "junk_5"); bj_5 = Buf("junk_5")
            ss_5 = sb([128, 4], F32, "ss_5"); bss_5 = Buf("ss_5")
            ot_5 = [sb([128, 1024], F32, "ot_5") for _ in range(2)]; bot_5 = [Buf("ot0"), Buf("ot1")]
            for t in range(NT):
                r = t % 2
                xt = xts_5[r]; bx = bxts_5[r]
                S.dma("sp", xt[:], h3[t * 128:(t + 1) * 128, :], writes=[bx])
                for (yy, byy, k) in ((y1_5[r], by1_5[r], 0), (y2_5[r], by2_5[r], 1)):
                    S.dma("pool", None, None, reads=[bslot, by], writes=[byy],
                          fn=lambda h, yy=yy, k=k, t=t: h.indirect_dma_start(out=yy[:, :], out_offset=None, in_=y_d[:, :],
                                                                            in_offset=IOA(ap=slot_i[k][:, t:t + 1], axis=0)))
                S.op("dve", lambda h, xt=xt, r=r, t=t: h.scalar_tensor_tensor(out=xt[:], in0=y1_5[r][:], scalar=w1_all[:, t:t + 1], in1=xt[:],
                                                                            op0=ALU.mult, op1=ALU.add), reads=[by1_5[r], bwa], writes=[bx])
                S.op("dve", lambda h, xt=xt, r=r, t=t: h.scalar_tensor_tensor(out=xt[:], in0=y2_5[r][:], scalar=w2_all[:, t:t + 1], in1=xt[:],
                                                                            op0=ALU.mult, op1=ALU.add), reads=[by2_5[r], bwa], writes=[bx])
                S.op("act", lambda h, xt=xt: h.activation(out=junk_5[:], in_=xt[:], func=AF.Square, accum_out=ss_5[:, 0:1]), reads=[bx], writes=[bj_5, bss_5])
                S.op("act", lambda h: h.activation(out=ss_5[:, 1:2], in_=ss_5[:, 0:1], func=AF.Sqrt, scale=1.0 / D, bias=kb.epst[:, 0:1]),
                     reads=[kb.bconst], writes=[bss_5])
                S.op("dve", lambda h: h.reciprocal(out=ss_5[:, 2:3], in_=ss_5[:, 1:2]), writes=[bss_5])
                S.op("dve", lambda h, xt=xt, r=r: h.scalar_tensor_tensor(out=ot_5[r][:], in0=xt[:], scalar=ss_5[:, 2:3], in1=g_fin[:], op0=ALU.mult, op1=ALU.mult),
                     reads=[bx, bss_5, bg], writes=[bot_5[r]])
                S.dma("sp", out[t * 128:(t + 1) * 128, :], ot_5[r][:], reads=[bot_5[r]])
            S.barrier()

IN_SHAPES = {
    "x": [S_LEN, D], "attn_norm": [2, D], "ffn_norm": [2, D], "kv_norm": [D], "final_norm": [D],
    "gla_w_in": [D, GLA_IN], "gla_w_gate_up": [16, 512], "gla_b_gate": [1, 512], "gla_head_norm": [256],
    "gla_w_out": [D, D], "sb_w_kv": [D, 2048], "sb_w_q": [D, D], "sb_w_out": [D, D],
    "ffn_w_gate": [D, FFN_DIM], "ffn_w_up": [D, FFN_DIM], "ffn_w_down": [FFN_DIM, D],
    "moe_w_router": [D, 8], "moe_w_gate": [8, D, EXPERT_DIM], "moe_w_up": [8, D, EXPERT_DIM], "moe_w_down": [8, EXPERT_DIM, D],
}

ALL_STAGES = ("gla", "ffn", "qkv", "sb", "ao", "moe")
SPARSE_MOE = True


def build(stages=ALL_STAGES, ext=()):
    nc = bass.Bass("TRN2", target_bir_lowering=False)
    I = {k: nc.dram_tensor(k, shp, F32, kind="ExternalInput").ap() for k, shp in IN_SHAPES.items()}

    def scratch(name, shape, dt):
        if name in ext and name != "dbg":
            first = ALL_STAGES.index(stages[0])
            prod = {"h1": 0, "h2": 1, "qT": 2, "kT": 2, "v": 2, "oT": 3, "h3": 4}[name]
            kind = "ExternalInput" if prod < first else "ExternalOutput"
            return nc.dram_tensor(name, shape, dt, kind=kind).ap()
        return nc.dram_tensor(name, shape, dt).ap()

    h1 = scratch("h1", [S_LEN, D], F32)
    h2 = scratch("h2", [S_LEN, D], F32)
    qT_d = scratch("qT", [8, 128, S_LEN], BF16)
    kT_d = scratch("kT", [8, 128, S_LEN], BF16)
    v_d = scratch("v", [S_LEN, D], BF16)
    oT_d = scratch("oT", [8, 128, S_LEN], BF16)
    h3 = scratch("h3", [S_LEN, D], F32)
    out = nc.dram_tensor("out", [S_LEN, D], F32, kind="ExternalOutput").ap()
    with ExitStack() as es:
        kb = KB(nc, es)
        if "dbg" in ext:
            kb.dbg = {"slot1": nc.dram_tensor("dbg_slot1", [128, NT], mybir.dt.int32, kind="ExternalOutput").ap(),
                      "slot2": nc.dram_tensor("dbg_slot2", [128, NT], mybir.dt.int32, kind="ExternalOutput").ap(),
                      "w1": nc.dram_tensor("dbg_w1", [128, NT], F32, kind="ExternalOutput").ap(),
                      "ge": nc.dram_tensor("dbg_ge", [128, MOE_NG], F32, kind="ExternalOutput").ap(),
                      "xn10": nc.dram_tensor("dbg_xn10", [128, 1024], BF16, kind="ExternalOutput").ap(),
                      "xn11": nc.dram_tensor("dbg_xn11", [128, 1024], BF16, kind="ExternalOutput").ap()}
        kb.consts()
        if "gla" in stages:
            stage_gla(kb, I["x"], h1, I["attn_norm"][0], I["gla_w_in"], I["gla_w_gate_up"], I["gla_b_gate"],
                      I["gla_head_norm"], I["gla_w_out"])
        if "ffn" in stages:
            stage_ffn(kb, h1, h2, I["ffn_norm"][0], I["ffn_w_gate"], I["ffn_w_up"], I["ffn_w_down"])
        if "qkv" in stages:
            stage_qkv(kb, h2, qT_d, kT_d, v_d, I["kv_norm"], I["attn_norm"][1], I["sb_w_kv"], I["sb_w_q"])
        if "sb" in stages:
            stage_sb(kb, qT_d, kT_d, v_d, oT_d)
        if "ao" in stages:
            stage_attn_out(kb, oT_d, h2, h3, I["sb_w_out"])
        if "moe" in stages and SPARSE_MOE:
            dk = {"kind": "ExternalOutput"} if "dbg" in ext else {}
            xs_d = nc.dram_tensor("xs_d", [MOE_NG * MOE_G, D], BF16, **dk).ap()
            y_d = nc.dram_tensor("y_d", [MOE_NG * MOE_G, D], F32, **dk).ap()
            stage_moe_sparse(kb, h3, out, xs_d, y_d, I["ffn_norm"][1], I["final_norm"], I["moe_w_router"], I["moe_w_gate"], I["moe_w_up"], I["moe_w_down"])
        elif "moe" in stages:
            stage_moe(kb, h3, out, I["ffn_norm"][1], I["final_norm"], I["moe_w_router"], I["moe_w_gate"], I["moe_w_up"], I["moe_w_down"])
        kb.S.emit()
    return nc


def host_inputs(inputs):
    f = lambda a: np.ascontiguousarray(np.asarray(a, dtype=np.float32))
    shared = {
        "attn_norm": f(inputs["attn_norm"]), "ffn_norm": f(inputs["ffn_norm"]), "kv_norm": f(inputs["kv_norm"]),
        "final_norm": f(inputs["final_norm"]), "gla_w_in": f(inputs["gla_w_in"][0]), "gla_w_gate_up": f(inputs["gla_w_gate_up"][0]),
        "gla_b_gate": f(inputs["gla_b_gate"]).reshape(1, 512), "gla_head_norm": f(inputs["gla_head_norm"][0]),
        "gla_w_out": f(inputs["gla_w_out"][0]), "sb_w_kv": f(inputs["sb_w_kv"]), "sb_w_q": f(inputs["sb_w_q"][0]),
        "sb_w_out": f(inputs["sb_w_out"][0]), "ffn_w_gate": f(inputs["ffn_w_gate"][0]), "ffn_w_up": f(inputs["ffn_w_up"][0]),
        "ffn_w_down": f(inputs["ffn_w_down"][0]), "moe_w_router": f(inputs["moe_w_router"][0]),
        "moe_w_gate": f(inputs["moe_w_gate"][0]), "moe_w_up": f(inputs["moe_w_up"][0]), "moe_w_down": f(inputs["moe_w_down"][0]),
    }
    x = f(inputs["x"])
    maps = []
    for b in range(x.shape[0]):
        m = dict(shared)
        m["x"] = x[b]
        maps.append(m)
    return maps


_NC_CACHE = {}


def kernel(**inputs):
    maps = host_inputs(inputs)
    if "full" not in _NC_CACHE:
        _NC_CACHE["full"] = build()
    nc = _NC_CACHE["full"]
    res = run_bass_kernel_spmd(nc, maps, core_ids=list(range(len(maps))))
    return np.stack([np.asarray(r["out"], dtype=np.float32) for r in res.results], axis=0)
```
